# Optimizing a Trainium2 kernel written in Bass

```python
import math
import jax, jax.numpy as jnp
from jax import lax
import numpy as np


D_MODEL = 1024
BATCH = 4
SEQ = 8192
DEPTH = 2

GRID_W = 64
CTX_LEN = 256
N_MIXERS = 2
N_DA_LAYERS = (DEPTH + N_MIXERS - 1) // N_MIXERS
N_GLA_LAYERS = DEPTH // N_MIXERS

DA_HEADS = 8
DA_HEAD_DIM = D_MODEL // (2 * DA_HEADS)
DA_Q_BLOCK = 128
ROPE_BASE = 10000.0
ROPE_PAIRS_AXIS = DA_HEAD_DIM // 4

GLA_HEADS = 4
GLA_DK = D_MODEL // 2
GLA_DV = D_MODEL
GLA_DK_HEAD = GLA_DK // GLA_HEADS
GLA_DV_HEAD = GLA_DV // GLA_HEADS
GLA_GATE_RANK = 16
GLA_TAU = 16.0
GLA_CHUNK = 64
GLA_IN = 2 * GLA_DK + 2 * GLA_DV + 2 * GLA_GATE_RANK

N_EXPERTS = 32
TOP_K = 4
D_EXPERT = D_MODEL
SWIGLU_ALPHA = 1.702
SWIGLU_LIMIT = 7.0
MOE_BLOCK = 256

DEEPNORM_ALPHA = (2.0 * DEPTH) ** 0.25
DEEPNORM_BETA = (8.0 * DEPTH) ** -0.25
NORM_EPS = 1e-5

kernel_name = 'hybrid_diffattn_gla_moe_dit'

F32 = jnp.float32


def layer_norm(x, g, b):
    xf = x.astype(F32)
    mu = jnp.mean(xf, -1, keepdims=True)
    xc = xf - mu
    y = xc * lax.rsqrt(jnp.mean(xc * xc, -1, keepdims=True) + NORM_EPS)
    return (y * g + b).astype(x.dtype)


def rms_norm(x, w):
    xf = x.astype(F32)
    y = xf * lax.rsqrt(jnp.mean(xf * xf, -1, keepdims=True) + NORM_EPS)
    return (y * w).astype(x.dtype)


def lambda_init(layer_idx):
    return 0.8 - 0.6 * math.exp(-0.3 * layer_idx)


def axial_rope(L):
    rows = L // GRID_W
    row = jnp.repeat(jnp.arange(rows), GRID_W).astype(F32)
    col = jnp.tile(jnp.arange(GRID_W), rows).astype(F32)
    inv = ROPE_BASE ** (-jnp.arange(ROPE_PAIRS_AXIS, dtype=F32) / ROPE_PAIRS_AXIS)
    ang = jnp.concatenate([row[:, None] * inv, col[:, None] * inv], -1)
    return jnp.cos(ang), jnp.sin(ang)


def apply_rope(x, cos, sin):
    half = x.shape[-1] // 2
    xf = x.astype(F32)
    x1, x2 = xf[..., :half], xf[..., half:]
    cos = cos[None, :, None, None, :]
    sin = sin[None, :, None, None, :]
    return jnp.concatenate([x1 * cos - x2 * sin, x1 * sin + x2 * cos], -1).astype(x.dtype)


def diff_attend(q, k, v, lam):
    s = jnp.einsum('bqhmd,bkhmd->bhmqk', q, k).astype(F32) * (DA_HEAD_DIM ** -0.5)
    p = jax.nn.softmax(s, axis=-1)
    a = p[:, :, 0] - lam * p[:, :, 1]
    return jnp.einsum('bhqk,bkhe->bqhe', a.astype(v.dtype), v)


def diff_attention_mixer(t_lat, t_ctx, w_in, w_out, lam_vecs, subln_w, lam_init, need_ctx):
    def qkv(t):
        bsz, L, _ = t.shape
        q, k, v = jnp.split(t @ w_in, 3, axis=-1)
        return (q.reshape(bsz, L, DA_HEADS, 2, DA_HEAD_DIM),
                k.reshape(bsz, L, DA_HEADS, 2, DA_HEAD_DIM),
                v.reshape(bsz, L, DA_HEADS, 2 * DA_HEAD_DIM))

    def out(o):
        bsz, L = o.shape[:2]
        o = rms_norm(o, subln_w) * (1.0 - lam_init)
        return o.reshape(bsz, L, D_MODEL) @ w_out

    lv = lam_vecs.astype(F32)
    lam = jnp.exp(jnp.sum(lv[0] * lv[1])) - jnp.exp(jnp.sum(lv[2] * lv[3])) + lam_init
    bsz, L, _ = t_lat.shape
    q, k, v = qkv(t_lat)
    cos, sin = axial_rope(L)
    q = apply_rope(q, cos, sin)
    k = apply_rope(k, cos, sin)
    qc, kc, vc = qkv(t_ctx)
    k_all = jnp.concatenate([kc, k], axis=1)
    v_all = jnp.concatenate([vc, v], axis=1)
    nb = L // DA_Q_BLOCK
    q_blocks = jnp.moveaxis(q.reshape(bsz, nb, DA_Q_BLOCK, DA_HEADS, 2, DA_HEAD_DIM), 1, 0)
    o = lax.map(lambda qb: diff_attend(qb, k_all, v_all, lam), q_blocks)
    o = jnp.moveaxis(o, 0, 1).reshape(bsz, L, DA_HEADS, 2 * DA_HEAD_DIM)
    y_lat = out(o)
    y_ctx = out(diff_attend(qc, kc, vc, lam)) if need_ctx else None
    return y_lat, y_ctx


def gla_project(t, w_in, w_gate, b_gate):
    bsz, L, _ = t.shape
    cuts = [GLA_DK, 2 * GLA_DK, 2 * GLA_DK + GLA_DV, 2 * GLA_DK + 2 * GLA_DV,
            2 * GLA_DK + 2 * GLA_DV + GLA_GATE_RANK]
    q, k, v, r, zf, zb = jnp.split(t @ w_in, cuts, axis=-1)

    def heads(a, dh):
        return a.reshape(bsz, L, GLA_HEADS, dh).transpose(0, 2, 1, 3).astype(F32)

    def log_gate(z, d):
        g = jax.nn.log_sigmoid((z @ w_gate[d] + b_gate[d]).astype(F32)) / GLA_TAU
        return heads(g, GLA_DK_HEAD)

    return (heads(q, GLA_DK_HEAD) * (GLA_DK_HEAD ** -0.5), heads(k, GLA_DK_HEAD),
            heads(v, GLA_DV_HEAD), r, log_gate(zf, 0), log_gate(zb, 1))


def gla_chunk_scan(q, k, v, g, s0):
    bsz, h, L, dk = q.shape
    dv = v.shape[-1]
    n = L // GLA_CHUNK
    q, k, g = (a.reshape(bsz, h, n, GLA_CHUNK, dk) for a in (q, k, g))
    v = v.reshape(bsz, h, n, GLA_CHUNK, dv)
    b = jnp.cumsum(g, axis=3)
    b_last = b[:, :, :, -1:, :]
    q_in = q * jnp.exp(b)
    k_in = k * jnp.exp(-b)
    k_state = k * jnp.exp(b_last - b)
    mask = jnp.tril(jnp.ones((GLA_CHUNK, GLA_CHUNK), bool))
    att = jnp.where(mask, jnp.einsum('bhncd,bhnsd->bhncs', q_in, k_in), 0.0)
    o = jnp.einsum('bhncs,bhnse->bhnce', att, v)
    ds = jnp.einsum('bhncd,bhnce->bhnde', k_state, v)
    decay = jnp.exp(b_last[:, :, :, 0, :])

    def step(s, inp):
        dec, d = inp
        return dec[..., None] * s + d, s

    s_final, s_prev = lax.scan(step, s0, (jnp.moveaxis(decay, 2, 0), jnp.moveaxis(ds, 2, 0)))
    o = o + jnp.einsum('bhncd,bhnde->bhnce', q_in, jnp.moveaxis(s_prev, 0, 2))
    return o.reshape(bsz, h, L, dv), s_final


def gla_final_state(k, v, g):
    bc = jnp.cumsum(g, axis=2)
    w = jnp.exp(bc[:, :, -1:, :] - bc)
    return jnp.einsum('bhld,bhle->bhde', k * w, v)


def gla_output(o, r, norm_w, w_out):
    bsz, _, L, _ = o.shape
    o = rms_norm(o, norm_w).transpose(0, 2, 1, 3).reshape(bsz, L, GLA_DV).astype(r.dtype)
    return (o * jax.nn.silu(r)) @ w_out


def gla_mixer(t_lat, t_ctx, w_in, w_gate, b_gate, norm_w, w_out, need_ctx):
    def flip(a):
        return jnp.flip(a, axis=2)

    q, k, v, r, gf, gb = gla_project(t_lat, w_in, w_gate, b_gate)
    qc, kc, vc, rc, gcf, gcb = gla_project(t_ctx, w_in, w_gate, b_gate)
    if need_ctx:
        s0 = jnp.zeros((kc.shape[0], GLA_HEADS, GLA_DK_HEAD, GLA_DV_HEAD), F32)
        ocf, s_f = gla_chunk_scan(qc, kc, vc, gcf, s0)
        ocb, s_b = gla_chunk_scan(flip(qc), flip(kc), flip(vc), flip(gcb), s0)
        y_ctx = gla_output(ocf + flip(ocb), rc, norm_w, w_out)
    else:
        s_f = gla_final_state(kc, vc, gcf)
        s_b = gla_final_state(flip(kc), flip(vc), flip(gcb))
        y_ctx = None
    of, _ = gla_chunk_scan(q, k, v, gf, s_f)
    ob, _ = gla_chunk_scan(flip(q), flip(k), flip(v), flip(gb), s_b)
    y_lat = gla_output(of + flip(ob), r, norm_w, w_out)
    return y_lat, y_ctx


def moe_ffn(h, router_w, router_b, w_gu, b_gu, w_down, b_down):
    T, D = h.shape
    logits = (h @ router_w + router_b).astype(F32)
    top_logit, top_idx = lax.top_k(logits, TOP_K)
    gates = jax.nn.softmax(top_logit, axis=-1)
    n_assign = T * TOP_K
    flat_e = top_idx.reshape(-1)
    order = jnp.argsort(flat_e)
    sorted_e = flat_e[order]
    sorted_tok = order // TOP_K
    sorted_gate = gates.reshape(-1)[order]
    counts = jnp.bincount(flat_e, length=N_EXPERTS)
    padded = (counts + MOE_BLOCK - 1) // MOE_BLOCK * MOE_BLOCK
    start = jnp.cumsum(counts) - counts
    padded_end = jnp.cumsum(padded)
    padded_start = padded_end - padded
    dest = padded_start[sorted_e] + jnp.arange(n_assign) - start[sorted_e]
    n_blocks = -(-n_assign // MOE_BLOCK) + N_EXPERTS
    n_rows = n_blocks * MOE_BLOCK
    row_tok = jnp.full((n_rows,), T, jnp.int32).at[dest].set(sorted_tok.astype(jnp.int32))
    row_gate = jnp.zeros((n_rows,), F32).at[dest].set(sorted_gate)
    block_expert = jnp.minimum(
        jnp.searchsorted(padded_end, jnp.arange(n_blocks) * MOE_BLOCK, side='right'), N_EXPERTS - 1)
    h_pad = jnp.concatenate([h, jnp.zeros((1, D), h.dtype)], axis=0)
    xb = h_pad[row_tok].reshape(n_blocks, MOE_BLOCK, D)

    def expert_block(args):
        xe, e = args
        gu = xe @ w_gu[e] + b_gu[e]
        glu, lin = jnp.split(gu, 2, axis=-1)
        glu = jnp.minimum(glu, SWIGLU_LIMIT)
        lin = jnp.clip(lin, -SWIGLU_LIMIT, SWIGLU_LIMIT)
        act = glu * jax.nn.sigmoid(SWIGLU_ALPHA * glu) * (lin + 1.0)
        return act @ w_down[e] + b_down[e]

    yb = lax.map(expert_block, (xb, block_expert)).reshape(n_rows, D)
    yb = yb * row_gate[:, None].astype(yb.dtype)
    out = jnp.zeros((T + 1, D), yb.dtype).at[row_tok].add(yb)
    return out[:T]


def setup_inputs(seed: int = 0) -> dict:
    key = jax.random.key(seed)
    ks = jax.random.split(key, 23)
    D = D_MODEL

    def nrm(k, shape, scale):
        return jax.random.normal(k, shape, F32) * scale

    return {
        'x': nrm(ks[0], (BATCH, SEQ, D), 1.0),
        'c': nrm(ks[1], (BATCH, D), 1.0),
        'ctx': nrm(ks[2], (BATCH, CTX_LEN, D), 1.0),
        'c_ctx': nrm(ks[3], (D,), 1.0),
        'ada_w': nrm(ks[4], (DEPTH, D, 6 * D), D ** -0.5),
        'ada_b': nrm(ks[5], (DEPTH, 6 * D), 0.02),
        'ln_g': 1.0 + nrm(ks[6], (DEPTH, 2, D), 0.02),
        'ln_b': nrm(ks[7], (DEPTH, 2, D), 0.02),
        'da_w_in': nrm(ks[8], (N_DA_LAYERS, D, 3 * D), D ** -0.5),
        'da_w_out': nrm(ks[9], (N_DA_LAYERS, D, D), D ** -0.5 * DEEPNORM_BETA),
        'da_lambda': nrm(ks[10], (N_DA_LAYERS, 4, DA_HEAD_DIM), 0.1),
        'da_subln_w': 1.0 + nrm(ks[11], (N_DA_LAYERS, 2 * DA_HEAD_DIM), 0.02),
        'gla_w_in': nrm(ks[12], (N_GLA_LAYERS, D, GLA_IN), D ** -0.5),
        'gla_w_gate': nrm(ks[13], (N_GLA_LAYERS, 2, GLA_GATE_RANK, GLA_DK), GLA_GATE_RANK ** -0.5),
        'gla_b_gate': nrm(ks[14], (N_GLA_LAYERS, 2, GLA_DK), 0.1),
        'gla_norm_w': 1.0 + nrm(ks[15], (N_GLA_LAYERS, GLA_DV_HEAD), 0.02),
        'gla_w_out': nrm(ks[16], (N_GLA_LAYERS, GLA_DV, D), GLA_DV ** -0.5 * DEEPNORM_BETA),
        'router_w': nrm(ks[17], (DEPTH, D, N_EXPERTS), D ** -0.5),
        'router_b': nrm(ks[18], (DEPTH, N_EXPERTS), 0.01),
        'moe_w_gu': nrm(ks[19], (DEPTH, N_EXPERTS, D, 2 * D_EXPERT), D ** -0.5),
        'moe_b_gu': nrm(ks[20], (DEPTH, N_EXPERTS, 2 * D_EXPERT), 0.01),
        'moe_w_down': nrm(ks[21], (DEPTH, N_EXPERTS, D_EXPERT, D), D_EXPERT ** -0.5 * DEEPNORM_BETA),
        'moe_b_down': nrm(ks[22], (DEPTH, N_EXPERTS, D), 0.01),
    }


def reference(x, c, ctx, c_ctx, ada_w, ada_b, ln_g, ln_b, da_w_in, da_w_out, da_lambda,
              da_subln_w, gla_w_in, gla_w_gate, gla_b_gate, gla_norm_w, gla_w_out,
              router_w, router_b, moe_w_gu, moe_b_gu, moe_w_down, moe_b_down):
    bsz, seq, _ = x.shape
    n_ctx = ctx.shape[1]
    silu_c = jax.nn.silu(c)
    silu_cc = jax.nn.silu(c_ctx)
    for i in range(DEPTH):
        last = i == DEPTH - 1
        sh1, sc1, g1, sh2, sc2, g2 = jnp.split((silu_c @ ada_w[i] + ada_b[i])[:, None, :], 6, axis=-1)
        csh1, csc1, cg1, csh2, csc2, cg2 = jnp.split(silu_cc @ ada_w[i] + ada_b[i], 6, axis=-1)
        t_lat = x * (1.0 + sc1) + sh1
        t_ctx = ctx * (1.0 + csc1) + csh1
        j = i // N_MIXERS
        if i % N_MIXERS == 0:
            y_lat, y_ctx = diff_attention_mixer(t_lat, t_ctx, da_w_in[j], da_w_out[j], da_lambda[j],
                                                da_subln_w[j], lambda_init(i), not last)
        else:
            y_lat, y_ctx = gla_mixer(t_lat, t_ctx, gla_w_in[j], gla_w_gate[j], gla_b_gate[j],
                                     gla_norm_w[j], gla_w_out[j], not last)
        x = layer_norm(DEEPNORM_ALPHA * x + g1 * y_lat, ln_g[i, 0], ln_b[i, 0])
        u_lat = (x * (1.0 + sc2) + sh2).reshape(-1, D_MODEL)
        if last:
            f_lat = moe_ffn(u_lat, router_w[i], router_b[i], moe_w_gu[i], moe_b_gu[i],
                            moe_w_down[i], moe_b_down[i])
        else:
            ctx = layer_norm(DEEPNORM_ALPHA * ctx + cg1 * y_ctx, ln_g[i, 0], ln_b[i, 0])
            u_ctx = (ctx * (1.0 + csc2) + csh2).reshape(-1, D_MODEL)
            f = moe_ffn(jnp.concatenate([u_lat, u_ctx], axis=0), router_w[i], router_b[i],
                        moe_w_gu[i], moe_b_gu[i], moe_w_down[i], moe_b_down[i])
            f_lat = f[:bsz * seq]
            ctx = layer_norm(DEEPNORM_ALPHA * ctx + cg2 * f[bsz * seq:].reshape(bsz, n_ctx, D_MODEL),
                             ln_g[i, 1], ln_b[i, 1])
        x = layer_norm(DEEPNORM_ALPHA * x + g2 * f_lat.reshape(bsz, seq, D_MODEL), ln_g[i, 1], ln_b[i, 1])
    return x
```

```python
import numpy as np
from contextlib import ExitStack
import concourse.bass as bass
import concourse.mybir as mybir
from concourse.bass_utils import run_bass_kernel_spmd

F32 = mybir.dt.float32
BF16 = mybir.dt.bfloat16
AF = mybir.ActivationFunctionType
ALU = mybir.AluOpType
AX = mybir.AxisListType

NCTX = 256
NOWN = 4096
NQ = NCTX + NOWN
NK = NQ + NOWN
DM = 1024
ALPHA = (2.0 * 2) ** 0.25
EPS = 1e-5


class T:
    __slots__ = ('t', 'w', 'wf', 'r', 'name')

    def __init__(self, t, name=''):
        self.t = t; self.w = []; self.wf = None; self.r = []; self.name = name

    def __getitem__(self, k):
        return self.t[k]


class Sched:
    ENG = ('tensor', 'vector', 'scalar', 'gpsimd', 'sync')
    EPOCH = 16000
    KDMA = 8

    def __init__(self, nc, stack):
        self.nc = nc; self.stack = stack
        self.cnt = {e: 0 for e in self.ENG}
        self.sems = {e: [] for e in self.ENG}
        self.dcnt = {e: 0 for e in self.ENG}
        self.dsems = {e: [] for e in self.ENG}
        self.waited = {e: {} for e in self.ENG}
        self.nwaits = 0

    def _sem(self, name):
        return self.stack.enter_context(self.nc.semaphore(name))

    def sbuf(self, name, shape, dt, stack=None):
        st = stack or self.stack
        return T(st.enter_context(self.nc.sbuf_tensor(name, list(shape), dt)), name)

    def psum(self, name, shape, dt=F32, stack=None):
        st = stack or self.stack
        return T(st.enter_context(self.nc.psum_tensor(name, list(shape), dt)), name)

    def _need(self, issuer, tok, same_ok=False):
        if tok is None:
            return None
        if tok[0] == 'c':
            _, e, ep, n = tok
            if e == issuer and same_ok:
                return None
            key = ('c', e)
            if (ep, n) <= self.waited[issuer].get(key, (-1, 0)):
                return None
            self.waited[issuer][key] = (ep, n)
            return tok
        _, e, slot, val = tok
        key = ('d', e, slot)
        if self.waited[issuer].get(key, 0) >= val:
            return None
        self.waited[issuer][key] = val
        return tok

    def _deps(self, issuer, reads, writes, pwrites):
        out = []

        def add(tk, same_ok):
            k = self._need(issuer, tk, same_ok)
            if k:
                out.append(k)
        for r in reads:
            for tk in r.w:
                add(tk, False)
        for w in writes:
            for tk in w.w:
                add(tk, True)
            for tk in w.r:
                add(tk, True)
        for w in pwrites:
            add(w.wf, True)
            for tk in w.r:
                add(tk, True)
        return out

    @staticmethod
    def _push(lst, tok):
        if tok[0] == 'c':
            lst[:] = [x for x in lst if not (x[0] == 'c' and x[1] == tok[1])]
        lst.append(tok)

    def _commit(self, tok, reads, writes, pwrites):
        for r in reads:
            self._push(r.r, tok)
        for w in writes:
            w.w = [tok]; w.wf = tok; w.r = []
        for w in pwrites:
            self._push(w.w, tok); w.r = []

    def semof(self, tok):
        if tok[0] == 'c':
            return self.sems[tok[1]][tok[2]], tok[3]
        return self.dsems[tok[1]][tok[2]], tok[3]

    def _emit_waits(self, eng, deps):
        e = getattr(self.nc, eng)
        for d in deps:
            s, v = self.semof(d)
            e.wait_ge(s, v)
            self.nwaits += 1
        return e

    def op(self, eng, fn, reads=(), writes=(), pwrites=()):
        deps = self._deps(eng, reads, writes, pwrites)
        n = self.cnt[eng]; ep, k = divmod(n, self.EPOCH)
        if k == 0:
            self.sems[eng].append(self._sem(f's_{eng}_{ep}'))
        self.cnt[eng] = n + 1
        tok = ('c', eng, ep, k + 1)
        e = self._emit_waits(eng, deps)
        fn(e).then_inc(self.sems[eng][ep], 1)
        self._commit(tok, reads, writes, pwrites)
        return tok

    def dma(self, eng, fn, reads=(), writes=(), pwrites=()):
        deps = self._deps(eng, reads, writes, pwrites)
        i = self.dcnt[eng]; self.dcnt[eng] = i + 1
        rnd, slot = divmod(i, self.KDMA)
        if rnd == 0:
            self.dsems[eng].append(self._sem(f'd_{eng}_{slot}'))
        else:
            k = self._need(eng, ('d', eng, slot, 16 * rnd))
            if k:
                deps.append(k)
        tok = ('d', eng, slot, 16 * (rnd + 1))
        e = self._emit_waits(eng, deps)
        fn(e).then_inc(self.dsems[eng][slot], 16)
        self._commit(tok, reads, writes, pwrites)
        return tok

    def all_tokens(self):
        toks = []
        for e in self.ENG:
            n = self.cnt[e]
            if n:
                ep, k = divmod(n - 1, self.EPOCH)
                toks.append(('c', e, ep, k + 1))
            for i in range(max(0, self.dcnt[e] - self.KDMA), self.dcnt[e]):
                rnd, slot = divmod(i, self.KDMA)
                toks.append(('d', e, slot, 16 * (rnd + 1)))
        return toks

    def barrier(self, engines=None):
        toks = self.all_tokens()
        for e in (engines or self.ENG):
            deps = [k for k in (self._need(e, t, same_ok=True) for t in toks) if k]
            self._emit_waits(e, deps)


def _dram(nc, name, shape, dt, kind="Internal"):
    return nc.dram_tensor(name, list(shape), dt, kind=kind).ap()


class RR:
    def __init__(self, items):
        self.items = items; self.i = 0

    def __call__(self):
        x = self.items[self.i % len(self.items)]; self.i += 1
        return x


def emit_mods(S, nc, st, cT, ada_w, ada_b, mods_d, modsT):
    cs = S.sbuf("m_cs", [128, 8, 2], F32, st)
    S.dma('sync', lambda e: e.dma_start(out=cs[:], in_=cT.rearrange("(kc p) m -> p kc m", p=128)),
          writes=[cs])
    S.op('scalar', lambda e: e.activation(out=cs[:], in_=cs[:], func=AF.Silu), reads=[cs], writes=[cs])
    ab = S.sbuf("m_ab", [2, 6144], F32, st)
    S.dma('sync', lambda e: e.dma_start(out=ab[:], in_=ada_b.partition_broadcast(2)), writes=[ab])
    msb = S.sbuf("m_sb", [2, 6144], F32, st)
    aw = [S.sbuf(f"m_aw{i}", [128, 8, 512], F32, st) for i in range(2)]
    ps = [S.psum(f"m_ps{i}", [2, 512], F32, st) for i in range(2)]
    awv = ada_w.rearrange("(kc p) n -> p kc n", p=128)
    for nb in range(12):
        a = aw[nb % 2]; p = ps[nb % 2]
        S.dma('sync', lambda e, a=a, nb=nb: e.dma_start(out=a[:], in_=awv[:, :, nb * 512:(nb + 1) * 512]),
              writes=[a])
        for kc in range(8):
            S.op('tensor', lambda e, a=a, p=p, kc=kc: e.matmul(p[:], lhsT=cs[:, kc, :], rhs=a[:, kc, :],
                                                                start=(kc == 0), stop=(kc == 7)),
                 reads=[cs, a], writes=[p])
        S.op('vector', lambda e, p=p, nb=nb: e.tensor_tensor(out=msb[:, nb * 512:(nb + 1) * 512], in0=p[:],
                                                             in1=ab[:, nb * 512:(nb + 1) * 512], op=ALU.add),
             reads=[p, ab], pwrites=[msb])
    S.dma('sync', lambda e: e.dma_start(out=mods_d[:, :], in_=msb[:]), reads=[msb], writes=[modsT])


def load_bcast(S, st, name, src_row, modsT=None, eng='sync'):
    t = S.sbuf(name, [128, DM], F32, st)
    S.dma(eng, lambda e: e.dma_start(out=t[:], in_=src_row.partition_broadcast(128)),
          reads=([modsT] if modsT is not None else []), writes=[t])
    return t


def build_attn(lam_init):
    nc = bass.Bass("TRN2", target_bir_lowering=False)
    EI = "ExternalInput"
    xk = _dram(nc, "xk", [DM, NK], F32, EI)
    xtm = _dram(nc, "xtm", [NQ, DM], F32, EI)
    ropeC = _dram(nc, "ropeC", [128, NK], F32, EI)
    ropeS = _dram(nc, "ropeS", [128, NK], F32, EI)
    cT = _dram(nc, "cT", [DM, 2], F32, EI)
    ada_w = _dram(nc, "ada_w", [DM, 6 * DM], F32, EI)
    ada_b = _dram(nc, "ada_b", [1, 6 * DM], F32, EI)
    w_in = _dram(nc, "w_in", [DM, 3 * DM], F32, EI)
    w_perm = _dram(nc, "w_perm", [DM, 2 * DM], F32, EI)
    w_out = _dram(nc, "w_out", [DM, DM], F32, EI)
    lamv = _dram(nc, "lamv", [1, 256], F32, EI)
    subln = _dram(nc, "subln", [1, 128], F32, EI)
    ln_g = _dram(nc, "ln_g", [1, DM], F32, EI)
    ln_b = _dram(nc, "ln_b", [1, DM], F32, EI)
    ident_d = _dram(nc, "ident", [128, 128], F32, EI)
    x1 = _dram(nc, "x1", [NQ, DM], F32, "ExternalOutput")
    mods_d = _dram(nc, "mods", [2, 6 * DM], F32, "ExternalOutput")
    QT = _dram(nc, "QT", [8, 128, NQ], BF16)
    KT = _dram(nc, "KT", [8, 128, NK], BF16)
    Vd = _dram(nc, "Vd", [8, NK, 129], BF16)

    with ExitStack() as st0:
        S = Sched(nc, st0)
        modsT = T(mods_d, "mods")
        QTt = [T(QT[h], f"QT{h}") for h in range(8)]
        KTt = [T(KT[h], f"KT{h}") for h in range(8)]
        Vt = [T(Vd[h], f"V{h}") for h in range(8)]
        x1T = T(x1, "x1")
        with ExitStack() as st:
            emit_mods(S, nc, st, cT, ada_w, ada_b, mods_d, modsT)
            S.barrier()
        modp = S.sbuf("modp", [128, 2, 6, 8], F32)
        with nc.allow_non_contiguous_dma(reason="tiny per-partition mod vectors"):
            for m in range(2):
                S.dma('sync', lambda e, m=m: e.dma_start(
                    out=modp[:, m], in_=mods_d[m:m + 1, :].rearrange("o (j kc p) -> p (o j) kc", j=6, kc=8, p=128)),
                    reads=[modsT], pwrites=[modp])
        onep = S.sbuf("onep", [128, 2, 8], F32)
        S.op('vector', lambda e: e.tensor_scalar_add(out=onep[:], in0=modp[:, :, 1, :], scalar1=1.0),
             reads=[modp], writes=[onep])

        with ExitStack() as st:
            wi = S.sbuf("wi", [128, 8, 3 * DM], BF16, st)
            wp = S.sbuf("wp", [128, 8, 2 * DM], BF16, st)
            wiv = w_in.rearrange("(kc p) n -> p kc n", p=128)
            wpv = w_perm.rearrange("(kc p) n -> p kc n", p=128)
            for kc in range(8):
                for c0 in range(0, 3 * DM, 1024):
                    S.dma('gpsimd', lambda e, kc=kc, c0=c0: e.dma_start(out=wi[:, kc, c0:c0 + 1024],
                                                                        in_=wiv[:, kc, c0:c0 + 1024]), pwrites=[wi])
                for c0 in range(0, 2 * DM, 1024):
                    S.dma('gpsimd', lambda e, kc=kc, c0=c0: e.dma_start(out=wp[:, kc, c0:c0 + 1024],
                                                                        in_=wpv[:, kc, c0:c0 + 1024]), pwrites=[wp])
            xb = [S.sbuf(f"xb{i}", [128, 8, 512], F32, st) for i in range(2)]
            tb = [S.sbuf(f"tb{i}", [128, 8, 512], BF16, st) for i in range(2)]
            rc = [S.sbuf(f"rc{i}", [128, 512], F32, st) for i in range(2)]
            rs = [S.sbuf(f"rs{i}", [128, 512], F32, st) for i in range(2)]
            psA = [S.psum(f"psA{i}", [128, 512], F32, st) for i in range(2)]
            psB = [S.psum(f"psB{i}", [128, 512], F32, st) for i in range(2)]
            psV = [S.psum(f"psV{i}", [128, 512], F32, st) for i in range(2)]
            t1 = [S.sbuf(f"t1_{i}", [128, 512], F32, st) for i in range(2)]
            t2 = [S.sbuf(f"t2_{i}", [128, 512], F32, st) for i in range(2)]
            qk = [S.sbuf(f"qk{i}", [128, 512], BF16, st) for i in range(4)]
            vs = [S.sbuf(f"vs{i}", [128, 8, 129], BF16, st) for i in range(3)]
            for v in vs:
                S.op('gpsimd', lambda e, v=v: e.memset(v[:], 1.0), writes=[v])
            xkv = xk.rearrange("(kc p) t -> p kc t", p=128)
            blocks = [(0, NCTX)] + [(NCTX + i * 512, 512) for i in range(16)]
            iqk = 0; ivs = 0; ips = 0
            for bi, (t0, nt) in enumerate(blocks):
                X = xb[bi % 2]; TB = tb[bi % 2]; RC = rc[bi % 2]; RS = rs[bi % 2]
                mset = 1 if bi == 0 else 0
                S.dma('sync', lambda e, X=X, t0=t0, nt=nt: e.dma_start(out=X[:, :, :nt], in_=xkv[:, :, t0:t0 + nt]),
                      writes=[X])
                S.dma('sync', lambda e, RC=RC, t0=t0, nt=nt: e.dma_start(out=RC[:, :nt], in_=ropeC[:, t0:t0 + nt]),
                      writes=[RC])
                S.dma('sync', lambda e, RS=RS, t0=t0, nt=nt: e.dma_start(out=RS[:, :nt], in_=ropeS[:, t0:t0 + nt]),
                      writes=[RS])
                for kc in range(8):
                    eng = 'vector' if kc % 2 == 0 else 'gpsimd'
                    S.op(eng, lambda e, X=X, TB=TB, kc=kc, nt=nt, mset=mset: e.tensor_scalar(
                        out=TB[:, kc, :nt], in0=X[:, kc, :nt], scalar1=onep[:, mset, kc:kc + 1],
                        scalar2=modp[:, mset, 0, kc:kc + 1], op0=ALU.mult, op1=ALU.add),
                        reads=[X, onep, modp], pwrites=[TB])
                has_q = t0 < NQ
                ccs = ([('q', h) for h in range(8)] if has_q else []) + [('k', h) for h in range(8)]
                for kind, h in ccs:
                    c0 = (0 if kind == 'q' else DM) + h * 128
                    pa = psA[ips % 2]; pb = psB[ips % 2]; a1 = t1[ips % 2]; a2 = t2[ips % 2]; ips += 1
                    for kc in range(8):
                        S.op('tensor', lambda e, pa=pa, TB=TB, kc=kc, c0=c0, nt=nt: e.matmul(
                            pa[:, :nt], lhsT=wi[:, kc, c0:c0 + 128], rhs=TB[:, kc, :nt], start=(kc == 0), stop=(kc == 7)),
                            reads=[wi, TB], writes=[pa])
                    for kc in range(8):
                        S.op('tensor', lambda e, pb=pb, TB=TB, kc=kc, c0=c0, nt=nt: e.matmul(
                            pb[:, :nt], lhsT=wp[:, kc, c0:c0 + 128], rhs=TB[:, kc, :nt], start=(kc == 0), stop=(kc == 7)),
                            reads=[wp, TB], writes=[pb])
                    S.op('vector', lambda e, pa=pa, a1=a1, RC=RC, nt=nt: e.tensor_tensor(
                        out=a1[:, :nt], in0=pa[:, :nt], in1=RC[:, :nt], op=ALU.mult), reads=[pa, RC], writes=[a1])
                    S.op('vector', lambda e, pb=pb, a2=a2, RS=RS, nt=nt: e.tensor_tensor(
                        out=a2[:, :nt], in0=pb[:, :nt], in1=RS[:, :nt], op=ALU.mult), reads=[pb, RS], writes=[a2])
                    o = qk[iqk % 4]; iqk += 1
                    S.op('gpsimd', lambda e, o=o, a1=a1, a2=a2, nt=nt: e.tensor_tensor(
                        out=o[:, :nt], in0=a1[:, :nt], in1=a2[:, :nt], op=ALU.add), reads=[a1, a2], writes=[o])
                    if kind == 'q':
                        S.dma('sync', lambda e, o=o, h=h, t0=t0, nt=nt: e.dma_start(out=QT[h, :, t0:t0 + nt], in_=o[:, :nt]),
                              reads=[o], pwrites=[QTt[h]])
                    else:
                        S.dma('sync', lambda e, o=o, h=h, t0=t0, nt=nt: e.dma_start(out=KT[h, :, t0:t0 + nt], in_=o[:, :nt]),
                              reads=[o], pwrites=[KTt[h]])
                for ti in range(nt // 128):
                    V = vs[ivs % 3]; ivs += 1
                    for nh in range(2):
                        pv = psV[nh]
                        for kc in range(8):
                            S.op('tensor', lambda e, pv=pv, TB=TB, kc=kc, ti=ti, nh=nh: e.matmul(
                                pv[:], lhsT=TB[:, kc, ti * 128:(ti + 1) * 128],
                                rhs=wi[:, kc, 2 * DM + nh * 512:2 * DM + (nh + 1) * 512], start=(kc == 0), stop=(kc == 7)),
                                reads=[wi, TB], writes=[pv])
                        S.op('scalar', lambda e, pv=pv, V=V, nh=nh: e.activation(
                            out=V[:, nh * 4:(nh + 1) * 4, 0:128], in_=pv[:].rearrange("p (h d) -> p h d", h=4),
                            func=AF.Copy), reads=[pv], pwrites=[V])
                    r0 = t0 + ti * 128
                    S.dma('sync', lambda e, V=V, r0=r0: e.dma_start(
                        out=Vd[:, r0:r0 + 128, :].rearrange("h t d -> t h d"), in_=V[:]),
                        reads=[V], pwrites=Vt)

        S.barrier()
        onT = S.sbuf("onT", [128, 8, NQ], BF16)
        with ExitStack() as st:
            ident = S.sbuf("ident_sb", [128, 128], BF16, st)
            S.dma('gpsimd', lambda e: e.dma_start(out=ident[:], in_=ident_d[:, :]), writes=[ident])
            lv = S.sbuf("lv", [1, 256], F32, st)
            S.dma('sync', lambda e: e.dma_start(out=lv[:], in_=lamv[:, :]), writes=[lv])
            pr = S.sbuf("pr", [1, 2, 64], F32, st)
            lvv = lv[:].rearrange("p (a b c) -> p a b c", a=2, b=2)
            S.op('vector', lambda e: e.tensor_tensor(out=pr[:], in0=lvv[:, :, 0, :], in1=lvv[:, :, 1, :], op=ALU.mult),
                 reads=[lv], writes=[pr])
            sm = S.sbuf("sm", [1, 2], F32, st)
            S.op('vector', lambda e: e.reduce_sum(out=sm[:], in_=pr[:], axis=AX.X), reads=[pr], writes=[sm])
            S.op('scalar', lambda e: e.activation(out=sm[:], in_=sm[:], func=AF.Exp), reads=[sm], writes=[sm])
            lam1 = S.sbuf("lam1", [1, 1], F32, st)
            S.op('vector', lambda e: e.tensor_tensor(out=lam1[:], in0=sm[:, 0:1], in1=sm[:, 1:2], op=ALU.subtract),
                 reads=[sm], writes=[lam1])
            S.op('vector', lambda e: e.tensor_scalar(out=lam1[:], in0=lam1[:], scalar1=float(lam_init), scalar2=-1.0,
                                                     op0=ALU.add, op1=ALU.mult), reads=[lam1], writes=[lam1])
            ones1 = S.sbuf("ones1", [1, 128], F32, st)
            S.op('vector', lambda e: e.memset(ones1[:], 1.0), writes=[ones1])
            psT = S.psum("psT", [128, 1024], BF16, st)
            psl = S.psum("psl", [128, 512], F32, st)
            S.op('tensor', lambda e: e.matmul(psl[:, 0:1], lhsT=ones1[0:1, :], rhs=lam1[0:1, 0:1], start=True, stop=True),
                 reads=[ones1, lam1], writes=[psl])
            nlam = S.sbuf("nlam", [128, 1], F32, st)
            S.op('vector', lambda e: e.tensor_copy(out=nlam[:], in_=psl[:, 0:1]), reads=[psl], writes=[nlam])
            sw = S.sbuf("sw", [128, 128], F32, st)
            S.dma('sync', lambda e: e.dma_start(out=sw[:], in_=subln.partition_broadcast(128)), writes=[sw])
            S.op('vector', lambda e: e.tensor_scalar_mul(out=sw[:], in0=sw[:], scalar1=float(1.0 - lam_init)),
                 reads=[sw], writes=[sw])

            KTs = [S.sbuf(f"KTs{i}", [128, NK], BF16, st) for i in range(2)]
            Vs = [S.sbuf(f"Vs{i}", [128, 66, 129], BF16, st) for i in range(2)]
            QTs = [S.sbuf(f"QTs{i}", [128, NQ], BF16, st) for i in range(2)]
            psS = [S.psum(f"psS{i}", [128, 512], F32, st) for i in range(3)]
            psO = [S.psum(f"psO{i}", [128, 512], F32, st) for i in range(3)]
            pts = [S.sbuf(f"pt{i}", [128, 512], BF16, st) for i in range(3)]
            Om = [S.sbuf(f"Om{i}", [128, 4, 129], F32, st) for i in range(2)]
            rec = S.sbuf("rec", [128, 2, 4], F32, st)
            osb = S.sbuf("osb", [128, 128], F32, st)
            sq = S.sbuf("sq", [128, 128], F32, st)
            ssq = S.sbuf("ssq", [128, 1], F32, st)
            onb = [S.sbuf(f"onb{i}", [128, 128], BF16, st) for i in range(2)]
            qblocks = [(0, NCTX, NCTX // 128)] + [(NCTX + i * 512, 512, NK // 128) for i in range(8)]
            iS = 0; iO = 0; iT = 0
            for h in range(8):
                KS = KTs[h % 2]; VS = Vs[h % 2]; QS = QTs[h % 2]
                S.dma('sync', lambda e, KS=KS, h=h: e.dma_start(out=KS[:], in_=KT[h]), reads=[KTt[h]], writes=[KS])
                S.dma('sync', lambda e, VS=VS, h=h: e.dma_start(out=VS[:], in_=Vd[h].rearrange("(kc p) d -> p kc d", p=128)),
                      reads=[Vt[h]], writes=[VS])
                S.dma('sync', lambda e, QS=QS, h=h: e.dma_start(out=QS[:], in_=QT[h]), reads=[QTt[h]], writes=[QS])
                for (q0, nq, nkc) in qblocks:
                    nqs = nq // 128
                    for m in range(2):
                        pO = [psO[iO % 3], psO[(iO + 1) % 3]] if nqs > 2 else [psO[iO % 3]]
                        iO += len(pO)
                        for kc in range(nkc):
                            pS = psS[iS % 3]; PT = pts[iS % 3]; iS += 1
                            S.op('tensor', lambda e, pS=pS, KS=KS, QS=QS, m=m, kc=kc, q0=q0, nq=nq: e.matmul(
                                pS[:, :nq], lhsT=KS[m * 64:(m + 1) * 64, kc * 128:(kc + 1) * 128],
                                rhs=QS[m * 64:(m + 1) * 64, q0:q0 + nq], start=True, stop=True),
                                reads=[KS, QS], writes=[pS])
                            S.op('scalar', lambda e, pS=pS, PT=PT, nq=nq: e.activation(
                                out=PT[:, :nq], in_=pS[:, :nq], func=AF.Exp, scale=0.125), reads=[pS], writes=[PT])
                            for qs in range(nqs):
                                po = pO[qs // 2]; c0 = (qs % 2) * 129
                                S.op('tensor', lambda e, po=po, c0=c0, PT=PT, VS=VS, qs=qs, kc=kc, nkc=nkc: e.matmul(
                                    po[:, c0:c0 + 129], lhsT=PT[:, qs * 128:(qs + 1) * 128], rhs=VS[:, kc, :],
                                    start=(kc == 0), stop=(kc == nkc - 1)), reads=[PT, VS], writes=[po])
                        for j, po in enumerate(pO):
                            S.op('vector', lambda e, po=po, j=j, m=m: e.tensor_copy(
                                out=Om[m][:, 2 * j:2 * j + 2, :], in_=po[:, 0:258].rearrange("p (a b) -> p a b", a=2)),
                                reads=[po], pwrites=[Om[m]])
                    S.op('vector', lambda e, nqs=nqs: e.reciprocal(out=rec[:, 0, :nqs], in_=Om[0][:, :nqs, 128]),
                         reads=[Om[0]], pwrites=[rec])
                    S.op('vector', lambda e, nqs=nqs: e.reciprocal(out=rec[:, 1, :nqs], in_=Om[1][:, :nqs, 128]),
                         reads=[Om[1]], pwrites=[rec])
                    S.op('vector', lambda e, nqs=nqs: e.tensor_scalar_mul(out=rec[:, 1, :nqs], in0=rec[:, 1, :nqs],
                                                                          scalar1=nlam[:, 0:1]),
                         reads=[rec, nlam], writes=[rec])
                    for qs in range(nqs):
                        S.op('vector', lambda e, qs=qs: e.tensor_scalar_mul(out=osb[:], in0=Om[0][:, qs, 0:128],
                                                                            scalar1=rec[:, 0, qs:qs + 1]),
                             reads=[Om[0], rec], writes=[osb])
                        S.op('vector', lambda e, qs=qs: e.scalar_tensor_tensor(
                            out=osb[:], in0=Om[1][:, qs, 0:128], scalar=rec[:, 1, qs:qs + 1], in1=osb[:],
                            op0=ALU.mult, op1=ALU.add), reads=[Om[1], rec, osb], writes=[osb])
                        S.op('gpsimd', lambda e: e.tensor_tensor(out=sq[:], in0=osb[:], in1=osb[:], op=ALU.mult),
                             reads=[osb], writes=[sq])
                        S.op('vector', lambda e: e.reduce_sum(out=ssq[:], in_=sq[:], axis=AX.X), reads=[sq], writes=[ssq])
                        S.op('vector', lambda e: e.tensor_scalar(out=ssq[:], in0=ssq[:], scalar1=1.0 / 128, scalar2=EPS,
                                                                 op0=ALU.mult, op1=ALU.add), reads=[ssq], writes=[ssq])
                        S.op('scalar', lambda e: e.activation(out=ssq[:], in_=ssq[:], func=AF.Sqrt), reads=[ssq], writes=[ssq])
                        S.op('vector', lambda e: e.reciprocal(out=ssq[:], in_=ssq[:]), reads=[ssq], writes=[ssq])
                        ob = onb[iT % 2]; iT += 1
                        S.op('vector', lambda e, ob=ob: e.scalar_tensor_tensor(
                            out=ob[:], in0=osb[:], scalar=ssq[:, 0:1], in1=sw[:], op0=ALU.mult, op1=ALU.mult),
                            reads=[osb, ssq, sw], writes=[ob])
                        S.op('tensor', lambda e, ob=ob: e.transpose(out=psT[:, 0:128], in_=ob[:], identity=ident[:]),
                             reads=[ob, ident], writes=[psT])
                        tq = q0 + qs * 128
                        S.op('scalar', lambda e, h=h, tq=tq: e.copy(out=onT[:, h, tq:tq + 128], in_=psT[:, 0:128]),
                             reads=[psT], pwrites=[onT])

        S.barrier()
        with ExitStack() as st:
            wo = S.sbuf("wo", [128, 8, DM], BF16, st)
            S.dma('gpsimd', lambda e: e.dma_start(out=wo[:], in_=w_out.rearrange("(kc p) n -> p kc n", p=128)),
                  writes=[wo])
            g1b = [load_bcast(S, st, f"g1b{m}", mods_d[m:m + 1, 2 * DM:3 * DM], modsT) for m in range(2)]
            lngb = load_bcast(S, st, "lngb", ln_g)
            lnbb = load_bcast(S, st, "lnbb", ln_b)
            psY = [S.psum(f"psY{i}", [128, 512], F32, st) for i in range(4)]
            xts = [S.sbuf(f"xts{i}", [128, DM], F32, st) for i in range(2)]
            zs = [S.sbuf(f"zs{i}", [128, DM], F32, st) for i in range(2)]
            x1s = [S.sbuf(f"x1s{i}", [128, DM], F32, st) for i in range(2)]
            lnsc = LNScratch(S, st, "lnc")
            outs = []
            for ti in range(NQ // 128):
                mset = 1 if ti < 2 else 0
                xt = xts[ti % 2]; z = zs[ti % 2]; xo = x1s[ti % 2]
                S.dma('sync', lambda e, xt=xt, ti=ti: e.dma_start(out=xt[:], in_=xtm[ti * 128:(ti + 1) * 128, :]), writes=[xt])
                for nh in range(2):
                    py = psY[(ti % 2) * 2 + nh]
                    for h in range(8):
                        S.op('tensor', lambda e, py=py, h=h, ti=ti, nh=nh: e.matmul(
                            py[:], lhsT=onT[:, h, ti * 128:(ti + 1) * 128], rhs=wo[:, h, nh * 512:(nh + 1) * 512],
                            start=(h == 0), stop=(h == 7)), reads=[onT, wo], writes=[py])
                    S.op('vector', lambda e, py=py, z=z, nh=nh, mset=mset: e.tensor_tensor(
                        out=z[:, nh * 512:(nh + 1) * 512], in0=py[:], in1=g1b[mset][:, nh * 512:(nh + 1) * 512], op=ALU.mult),
                        reads=[py, g1b[mset]], pwrites=[z])
                S.op('vector', lambda e, xt=xt, z=z: e.scalar_tensor_tensor(
                    out=z[:], in0=xt[:], scalar=ALPHA, in1=z[:], op0=ALU.mult, op1=ALU.add), reads=[xt, z], writes=[z])
                emit_ln(S, lnsc, z, xo, lngb, lnbb)
                outs.append(S.dma('sync', lambda e, xo=xo, ti=ti: e.dma_start(out=x1[ti * 128:(ti + 1) * 128, :], in_=xo[:]),
                                  reads=[xo], pwrites=[x1T]))
        S.barrier()
    return nc


class LNScratch:
    def __init__(self, S, st, pfx):
        self.stats = S.sbuf(pfx + "_st", [128, 2, 6], F32, st)
        self.mv = S.sbuf(pfx + "_mv", [128, 2], F32, st)
        self.rstd = S.sbuf(pfx + "_rs", [128, 1], F32, st)


def emit_ln(S, sc, z, out, lng, lnb, eng2='gpsimd'):
    for i in range(2):
        S.op('vector', lambda e, i=i: e.bn_stats(out=sc.stats[:, i, :], in_=z[:, i * 512:(i + 1) * 512]),
             reads=[z], pwrites=[sc.stats])
    S.op('vector', lambda e: e.bn_aggr(out=sc.mv[:], in_=sc.stats[:].rearrange("p a b -> p (a b)")),
         reads=[sc.stats], writes=[sc.mv])
    S.op('vector', lambda e: e.tensor_scalar_add(out=sc.rstd[:], in0=sc.mv[:, 1:2], scalar1=EPS),
         reads=[sc.mv], writes=[sc.rstd])
    S.op('scalar', lambda e: e.activation(out=sc.rstd[:], in_=sc.rstd[:], func=AF.Sqrt), reads=[sc.rstd], writes=[sc.rstd])
    S.op('vector', lambda e: e.reciprocal(out=sc.rstd[:], in_=sc.rstd[:]), reads=[sc.rstd], writes=[sc.rstd])
    S.op('vector', lambda e: e.tensor_scalar(out=z[:], in0=z[:], scalar1=sc.mv[:, 0:1], scalar2=sc.rstd[:, 0:1],
                                             op0=ALU.subtract, op1=ALU.mult), reads=[z, sc.mv, sc.rstd], writes=[z])
    S.op(eng2, lambda e: e.tensor_tensor(out=z[:], in0=z[:], in1=lng[:], op=ALU.mult),
         reads=[z, lng], writes=[z])
    S.op(eng2, lambda e: e.tensor_tensor(out=out[:], in0=z[:], in1=lnb[:], op=ALU.add),
         reads=[z, lnb], writes=[out])


def _lambda_init(layer_idx):
    import math
    return 0.8 - 0.6 * math.exp(-0.3 * layer_idx)


def _rope_tables(pos, n_ctx):
    pos = np.asarray(pos)
    row = (pos // 64).astype(np.float32); col = (pos % 64).astype(np.float32)
    inv = (np.float32(10000.0) ** (-np.arange(16, dtype=np.float32) / np.float32(16))).astype(np.float32)
    ang = np.concatenate([row[:, None] * inv, col[:, None] * inv], -1).astype(np.float32)
    cos = np.cos(ang).astype(np.float32); sin = np.sin(ang).astype(np.float32)
    C64 = np.concatenate([cos, cos], -1); S64 = np.concatenate([-sin, sin], -1)
    C = np.concatenate([np.ones((n_ctx, 64), np.float32), C64], 0)
    Sg = np.concatenate([np.zeros((n_ctx, 64), np.float32), S64], 0)
    C = np.concatenate([C, C], -1).T; Sg = np.concatenate([Sg, Sg], -1).T
    return np.ascontiguousarray(C), np.ascontiguousarray(Sg)


def _perm_cols():
    idx = np.arange(2 * DM).reshape(2, 8, 2, 64)
    return np.concatenate([idx[..., 32:], idx[..., :32]], -1).reshape(-1)


def prep_attn(inp, core, shared):
    b, hf = divmod(core, 2)
    x = inp['x'][b]; ctx = inp['ctx'][b]
    own = x[hf * NOWN:(hf + 1) * NOWN]; oth = x[(1 - hf) * NOWN:(2 - hf) * NOWN]
    pos_own = np.arange(hf * NOWN, (hf + 1) * NOWN)
    if hf == 1:
        own = own[::-1]; ctx = ctx[::-1]; pos_own = pos_own[::-1]
    pos = np.concatenate([pos_own, np.arange((1 - hf) * NOWN, (2 - hf) * NOWN)])
    C, Sg = _rope_tables(pos, NCTX)
    d = dict(shared)
    d.update(
        xk=np.ascontiguousarray(np.concatenate([ctx, own, oth], 0).T),
        xtm=np.ascontiguousarray(np.concatenate([ctx, own], 0)),
        ropeC=C, ropeS=Sg,
        cT=np.ascontiguousarray(np.stack([inp['c'][b], inp['c_ctx']], 1)),
    )
    return d


def shared_attn(inp):
    w_in = inp['da_w_in'][0]
    return dict(
        ada_w=inp['ada_w'][0], ada_b=inp['ada_b'][0][None, :],
        w_in=w_in, w_perm=np.ascontiguousarray(w_in[:, :2 * DM][:, _perm_cols()]),
        w_out=inp['da_w_out'][0], lamv=inp['da_lambda'][0].reshape(1, 256),
        subln=inp['da_subln_w'][0][None, :], ln_g=inp['ln_g'][0, 0][None, :], ln_b=inp['ln_b'][0, 0][None, :],
        ident=np.eye(128, dtype=np.float32),
    )


def emit_moe(S, nc, xin, xinT, mods_d, modsT, router_w, router_b, w_gu, b_gu, w_down, b_down, ln_g, ln_b,
             ident_d, xout, xoutT, ntiles, nctx_tiles):
    NE = 32
    group = -(-ntiles // 3)
    with ExitStack() as st:
        identb = S.sbuf("mo_identb", [128, 128], BF16, st)
        S.dma('gpsimd', lambda e: e.dma_start(out=identb[:], in_=ident_d[:, :]), writes=[identb])
        identf = S.sbuf("mo_identf", [128, 128], F32, st)
        S.dma('sync', lambda e: e.dma_start(out=identf[:], in_=ident_d[:, :]), writes=[identf])
        nsets = 2 if nctx_tiles else 1
        sc2b = [load_bcast(S, st, f"mo_sc2b{m}", mods_d[m:m + 1, 4 * DM:5 * DM], modsT) for m in range(nsets)]
        sh2b = [load_bcast(S, st, f"mo_sh2b{m}", mods_d[m:m + 1, 3 * DM:4 * DM], modsT) for m in range(nsets)]
        g2b = [load_bcast(S, st, f"mo_g2b{m}", mods_d[m:m + 1, 5 * DM:6 * DM], modsT) for m in range(nsets)]
        for t in sc2b:
            S.op('gpsimd', lambda e, t=t: e.tensor_scalar_add(out=t[:], in0=t[:], scalar1=1.0), reads=[t], writes=[t])
        lngb = load_bcast(S, st, "mo_lngb", ln_g)
        lnbb = load_bcast(S, st, "mo_lnbb", ln_b)
        rw = S.sbuf("mo_rw", [128, 8, NE], BF16, st)
        S.dma('gpsimd', lambda e: e.dma_start(out=rw[:], in_=router_w.rearrange("(kc p) n -> p kc n", p=128)), writes=[rw])
        rbb = S.sbuf("mo_rbb", [128, NE], F32, st)
        S.dma('sync', lambda e: e.dma_start(out=rbb[:], in_=router_b.partition_broadcast(128)), writes=[rbb])
        bdn = S.sbuf("mo_bdn", [NE, DM], F32, st)
        S.dma('sync', lambda e: e.dma_start(out=bdn[:], in_=b_down[:, :]), writes=[bdn])
        bguT = S.sbuf("mo_bguT", [128, 16, NE], F32, st)
        st_tmp = ExitStack()
        bsb = S.sbuf("mo_bsb", [NE, 2 * DM], F32, st_tmp)
        S.dma('sync', lambda e: e.dma_start(out=bsb[:], in_=b_gu[:, :]), writes=[bsb])
        psX = [S.psum(f"mo_psX{i}", [128, 512], F32, st) for i in range(2)]
        psXb = S.psum("mo_psXb", [128, 1024], BF16, st)
        for c in range(16):
            p = psX[c % 2]
            S.op('tensor', lambda e, p=p, c=c: e.transpose(out=p[:, 0:NE], in_=bsb[0:NE, c * 128:(c + 1) * 128],
                                                           identity=identf[0:NE, 0:NE]), reads=[bsb, identf], writes=[p])
            if c < 8:
                S.op('vector', lambda e, p=p, c=c: e.tensor_copy(out=bguT[:, c, :], in_=p[:, 0:NE]), reads=[p], pwrites=[bguT])
            else:
                S.op('vector', lambda e, p=p, c=c: e.tensor_scalar_add(out=bguT[:, c, :], in0=p[:, 0:NE], scalar1=1.0),
                     reads=[p], pwrites=[bguT])
        S.barrier()
        st_tmp.close()
        wgu = S.sbuf("mo_wgu", [128, 8, 2 * DM], BF16, st)
        wdn = S.sbuf("mo_wdn", [128, 8, DM], BF16, st)
        wguT = [T(None) for _ in range(8)]
        wdnT = [T(None) for _ in range(8)]
        uT = S.sbuf("mo_uT", [128, 8, group * 128], BF16, st)
        acc = S.sbuf("mo_acc", [128, group, DM], F32, st)
        accT = [T(None) for _ in range(group)]
        G = S.sbuf("mo_G", [128, group, NE], F32, st)
        GT = S.sbuf("mo_GT", [NE, 128], F32, st)
        xt2 = [S.sbuf(f"mo_xt{i}", [128, DM], F32, st) for i in range(2)]
        ub = [S.sbuf(f"mo_ub{i}", [128, DM], BF16, st) for i in range(2)]
        lg = S.sbuf("mo_lg", [128, NE], F32, st)
        m8 = S.sbuf("mo_m8", [128, 8], F32, st)
        msk = S.sbuf("mo_msk", [128, NE], F32, st)
        ssum = S.sbuf("mo_ssum", [128, 1], F32, st)
        psG = [S.psum(f"mo_psG{i}", [128, 512], F32, st) for i in range(2)]
        psL = [S.psum(f"mo_psL{i}", [128, 512], F32, st) for i in range(2)]
        psY = psX
        gl = [S.sbuf(f"mo_gl{i}", [128, 512], F32, st) for i in range(2)]
        sg = [S.sbuf(f"mo_sg{i}", [128, 512], F32, st) for i in range(2)]
        l1 = [S.sbuf(f"mo_l1{i}", [128, 512], F32, st) for i in range(2)]
        actT = [S.sbuf(f"mo_act{i}", [128, 8, 512], BF16, st) for i in range(2)]
        xo = [S.sbuf(f"mo_xo{i}", [128, DM], F32, st) for i in range(1)]
        lnsc = LNScratch(S, st, "mo_lnc")
        wguv = w_gu.rearrange("e (kc p) n -> e p kc n", p=128)
        wdnv = w_down.rearrange("e (kc p) n -> e p kc n", p=128)
        it = 0
        for g0 in range(0, ntiles, group):
            gt = min(group, ntiles - g0)
            ntok = gt * 128
            for ti in range(gt):
                tg = g0 + ti
                mset = 1 if tg < nctx_tiles else 0
                xt = xt2[ti % 2]; u = ub[ti % 2]
                S.dma('sync', lambda e, xt=xt, tg=tg: e.dma_start(out=xt[:], in_=xin[tg * 128:(tg + 1) * 128, :]),
                      reads=[xinT], writes=[xt])
                S.op('vector', lambda e, xt=xt, mset=mset: e.tensor_tensor(out=xt[:], in0=xt[:], in1=sc2b[mset][:], op=ALU.mult),
                     reads=[xt, sc2b[mset]], writes=[xt])
                S.op('gpsimd', lambda e, xt=xt, u=u, mset=mset: e.tensor_tensor(out=u[:], in0=xt[:], in1=sh2b[mset][:], op=ALU.add),
                     reads=[xt, sh2b[mset]], writes=[u])
                for kc in range(8):
                    S.op('tensor', lambda e, u=u, kc=kc: e.transpose(out=psXb[:, kc * 128:(kc + 1) * 128],
                                                                     in_=u[:, kc * 128:(kc + 1) * 128], identity=identb[:]),
                         reads=[u, identb], pwrites=[psXb])
                S.op('scalar', lambda e, ti=ti: e.copy(out=uT[:, :, ti * 128:(ti + 1) * 128],
                                                       in_=psXb[:].rearrange("p (k t) -> p k t", k=8)),
                     reads=[psXb], pwrites=[uT])
                pr = psX[ti % 2]
                for kc in range(8):
                    S.op('tensor', lambda e, pr=pr, kc=kc, ti=ti: e.matmul(pr[:, 0:NE], lhsT=uT[:, kc, ti * 128:(ti + 1) * 128],
                                                                           rhs=rw[:, kc, :], start=(kc == 0), stop=(kc == 7)),
                         reads=[uT, rw], writes=[pr])
                S.op('vector', lambda e, pr=pr: e.tensor_tensor(out=lg[:], in0=pr[:, 0:NE], in1=rbb[:], op=ALU.add),
                     reads=[pr, rbb], writes=[lg])
                S.op('vector', lambda e: e.max(out=m8[:], in_=lg[:]), reads=[lg], writes=[m8])
                S.op('vector', lambda e: e.tensor_scalar(out=msk[:], in0=lg[:], scalar1=m8[:, 3:4], scalar2=None, op0=ALU.is_ge),
                     reads=[lg, m8], writes=[msk])
                S.op('vector', lambda e: e.tensor_scalar(out=lg[:], in0=lg[:], scalar1=m8[:, 0:1], scalar2=None, op0=ALU.subtract),
                     reads=[lg, m8], writes=[lg])
                S.op('scalar', lambda e: e.activation(out=lg[:], in_=lg[:], func=AF.Exp), reads=[lg], writes=[lg])
                S.op('vector', lambda e: e.tensor_tensor(out=lg[:], in0=lg[:], in1=msk[:], op=ALU.mult), reads=[lg, msk], writes=[lg])
                S.op('vector', lambda e: e.reduce_sum(out=ssum[:], in_=lg[:], axis=AX.X), reads=[lg], writes=[ssum])
                S.op('vector', lambda e: e.reciprocal(out=ssum[:], in_=ssum[:]), reads=[ssum], writes=[ssum])
                S.op('vector', lambda e, ti=ti: e.tensor_scalar_mul(out=G[:, ti, :], in0=lg[:], scalar1=ssum[:, 0:1]),
                     reads=[lg, ssum], pwrites=[G])
                pg = psX[(ti + 1) % 2]
                S.op('tensor', lambda e, pg=pg, ti=ti: e.transpose(out=pg[0:NE, 0:128], in_=G[:, ti, :], identity=identf[:]),
                     reads=[G, identf], writes=[pg])
                S.op('vector', lambda e, pg=pg: e.tensor_copy(out=GT[:], in_=pg[0:NE, 0:128]), reads=[pg], writes=[GT])
                for nh in range(2):
                    pb = psG[nh]
                    S.op('tensor', lambda e, pb=pb, nh=nh: e.matmul(pb[:], lhsT=GT[:], rhs=bdn[:, nh * 512:(nh + 1) * 512],
                                                                    start=True, stop=True), reads=[GT, bdn], writes=[pb])
                    S.op('scalar', lambda e, pb=pb, nh=nh, ti=ti: e.copy(out=acc[:, ti, nh * 512:(nh + 1) * 512], in_=pb[:]),
                         reads=[pb], pwrites=[accT[ti]])
            tblocks = [(t0, min(512, ntok - t0)) for t0 in range(0, ntok, 512)]
            for ex in range(NE):
                for kc in range(8):
                    S.dma('gpsimd', lambda e, ex=ex, kc=kc: e.dma_start(out=wgu[:, kc, :], in_=wguv[ex, :, kc, :]),
                          writes=[wguT[kc]])
                for kc in range(8):
                    S.dma('gpsimd', lambda e, ex=ex, kc=kc: e.dma_start(out=wdn[:, kc, :], in_=wdnv[ex, :, kc, :]),
                          writes=[wdnT[kc]])
                for (t0, nt) in tblocks:
                    A = actT[it % 2]; it += 1
                    for j in range(8):
                        pg = psG[j % 2]; pl = psL[j % 2]
                        for kc in range(8):
                            S.op('tensor', lambda e, pg=pg, kc=kc, j=j, t0=t0, nt=nt: e.matmul(
                                pg[:, :nt], lhsT=wgu[:, kc, j * 128:(j + 1) * 128], rhs=uT[:, kc, t0:t0 + nt],
                                start=(kc == 0), stop=(kc == 7)), reads=[wguT[kc], uT], writes=[pg])
                        for kc in range(8):
                            S.op('tensor', lambda e, pl=pl, kc=kc, j=j, t0=t0, nt=nt: e.matmul(
                                pl[:, :nt], lhsT=wgu[:, kc, DM + j * 128:DM + (j + 1) * 128], rhs=uT[:, kc, t0:t0 + nt],
                                start=(kc == 0), stop=(kc == 7)), reads=[wguT[kc], uT], writes=[pl])
                        g_ = gl[j % 2]; s_ = sg[j % 2]; l_ = l1[j % 2]
                        S.op('vector', lambda e, pg=pg, g_=g_, j=j, ex=ex, nt=nt: e.tensor_scalar(
                            out=g_[:, :nt], in0=pg[:, :nt], scalar1=bguT[:, j, ex:ex + 1], scalar2=7.0, op0=ALU.add, op1=ALU.min),
                            reads=[pg, bguT], writes=[g_])
                        S.op('scalar', lambda e, g_=g_, s_=s_, nt=nt: e.activation(out=s_[:, :nt], in_=g_[:, :nt], func=AF.Sigmoid,
                                                                                   scale=1.702), reads=[g_], writes=[s_])
                        S.op('scalar', lambda e, pl=pl, l_=l_, j=j, ex=ex, nt=nt: e.activation(
                            out=l_[:, :nt], in_=pl[:, :nt], func=AF.Identity, bias=bguT[:, 8 + j, ex:ex + 1]),
                            reads=[pl, bguT], writes=[l_])
                        S.op('gpsimd', lambda e, l_=l_, nt=nt: e.tensor_scalar(out=l_[:, :nt], in0=l_[:, :nt], scalar1=-6.0, scalar2=8.0,
                                                                                op0=ALU.max, op1=ALU.min), reads=[l_], writes=[l_])
                        S.op('gpsimd', lambda e, g_=g_, s_=s_, nt=nt: e.tensor_tensor(out=g_[:, :nt], in0=g_[:, :nt], in1=s_[:, :nt],
                                                                                      op=ALU.mult), reads=[g_, s_], writes=[g_])
                        S.op('vector', lambda e, A=A, g_=g_, l_=l_, j=j, nt=nt: e.tensor_tensor(out=A[:, j, :nt], in0=g_[:, :nt],
                                                                                                  in1=l_[:, :nt], op=ALU.mult),
                             reads=[g_, l_], pwrites=[A])
                    for tt in range(nt // 128):
                        ti = (t0 // 128) + tt
                        for nh in range(2):
                            py = psY[nh]
                            for j in range(8):
                                S.op('tensor', lambda e, py=py, A=A, j=j, tt=tt, nh=nh: e.matmul(
                                    py[:], lhsT=A[:, j, tt * 128:(tt + 1) * 128], rhs=wdn[:, j, nh * 512:(nh + 1) * 512],
                                    start=(j == 0), stop=(j == 7)), reads=[A, wdnT[j]], writes=[py])
                            S.op('vector', lambda e, py=py, ti=ti, nh=nh, ex=ex: e.scalar_tensor_tensor(
                                out=acc[:, ti, nh * 512:(nh + 1) * 512], in0=py[:], scalar=G[:, ti, ex:ex + 1],
                                in1=acc[:, ti, nh * 512:(nh + 1) * 512], op0=ALU.mult, op1=ALU.add),
                                reads=[py, G, accT[ti]], pwrites=[accT[ti]])
            for ti in range(gt):
                tg = g0 + ti
                mset = 1 if tg < nctx_tiles else 0
                xt = xt2[ti % 2]; o = xo[0]
                S.dma('sync', lambda e, xt=xt, tg=tg: e.dma_start(out=xt[:], in_=xin[tg * 128:(tg + 1) * 128, :]),
                      reads=[xinT], writes=[xt])
                S.op('gpsimd', lambda e, ti=ti, mset=mset: e.tensor_tensor(out=acc[:, ti, :], in0=acc[:, ti, :], in1=g2b[mset][:], op=ALU.mult),
                     reads=[accT[ti], g2b[mset]], writes=[accT[ti]])
                S.op('vector', lambda e, xt=xt, ti=ti: e.scalar_tensor_tensor(out=xt[:], in0=xt[:], scalar=ALPHA, in1=acc[:, ti, :],
                                                                             op0=ALU.mult, op1=ALU.add), reads=[xt, accT[ti]], writes=[xt])
                emit_ln(S, lnsc, xt, o, lngb, lnbb)
                S.dma('sync', lambda e, o=o, tg=tg: e.dma_start(out=xout[tg * 128:(tg + 1) * 128, :], in_=o[:]),
                      reads=[o], pwrites=[xoutT])
        S.barrier()


def build_moe(ntiles, nctx_tiles):
    nc = bass.Bass("TRN2", target_bir_lowering=False)
    EI = "ExternalInput"
    NTk = ntiles * 128
    xin = _dram(nc, "xin", [NTk, DM], F32, EI)
    mods_d = _dram(nc, "mods", [2, 6 * DM], F32, EI)
    router_w = _dram(nc, "router_w", [DM, 32], F32, EI)
    router_b = _dram(nc, "router_b", [1, 32], F32, EI)
    w_gu = _dram(nc, "w_gu", [32, DM, 2 * DM], F32, EI)
    b_gu = _dram(nc, "b_gu", [32, 2 * DM], F32, EI)
    w_down = _dram(nc, "w_down", [32, DM, DM], F32, EI)
    b_down = _dram(nc, "b_down", [32, DM], F32, EI)
    ln_g = _dram(nc, "ln_g", [1, DM], F32, EI)
    ln_b = _dram(nc, "ln_b", [1, DM], F32, EI)
    ident_d = _dram(nc, "ident", [128, 128], F32, EI)
    xout = _dram(nc, "xout", [NTk, DM], F32, "ExternalOutput")
    with ExitStack() as st0:
        S = Sched(nc, st0)
        emit_moe(S, nc, xin, T(xin), mods_d, T(mods_d), router_w, router_b, w_gu, b_gu, w_down, b_down, ln_g, ln_b,
                 ident_d, xout, T(xout), ntiles, nctx_tiles)
    return nc


def shared_moe(inp, i):
    return dict(router_w=inp['router_w'][i], router_b=inp['router_b'][i][None, :], w_gu=inp['moe_w_gu'][i],
                b_gu=inp['moe_b_gu'][i], w_down=inp['moe_w_down'][i], b_down=inp['moe_b_down'][i],
                ln_g=inp['ln_g'][i, 1][None, :], ln_b=inp['ln_b'][i, 1][None, :], ident=np.eye(128, dtype=np.float32))


NU = NQ // 128
GIN = 3104


class GlaScratch:
    def __init__(self, nc, kind="Internal", sfx=""):
        self.qT = _dram(nc, "g_qT" + sfx, [NU, 128, 4, 128], F32, kind)
        self.kT = _dram(nc, "g_kT" + sfx, [NU, 128, 4, 128], F32, kind)
        self.k = _dram(nc, "g_k" + sfx, [NQ, 512], F32, kind)
        self.v = _dram(nc, "g_v" + sfx, [NQ, DM], BF16, kind)
        self.LgA = _dram(nc, "g_LgA" + sfx, [NQ, 512], F32, kind)
        self.LgB = _dram(nc, "g_LgB" + sfx, [NQ, 512], F32, kind)
        self.r = _dram(nc, "g_r" + sfx, [NQ, DM], F32, kind)
        self.T = {n: T(getattr(self, n), n) for n in ('qT', 'kT', 'k', 'v', 'LgA', 'LgB', 'r')}


def emit_gla_proj(S, nc, xin, xinT, mods_d, modsT, w_in, wgA, wgB, ident_d, G):
    with ExitStack() as st:
        identb = S.sbuf("gp_identb", [128, 128], BF16, st)
        S.dma('gpsimd', lambda e: e.dma_start(out=identb[:], in_=ident_d[:, :]), writes=[identb])
        sc1b = [load_bcast(S, st, f"gp_sc1b{m}", mods_d[m:m + 1, DM:2 * DM], modsT) for m in range(2)]
        sh1b = [load_bcast(S, st, f"gp_sh1b{m}", mods_d[m:m + 1, 0:DM], modsT) for m in range(2)]
        for t in sc1b:
            S.op('gpsimd', lambda e, t=t: e.tensor_scalar_add(out=t[:], in0=t[:], scalar1=1.0), reads=[t], writes=[t])
        wi = S.sbuf("gp_wi", [128, 8, GIN], BF16, st)
        wiv = w_in.rearrange("(kc p) n -> p kc n", p=128)
        for kc in range(8):
            for c0, c1 in ((0, 1024), (1024, 2048), (2048, GIN)):
                S.dma('gpsimd', lambda e, kc=kc, c0=c0, c1=c1: e.dma_start(out=wi[:, kc, c0:c1], in_=wiv[:, kc, c0:c1]), pwrites=[wi])
        wg = []
        for nm, src in (("A", wgA), ("B", wgB)):
            t = S.sbuf("gp_wg" + nm, [17, 512], F32, st)
            S.dma('sync', lambda e, t=t, src=src: e.dma_start(out=t[:], in_=src[:, :]), writes=[t])
            wg.append(t)
        zaug = [S.sbuf(f"gp_zaug{i}", [32, 512], F32, st) for i in range(2)]
        for z in zaug:
            S.op('vector', lambda e, z=z: e.memset(z[:], 1.0), writes=[z])
        xts = [S.sbuf(f"gp_xt{i}", [128, DM], F32, st) for i in range(2)]
        tbf = [S.sbuf(f"gp_tbf{i}", [128, DM], BF16, st) for i in range(2)]
        tT = [S.sbuf(f"gp_tT{i}", [128, 8, 512], BF16, st) for i in range(2)]
        psXb = S.psum("gp_psXb", [128, 1024], BF16, st)
        psF = [S.psum(f"gp_psF{i}", [128, 512], F32, st) for i in range(2)]
        psZ = S.psum("gp_psZ", [128, 512], F32, st)
        psK = [S.psum(f"gp_psK{i}", [128, 512], F32, st) for i in range(3)]
        fst = [S.sbuf(f"gp_fst{i}", [128, 512], F32, st) for i in range(3)]
        kst = [S.sbuf(f"gp_kst{i}", [128, 512], F32, st) for i in range(2)]
        vst = [S.sbuf(f"gp_vst{i}", [128, DM], BF16, st) for i in range(2)]
        rst = [S.sbuf(f"gp_rst{i}", [128, DM], F32, st) for i in range(2)]
        gex = [S.sbuf(f"gp_gex{i}", [128, 512], F32, st) for i in range(2)]
        gst = [S.sbuf(f"gp_gst{i}", [128, 512], F32, st) for i in range(2)]
        blocks = [(0, NCTX)] + [(NCTX + i * 512, 512) for i in range(8)]
        iF = 0; iK = 0; ig = 0
        for bi, (t0, nt) in enumerate(blocks):
            TT = tT[bi % 2]
            mset = 1 if bi == 0 else 0
            ntl = nt // 128
            for ti in range(ntl):
                r0 = t0 + ti * 128
                xt = xts[ti % 2]; tb = tbf[ti % 2]
                S.dma('sync', lambda e, xt=xt, r0=r0: e.dma_start(out=xt[:], in_=xin[r0:r0 + 128, :]), reads=[xinT], writes=[xt])
                S.op('vector', lambda e, xt=xt, mset=mset: e.tensor_tensor(out=xt[:], in0=xt[:], in1=sc1b[mset][:], op=ALU.mult),
                     reads=[xt, sc1b[mset]], writes=[xt])
                S.op('gpsimd', lambda e, xt=xt, tb=tb, mset=mset: e.tensor_tensor(out=tb[:], in0=xt[:], in1=sh1b[mset][:], op=ALU.add),
                     reads=[xt, sh1b[mset]], writes=[tb])
                for kc in range(8):
                    S.op('tensor', lambda e, tb=tb, kc=kc: e.transpose(out=psXb[:, kc * 128:(kc + 1) * 128],
                                                                       in_=tb[:, kc * 128:(kc + 1) * 128], identity=identb[:]),
                         reads=[tb, identb], pwrites=[psXb])
                S.op('scalar', lambda e, TT=TT, ti=ti: e.copy(out=TT[:, :, ti * 128:(ti + 1) * 128],
                                                              in_=psXb[:].rearrange("p (k t) -> p k t", k=8)),
                     reads=[psXb], pwrites=[TT])
            n0 = t0 // 128
            for kind in ('q', 'k'):
                for h in range(4):
                    c0 = (0 if kind == 'q' else 512) + h * 128
                    pf = psF[iF % 2]; fs = fst[iF % 3]; iF += 1
                    for kc in range(8):
                        S.op('tensor', lambda e, pf=pf, TT=TT, kc=kc, c0=c0, nt=nt: e.matmul(
                            pf[:, :nt], lhsT=wi[:, kc, c0:c0 + 128], rhs=TT[:, kc, :nt], start=(kc == 0), stop=(kc == 7)),
                            reads=[wi, TT], writes=[pf])
                    S.op('scalar', lambda e, pf=pf, fs=fs, nt=nt, kind=kind: e.activation(
                        out=fs[:, :nt], in_=pf[:, :nt], func=AF.Copy, scale=(128.0 ** -0.5 if kind == 'q' else 1.0)),
                        reads=[pf], writes=[fs])
                    dst = G.qT if kind == 'q' else G.kT
                    S.dma('sync', lambda e, fs=fs, dst=dst, n0=n0, ntl=ntl, h=h, nt=nt: e.dma_start(
                        out=dst[n0:n0 + ntl, :, h, :].rearrange("n p t -> p n t"),
                        in_=fs[:, :nt].rearrange("p (n t) -> p n t", t=128)),
                        reads=[fs], pwrites=[G.T['qT' if kind == 'q' else 'kT']])
            for d in range(2):
                for kc in range(8):
                    S.op('tensor', lambda e, kc=kc, d=d, nt=nt: e.matmul(
                        psZ[0:16, :nt], lhsT=wi[:, kc, 3072 + d * 16:3072 + (d + 1) * 16], rhs=TT[:, kc, :nt],
                        start=(kc == 0), stop=(kc == 7)), reads=[wi, TT], writes=[psZ])
                S.op('vector', lambda e, d=d, nt=nt: e.tensor_copy(out=zaug[d][0:16, :nt], in_=psZ[0:16, :nt]),
                     reads=[psZ], pwrites=[zaug[d]])
            for ti in range(ntl):
                r0 = t0 + ti * 128
                tsl = slice(ti * 128, (ti + 1) * 128)
                pk = psK[iK % 3]; iK += 1
                ks = kst[ti % 2]
                for kc in range(8):
                    S.op('tensor', lambda e, pk=pk, kc=kc: e.matmul(pk[:], lhsT=TT[:, kc, tsl], rhs=wi[:, kc, 512:1024],
                                                                    start=(kc == 0), stop=(kc == 7)), reads=[wi, TT], writes=[pk])
                S.op('scalar', lambda e, pk=pk, ks=ks: e.copy(out=ks[:], in_=pk[:]), reads=[pk], writes=[ks])
                S.dma('sync', lambda e, ks=ks, r0=r0: e.dma_start(out=G.k[r0:r0 + 128, :], in_=ks[:]), reads=[ks], pwrites=[G.T['k']])
                vs_ = vst[ti % 2]; rs_ = rst[ti % 2]
                for which, c00 in (('v', 1024), ('r', 2048)):
                    if which == 'r' and bi == 0:
                        continue
                    for nh in range(2):
                        pk = psK[iK % 3]; iK += 1
                        for kc in range(8):
                            S.op('tensor', lambda e, pk=pk, kc=kc, c00=c00, nh=nh: e.matmul(
                                pk[:], lhsT=TT[:, kc, tsl], rhs=wi[:, kc, c00 + nh * 512:c00 + (nh + 1) * 512],
                                start=(kc == 0), stop=(kc == 7)), reads=[wi, TT], writes=[pk])
                        dstt = vs_ if which == 'v' else rs_
                        eng = 'vector' if which == 'v' else 'scalar'
                        if eng == 'vector':
                            S.op('vector', lambda e, pk=pk, dstt=dstt, nh=nh: e.tensor_copy(out=dstt[:, nh * 512:(nh + 1) * 512], in_=pk[:]),
                                 reads=[pk], pwrites=[dstt])
                        else:
                            S.op('scalar', lambda e, pk=pk, dstt=dstt, nh=nh: e.copy(out=dstt[:, nh * 512:(nh + 1) * 512], in_=pk[:]),
                                 reads=[pk], pwrites=[dstt])
                S.dma('sync', lambda e, vs_=vs_, r0=r0: e.dma_start(out=G.v[r0:r0 + 128, :], in_=vs_[:]), reads=[vs_], pwrites=[G.T['v']])
                if bi != 0:
                    S.dma('sync', lambda e, rs_=rs_, r0=r0: e.dma_start(out=G.r[r0:r0 + 128, :], in_=rs_[:]), reads=[rs_], pwrites=[G.T['r']])
                for d in range(2):
                    pk = psK[iK % 3]; iK += 1
                    ge = gex[ig % 2]; gs = gst[ig % 2]; ig += 1
                    S.op('tensor', lambda e, pk=pk, d=d: e.matmul(pk[:], lhsT=zaug[d][0:17, tsl], rhs=wg[d][0:17, :],
                                                                  start=True, stop=True), reads=[zaug[d], wg[d]], writes=[pk])
                    S.op('scalar', lambda e, pk=pk, ge=ge: e.activation(out=ge[:], in_=pk[:], func=AF.Exp, scale=-1.0),
                         reads=[pk], writes=[ge])
                    S.op('scalar', lambda e, ge=ge, gs=gs: e.activation(out=gs[:], in_=ge[:], func=AF.Ln, bias=1.0),
                         reads=[ge], writes=[gs])
                    dst = G.LgA if d == 0 else G.LgB
                    S.dma('sync', lambda e, gs=gs, dst=dst, r0=r0: e.dma_start(out=dst[r0:r0 + 128, :], in_=gs[:]),
                          reads=[gs], pwrites=[G.T['LgA' if d == 0 else 'LgB']])
        S.barrier()


def emit_gla_scan(S, nc, G, direction, cmats, S_init, S_final, oA, oAT, post=None):
    A = direction == 'A'
    Lg = G.LgA if A else G.LgB
    LgT = G.T['LgA' if A else 'LgB']
    col = 127 if A else 0
    with ExitStack() as st:
        cm = {}
        for nm in ('MinclT', 'MafterT', 'maskT'):
            t = S.sbuf("gs_" + nm, [128, 128], F32, st)
            S.dma('sync', lambda e, t=t, nm=nm: e.dma_start(out=t[:], in_=cmats[nm][:, :]), writes=[t])
            cm[nm] = t
        Sf = S.sbuf("gs_Sf", [128, 4, 256], F32, st)
        Sb = S.sbuf("gs_Sb", [128, 4, 256], BF16, st)
        SfT = [T(None) for _ in range(4)]; SbT = [T(None) for _ in range(4)]
        if S_init is None:
            S.op('vector', lambda e: e.memset(Sf[:], 0.0), writes=SfT)
        else:
            S.dma('sync', lambda e: e.dma_start(out=Sf[:], in_=S_init[:, :, :]), writes=SfT)
        for h in range(4):
            S.op('gpsimd', lambda e, h=h: e.tensor_copy(out=Sb[:, h, :], in_=Sf[:, h, :]), reads=[SfT[h]], writes=[SbT[h]])
        NB = 3
        qTu = [S.sbuf(f"gs_qT{i}", [128, 4, 128], F32, st) for i in range(NB)]
        kTu = [S.sbuf(f"gs_kT{i}", [128, 4, 128], F32, st) for i in range(NB)]
        ku = [S.sbuf(f"gs_k{i}", [128, 512], F32, st) for i in range(NB)]
        vu = [S.sbuf(f"gs_v{i}", [128, DM], BF16, st) for i in range(NB)]
        Lgu = [S.sbuf(f"gs_Lg{i}", [128, 512], F32, st) for i in range(NB)]
        bank = [S.psum(f"gs_bank{i}", [128, 512], F32, st) for i in range(5)]
        psB = [T(bank[0].t[:, h * 128:(h + 1) * 128]) for h in range(4)]
        psW = [T(bank[1].t[:, h * 128:(h + 1) * 128]) for h in range(4)]
        psA = [T(bank[2].t[:, h * 128:(h + 1) * 128]) for h in range(4)]
        psO = [T(bank[3].t[:, i * 256:(i + 1) * 256]) for i in range(2)]
        psD = [T(bank[4].t[:, i * 256:(i + 1) * 256]) for i in range(2)]
        Eq = [S.sbuf(f"gs_Eq{h}", [128, 128], F32, st) for h in range(4)]
        Ek = [S.sbuf(f"gs_Ek{h}", [128, 128], F32, st) for h in range(4)]
        Ew = [S.sbuf(f"gs_Ew{h}", [128, 128], F32, st) for h in range(4)]
        qin = [S.sbuf(f"gs_qin{h}", [128, 128], BF16, st) for h in range(4)]
        kin = [S.sbuf(f"gs_kin{h}", [128, 128], BF16, st) for h in range(4)]
        kst = [S.sbuf(f"gs_kst{h}", [128, 128], BF16, st) for h in range(4)]
        atm = [S.sbuf(f"gs_atm{h}", [128, 128], BF16, st) for h in range(4)]
        ou = [S.sbuf(f"gs_ou{i}", [128, DM], F32, st) for i in range(2)]
        if post is not None:
            P = post
            identb = S.sbuf("go_identb", [128, 128], BF16, st)
            S.dma('gpsimd', lambda e: e.dma_start(out=identb[:], in_=P['ident'][:, :]), writes=[identb])
            wo = S.sbuf("go_wo", [128, 8, DM], BF16, st)
            S.dma('gpsimd', lambda e: e.dma_start(out=wo[:], in_=P['w_out'].rearrange("(kc p) n -> p kc n", p=128)), writes=[wo])
            g1b = load_bcast(S, st, "go_g1b", P['mods_d'][0:1, 2 * DM:3 * DM], P['modsT'])
            lngb = load_bcast(S, st, "go_lngb", P['ln_g'])
            lnbb = load_bcast(S, st, "go_lnbb", P['ln_b'])
            nwb = S.sbuf("go_nwb", [128, 256], F32, st)
            S.dma('sync', lambda e: e.dma_start(out=nwb[:], in_=P['norm_w'].partition_broadcast(128)), writes=[nwb])
            oAu = [S.sbuf(f"go_oA{i}", [128, DM], F32, st) for i in range(NB)]
            ru = [S.sbuf(f"go_r{i}", [128, DM], F32, st) for i in range(NB)]
            xu = [S.sbuf(f"go_x{i}", [128, DM], F32, st) for i in range(NB)]
            sqt = S.sbuf("go_sq", [128, DM], F32, st)
            ms = S.sbuf("go_ms", [128, 4], F32, st)
            onb = S.sbuf("go_onb", [128, DM], BF16, st)
            onT = S.sbuf("go_onT", [128, 8, 128], BF16, st)
            psXb = S.psum("go_psXb", [128, 1024], BF16, st)
            psY = [S.psum(f"go_psY{i}", [128, 512], F32, st) for i in range(2)]
            xo = S.sbuf("go_xo", [128, DM], F32, st)
            lnsc = LNScratch(S, st, "go_lnc")
        units = list(range(NU)) if A else list(range(NU - 1, 1, -1))

        def load(i, n):
            r0 = n * 128
            S.dma('sync', lambda e: e.dma_start(out=kTu[i][:], in_=G.kT[n]), reads=[G.T['kT']], writes=[kTu[i]])
            S.dma('sync', lambda e: e.dma_start(out=ku[i][:], in_=G.k[r0:r0 + 128, :]), reads=[G.T['k']], writes=[ku[i]])
            S.dma('sync', lambda e: e.dma_start(out=vu[i][:], in_=G.v[r0:r0 + 128, :]), reads=[G.T['v']], writes=[vu[i]])
            S.dma('sync', lambda e: e.dma_start(out=Lgu[i][:], in_=Lg[r0:r0 + 128, :]), reads=[LgT], writes=[Lgu[i]])
            if n >= 2:
                S.dma('sync', lambda e: e.dma_start(out=qTu[i][:], in_=G.qT[n]), reads=[G.T['qT']], writes=[qTu[i]])
                if post is not None:
                    j = i
                    S.dma('sync', lambda e: e.dma_start(out=oAu[j][:], in_=oA[r0 - NCTX:r0 - NCTX + 128, :]), reads=[oAT], writes=[oAu[j]])
                    S.dma('sync', lambda e: e.dma_start(out=ru[j][:], in_=G.r[r0:r0 + 128, :]), reads=[G.T['r']], writes=[ru[j]])
                    S.dma('sync', lambda e: e.dma_start(out=xu[j][:], in_=P['xin'][r0:r0 + 128, :]), reads=[P['xinT']], writes=[xu[j]])

        load(0, units[0])
        for ui, n in enumerate(units):
            i = ui % NB
            if ui + 1 < len(units):
                load((ui + 1) % NB, units[ui + 1])
            full = n >= 2
            O = ou[ui % 2]
            for h in range(4):
                hs = slice(h * 128, (h + 1) * 128)
                S.op('tensor', lambda e: e.matmul(psB[h][:], lhsT=Lgu[i][:, hs], rhs=cm['MinclT'][:], start=True, stop=True),
                     reads=[Lgu[i], cm['MinclT']], writes=[psB[h]])
                S.op('tensor', lambda e: e.matmul(psW[h][:], lhsT=cm['MafterT'][:], rhs=Lgu[i][:, hs], start=True, stop=True),
                     reads=[Lgu[i], cm['MafterT']], writes=[psW[h]])
                S.op('scalar', lambda e: e.activation(out=Eq[h][:], in_=psB[h][:], func=AF.Exp), reads=[psB[h]], writes=[Eq[h]])
                S.op('scalar', lambda e: e.activation(out=Ew[h][:], in_=psW[h][:], func=AF.Exp), reads=[psW[h]], writes=[Ew[h]])
                S.op('gpsimd', lambda e: e.tensor_tensor(out=kst[h][:], in0=ku[i][:, hs], in1=Ew[h][:], op=ALU.mult),
                     reads=[ku[i], Ew[h]], writes=[kst[h]])
                if full:
                    S.op('scalar', lambda e: e.activation(out=Ek[h][:], in_=psB[h][:], func=AF.Exp, scale=-1.0),
                         reads=[psB[h]], writes=[Ek[h]])
                    S.op('vector', lambda e: e.tensor_tensor(out=qin[h][:], in0=qTu[i][:, h, :], in1=Eq[h][:], op=ALU.mult),
                         reads=[qTu[i], Eq[h]], writes=[qin[h]])
                    S.op('gpsimd', lambda e: e.tensor_tensor(out=kin[h][:], in0=kTu[i][:, h, :], in1=Ek[h][:], op=ALU.mult),
                         reads=[kTu[i], Ek[h]], writes=[kin[h]])
                    S.op('tensor', lambda e: e.matmul(psA[h][:], lhsT=kin[h][:], rhs=qin[h][:], start=True, stop=True),
                         reads=[kin[h], qin[h]], writes=[psA[h]])
                    S.op('vector', lambda e: e.tensor_tensor(out=atm[h][:], in0=psA[h][:], in1=cm['maskT'][:], op=ALU.mult),
                         reads=[psA[h], cm['maskT']], writes=[atm[h]])
                    po = psO[h % 2]
                    S.op('tensor', lambda e: e.matmul(po[:], lhsT=atm[h][:], rhs=vu[i][:, h * 256:(h + 1) * 256], start=True, stop=False),
                         reads=[atm[h], vu[i]], writes=[po])
                    S.op('tensor', lambda e: e.matmul(po[:], lhsT=qin[h][:], rhs=Sb[:, h, :], start=False, stop=True),
                         reads=[qin[h], SbT[h]], writes=[po])
                    if post is None:
                        S.op('scalar', lambda e: e.copy(out=O[:, h * 256:(h + 1) * 256], in_=po[:]), reads=[po], pwrites=[O])
                    else:
                        S.op('vector', lambda e: e.tensor_tensor(out=O[:, h * 256:(h + 1) * 256], in0=po[:],
                                                                 in1=oAu[i][:, h * 256:(h + 1) * 256], op=ALU.add),
                             reads=[po, oAu[i]], pwrites=[O])
                pd = psD[h % 2]
                S.op('tensor', lambda e: e.matmul(pd[:], lhsT=kst[h][:], rhs=vu[i][:, h * 256:(h + 1) * 256], start=True, stop=True),
                     reads=[kst[h], vu[i]], writes=[pd])
                S.op('vector', lambda e: e.scalar_tensor_tensor(out=Sf[:, h, :], in0=Sf[:, h, :], scalar=Eq[h][:, col:col + 1],
                                                                in1=pd[:], op0=ALU.mult, op1=ALU.add),
                     reads=[SfT[h], Eq[h], pd], writes=[SfT[h]])
                S.op('gpsimd', lambda e: e.tensor_copy(out=Sb[:, h, :], in_=Sf[:, h, :]), reads=[SfT[h]], writes=[SbT[h]])
            if not full:
                continue
            r0 = n * 128
            if post is None:
                S.dma('sync', lambda e: e.dma_start(out=oA[r0 - NCTX:r0 - NCTX + 128, :], in_=O[:]), reads=[O], pwrites=[oAT])
                continue
            j = i
            S.op('gpsimd', lambda e: e.tensor_tensor(out=sqt[:], in0=O[:], in1=O[:], op=ALU.mult), reads=[O], writes=[sqt])
            S.op('vector', lambda e: e.reduce_sum(out=ms[:], in_=sqt[:].rearrange("p (h d) -> p h d", h=4), axis=AX.X),
                 reads=[sqt], writes=[ms])
            S.op('vector', lambda e: e.tensor_scalar(out=ms[:], in0=ms[:], scalar1=1.0 / 256, scalar2=EPS, op0=ALU.mult, op1=ALU.add),
                 reads=[ms], writes=[ms])
            S.op('scalar', lambda e: e.activation(out=ms[:], in_=ms[:], func=AF.Sqrt), reads=[ms], writes=[ms])
            S.op('vector', lambda e: e.reciprocal(out=ms[:], in_=ms[:]), reads=[ms], writes=[ms])
            for h in range(4):
                S.op('vector', lambda e: e.scalar_tensor_tensor(out=O[:, h * 256:(h + 1) * 256], in0=O[:, h * 256:(h + 1) * 256],
                                                                scalar=ms[:, h:h + 1], in1=nwb[:], op0=ALU.mult, op1=ALU.mult),
                     reads=[O, ms, nwb], writes=[O])
            S.op('scalar', lambda e: e.activation(out=sqt[:], in_=ru[j][:], func=AF.Silu), reads=[ru[j]], writes=[sqt])
            S.op('gpsimd', lambda e: e.tensor_tensor(out=onb[:], in0=O[:], in1=sqt[:], op=ALU.mult), reads=[O, sqt], writes=[onb])
            for kc in range(8):
                S.op('tensor', lambda e: e.transpose(out=psXb[:, kc * 128:(kc + 1) * 128], in_=onb[:, kc * 128:(kc + 1) * 128],
                                                     identity=identb[:]), reads=[onb, identb], pwrites=[psXb])
            S.op('scalar', lambda e: e.copy(out=onT[:], in_=psXb[:].rearrange("p (k t) -> p k t", k=8)), reads=[psXb], writes=[onT])
            for nh in range(2):
                py = psY[nh]
                for kc in range(8):
                    S.op('tensor', lambda e: e.matmul(py[:], lhsT=onT[:, kc, :], rhs=wo[:, kc, nh * 512:(nh + 1) * 512],
                                                      start=(kc == 0), stop=(kc == 7)), reads=[onT, wo], writes=[py])
                S.op('vector', lambda e: e.tensor_tensor(out=sqt[:, nh * 512:(nh + 1) * 512], in0=py[:],
                                                         in1=g1b[:, nh * 512:(nh + 1) * 512], op=ALU.mult),
                     reads=[py, g1b], pwrites=[sqt])
            S.op('vector', lambda e: e.scalar_tensor_tensor(out=sqt[:], in0=xu[j][:], scalar=ALPHA, in1=sqt[:],
                                                            op0=ALU.mult, op1=ALU.add), reads=[xu[j], sqt], writes=[sqt])
            emit_ln(S, lnsc, sqt, xo, lngb, lnbb)
            S.dma('sync', lambda e: e.dma_start(out=P['xout'][r0 - NCTX:r0 - NCTX + 128, :], in_=xo[:]),
                  reads=[xo], pwrites=[P['xoutT']])
        if S_final is not None:
            S.dma('sync', lambda e: e.dma_start(out=S_final[:, :, :], in_=Sf[:]), reads=SfT)
        S.barrier()


def _gla_common_inputs(nc):
    EI = "ExternalInput"
    d = dict(
        xin=_dram(nc, "xin", [NQ, DM], F32, EI),
        w_in=_dram(nc, "w_in", [DM, GIN], F32, EI),
        wgA=_dram(nc, "wgA", [17, 512], F32, EI),
        wgB=_dram(nc, "wgB", [17, 512], F32, EI),
        ident=_dram(nc, "ident", [128, 128], F32, EI),
    )
    return d


def _cmats(nc, sfx):
    return {nm: _dram(nc, nm + sfx, [128, 128], F32, "ExternalInput") for nm in ('MinclT', 'MafterT', 'maskT')}


def build_gla1():
    nc = bass.Bass("TRN2", target_bir_lowering=False)
    EI = "ExternalInput"
    I = _gla_common_inputs(nc)
    cT = _dram(nc, "cT", [DM, 2], F32, EI)
    ada_w = _dram(nc, "ada_w", [DM, 6 * DM], F32, EI)
    ada_b = _dram(nc, "ada_b", [1, 6 * DM], F32, EI)
    cmA = _cmats(nc, "A")
    mods_d = _dram(nc, "mods", [2, 6 * DM], F32, "ExternalOutput")
    oA = _dram(nc, "oA", [NOWN, DM], F32, "ExternalOutput")
    SA = _dram(nc, "SA", [128, 4, 256], F32, "ExternalOutput")
    G = GlaScratch(nc)
    with ExitStack() as st0:
        S = Sched(nc, st0)
        modsT = T(mods_d)
        with ExitStack() as st:
            emit_mods(S, nc, st, cT, ada_w, ada_b, mods_d, modsT)
            S.barrier()
        xinT = T(I['xin'])
        emit_gla_proj(S, nc, I['xin'], xinT, mods_d, modsT, I['w_in'], I['wgA'], I['wgB'], I['ident'], G)
        emit_gla_scan(S, nc, G, 'A', cmA, None, SA, oA, T(oA))
    return nc


def build_gla2():
    nc = bass.Bass("TRN2", target_bir_lowering=False)
    EI = "ExternalInput"
    I = _gla_common_inputs(nc)
    mods_d = _dram(nc, "mods", [2, 6 * DM], F32, EI)
    cmB = _cmats(nc, "B")
    oA = _dram(nc, "oA", [NOWN, DM], F32, EI)
    SB0 = _dram(nc, "SB0", [128, 4, 256], F32, EI)
    w_out = _dram(nc, "w_out", [DM, DM], F32, EI)
    norm_w = _dram(nc, "norm_w", [1, 256], F32, EI)
    ln_g = _dram(nc, "ln_g", [1, DM], F32, EI)
    ln_b = _dram(nc, "ln_b", [1, DM], F32, EI)
    xout = _dram(nc, "xout", [NOWN, DM], F32, "ExternalOutput")
    G = GlaScratch(nc)
    with ExitStack() as st0:
        S = Sched(nc, st0)
        modsT = T(mods_d); xinT = T(I['xin'])
        emit_gla_proj(S, nc, I['xin'], xinT, mods_d, modsT, I['w_in'], I['wgA'], I['wgB'], I['ident'], G)
        post = dict(ident=I['ident'], w_out=w_out, mods_d=mods_d, modsT=modsT, ln_g=ln_g, ln_b=ln_b, norm_w=norm_w,
                    xin=I['xin'], xinT=xinT, xout=xout, xoutT=T(xout))
        emit_gla_scan(S, nc, G, 'B', cmB, SB0, None, oA, T(oA), post=post)
    return nc


def _gla_cmats():
    s = np.arange(128)[:, None]; t = np.arange(128)[None, :]
    c = np.float32(-1.0 / 16.0)
    return dict(
        MinclTA=(s <= t) * c, MafterTA=(s > t) * c, maskTA=(s <= t) * np.float32(1),
        MinclTB=(s >= t) * c, MafterTB=(s < t) * c, maskTB=(s >= t) * np.float32(1),
    )


def shared_gla(inp):
    cm = {k: np.ascontiguousarray(v.astype(np.float32)) for k, v in _gla_cmats().items()}
    w = inp['gla_w_in'][0]
    wsw = np.ascontiguousarray(np.concatenate([w[:, :3072], w[:, 3088:3104], w[:, 3072:3088]], 1))
    wg = inp['gla_w_gate'][0]; bg = inp['gla_b_gate'][0]
    aug = [np.ascontiguousarray(np.concatenate([wg[d], bg[d][None, :]], 0)) for d in range(2)]
    return dict(cm=cm, w_in=[w, wsw], aug=aug, ada_w=inp['ada_w'][1], ada_b=inp['ada_b'][1][None, :],
                w_out=inp['gla_w_out'][0], norm_w=inp['gla_norm_w'][0][None, :],
                ln_g=inp['ln_g'][1, 0][None, :], ln_b=inp['ln_b'][1, 0][None, :], ident=np.eye(128, dtype=np.float32))


def prep_gla_common(sh, hf, xin):
    return dict(xin=xin, w_in=sh['w_in'][hf], wgA=sh['aug'][hf], wgB=sh['aug'][1 - hf], ident=sh['ident'])


_NC_CACHE = {}


def _get(name, fn):
    if name not in _NC_CACHE:
        _NC_CACHE[name] = fn()
    return _NC_CACHE[name]


def kernel(**inp):
    inp = {k: np.asarray(v) for k, v in inp.items()}
    cores = list(range(8))
    run = lambda nc, maps: run_bass_kernel_spmd(nc, maps, core_ids=cores).results
    sh = shared_attn(inp)
    r = run(_get('attn', lambda: build_attn(_lambda_init(0))), [prep_attn(inp, c, sh) for c in cores])
    mods0 = [r[c]['mods'] for c in cores]; x1 = [r[c]['x1'] for c in cores]
    shm = shared_moe(inp, 0)
    maps = []
    for c in cores:
        d = dict(shm); d['mods'] = mods0[c]; d['xin'] = x1[c]; maps.append(d)
    r = run(_get('moe0', lambda: build_moe(NQ // 128, NCTX // 128)), maps)
    x2 = [r[c]['xout'] for c in cores]
    shg = shared_gla(inp)
    maps = []
    for c in cores:
        b, hf = divmod(c, 2)
        d = prep_gla_common(shg, hf, x2[c])
        d.update(cT=np.ascontiguousarray(np.stack([inp['c'][b], inp['c_ctx']], 1)), ada_w=shg['ada_w'], ada_b=shg['ada_b'],
                 MinclTA=shg['cm']['MinclTA'], MafterTA=shg['cm']['MafterTA'], maskTA=shg['cm']['maskTA'])
        maps.append(d)
    r1 = run(_get('gla1', build_gla1), maps)
    maps = []
    for c in cores:
        b, hf = divmod(c, 2)
        d = prep_gla_common(shg, hf, x2[c])
        d.update(mods=r1[c]['mods'], oA=r1[c]['oA'], SB0=r1[c ^ 1]['SA'], w_out=shg['w_out'], norm_w=shg['norm_w'],
                 ln_g=shg['ln_g'], ln_b=shg['ln_b'],
                 MinclTB=shg['cm']['MinclTB'], MafterTB=shg['cm']['MafterTB'], maskTB=shg['cm']['maskTB'])
        maps.append(d)
    r2 = run(_get('gla2', build_gla2), maps)
    shm = shared_moe(inp, 1)
    maps = []
    for c in cores:
        d = dict(shm); d['mods'] = r1[c]['mods']; d['xin'] = r2[c]['xout']; maps.append(d)
    r3 = run(_get('moe1', lambda: build_moe(NOWN // 128, 0)), maps)
    out = np.empty((4, 2 * NOWN, DM), np.float32)
    for c in cores:
        b, hf = divmod(c, 2)
        xo = r3[c]['xout']
        out[b, hf * NOWN:(hf + 1) * NOWN] = xo[::-1] if hf == 1 else xo
    return out
```

```python
import numpy as np
from contextlib import ExitStack
import concourse.bass as bass
import concourse.mybir as mybir
from concourse.bass_utils import run_bass_kernel_spmd

F32 = mybir.dt.float32
BF16 = mybir.dt.bfloat16
AF = mybir.ActivationFunctionType
ALU = mybir.AluOpType
AX = mybir.AxisListType

NCTX = 256
NOWN = 4096
NQ = NCTX + NOWN
NK = NQ + NOWN
DM = 1024
ALPHA = (2.0 * 2) ** 0.25
EPS = 1e-5


class T:
    __slots__ = ('t', 'w', 'wf', 'r', 'name')

    def __init__(self, t, name=''):
        self.t = t; self.w = []; self.wf = None; self.r = []; self.name = name

    def __getitem__(self, k):
        return self.t[k]


class Sched:
    ENG = ('tensor', 'vector', 'scalar', 'gpsimd', 'sync')
    EPOCH = 16000
    KDMA = 8

    def __init__(self, nc, stack):
        self.nc = nc; self.stack = stack
        self.cnt = {e: 0 for e in self.ENG}
        self.sems = {e: [] for e in self.ENG}
        self.dcnt = {e: 0 for e in self.ENG}
        self.dsems = {e: [] for e in self.ENG}
        self.waited = {e: {} for e in self.ENG}
        self.nwaits = 0

    def _sem(self, name):
        return self.stack.enter_context(self.nc.semaphore(name))

    def _uname(self, name):
        self.uid = getattr(self, 'uid', 0) + 1
        return f"{name}_{self.uid}"

    def sbuf(self, name, shape, dt, stack=None):
        st = stack or self.stack
        return T(st.enter_context(self.nc.sbuf_tensor(self._uname(name), list(shape), dt)), name)

    def psum(self, name, shape, dt=F32, stack=None):
        st = stack or self.stack
        return T(st.enter_context(self.nc.psum_tensor(self._uname(name), list(shape), dt)), name)

    def special(self, eng, fn, reads=(), writes=()):
        deps = self._deps(eng, reads, writes, ())
        sem = self._sem(self._uname('x_' + eng))
        tok = ('x', sem, 1)
        e = self._emit_waits(eng, deps)
        fn(e).then_inc(sem)
        self._commit(tok, reads, writes, ())
        return tok

    def _need(self, issuer, tok, same_ok=False):
        if tok is None:
            return None
        if tok[0] == 'c':
            _, e, ep, n = tok
            if e == issuer and same_ok:
                return None
            key = ('c', e)
            if (ep, n) <= self.waited[issuer].get(key, (-1, 0)):
                return None
            self.waited[issuer][key] = (ep, n)
            return tok
        if tok[0] == 'x':
            key = ('x', id(tok[1]))
            if self.waited[issuer].get(key, 0) >= tok[2]:
                return None
            self.waited[issuer][key] = tok[2]
            return tok
        _, e, slot, val = tok
        key = ('d', e, slot)
        if self.waited[issuer].get(key, 0) >= val:
            return None
        self.waited[issuer][key] = val
        return tok

    def _deps(self, issuer, reads, writes, pwrites):
        out = []

        def add(tk, same_ok):
            k = self._need(issuer, tk, same_ok)
            if k:
                out.append(k)
        for r in reads:
            for tk in r.w:
                add(tk, False)
        for w in writes:
            for tk in w.w:
                add(tk, True)
            for tk in w.r:
                add(tk, True)
        for w in pwrites:
            add(w.wf, True)
            for tk in w.r:
                add(tk, True)
        return out

    @staticmethod
    def _push(lst, tok):
        if tok[0] == 'c':
            lst[:] = [x for x in lst if not (x[0] == 'c' and x[1] == tok[1])]
        lst.append(tok)

    def _commit(self, tok, reads, writes, pwrites):
        for r in reads:
            self._push(r.r, tok)
        for w in writes:
            w.w = [tok]; w.wf = tok; w.r = []
        for w in pwrites:
            self._push(w.w, tok); w.r = []

    def semof(self, tok):
        if tok[0] == 'c':
            return self.sems[tok[1]][tok[2]], tok[3]
        if tok[0] == 'x':
            return tok[1], tok[2]
        return self.dsems[tok[1]][tok[2]], tok[3]

    def _emit_waits(self, eng, deps):
        e = getattr(self.nc, eng)
        for d in deps:
            s, v = self.semof(d)
            e.wait_ge(s, v)
            self.nwaits += 1
        return e

    def op(self, eng, fn, reads=(), writes=(), pwrites=()):
        deps = self._deps(eng, reads, writes, pwrites)
        n = self.cnt[eng]; ep, k = divmod(n, self.EPOCH)
        if k == 0:
            self.sems[eng].append(self._sem(f's_{eng}_{ep}'))
        self.cnt[eng] = n + 1
        tok = ('c', eng, ep, k + 1)
        e = self._emit_waits(eng, deps)
        fn(e).then_inc(self.sems[eng][ep], 1)
        self._commit(tok, reads, writes, pwrites)
        return tok

    def dma(self, eng, fn, reads=(), writes=(), pwrites=()):
        deps = self._deps(eng, reads, writes, pwrites)
        i = self.dcnt[eng]; self.dcnt[eng] = i + 1
        rnd, slot = divmod(i, self.KDMA)
        if rnd == 0:
            self.dsems[eng].append(self._sem(f'd_{eng}_{slot}'))
        else:
            k = self._need(eng, ('d', eng, slot, 16 * rnd))
            if k:
                deps.append(k)
        tok = ('d', eng, slot, 16 * (rnd + 1))
        e = self._emit_waits(eng, deps)
        fn(e).then_inc(self.dsems[eng][slot], 16)
        self._commit(tok, reads, writes, pwrites)
        return tok

    def all_tokens(self):
        toks = []
        for e in self.ENG:
            n = self.cnt[e]
            if n:
                ep, k = divmod(n - 1, self.EPOCH)
                toks.append(('c', e, ep, k + 1))
            for i in range(max(0, self.dcnt[e] - self.KDMA), self.dcnt[e]):
                rnd, slot = divmod(i, self.KDMA)
                toks.append(('d', e, slot, 16 * (rnd + 1)))
        return toks

    def barrier(self, engines=None):
        toks = self.all_tokens()
        for e in (engines or self.ENG):
            deps = [k for k in (self._need(e, t, same_ok=True) for t in toks) if k]
            self._emit_waits(e, deps)


def _dram(nc, name, shape, dt, kind="Internal"):
    return nc.dram_tensor(name, list(shape), dt, kind=kind).ap()


class RR:
    def __init__(self, items):
        self.items = items; self.i = 0

    def __call__(self):
        x = self.items[self.i % len(self.items)]; self.i += 1
        return x


def emit_mods(S, nc, st, cT, ada_w, ada_b, mods_d, modsT):
    cs = S.sbuf("m_cs", [128, 8, 2], F32, st)
    S.dma('sync', lambda e: e.dma_start(out=cs[:], in_=cT.rearrange("(kc p) m -> p kc m", p=128)),
          writes=[cs])
    S.op('scalar', lambda e: e.activation(out=cs[:], in_=cs[:], func=AF.Silu), reads=[cs], writes=[cs])
    ab = S.sbuf("m_ab", [2, 6144], F32, st)
    S.dma('sync', lambda e: e.dma_start(out=ab[:], in_=ada_b.partition_broadcast(2)), writes=[ab])
    msb = S.sbuf("m_sb", [2, 6144], F32, st)
    aw = [S.sbuf(f"m_aw{i}", [128, 8, 512], F32, st) for i in range(2)]
    ps = [S.psum(f"m_ps{i}", [2, 512], F32, st) for i in range(2)]
    awv = ada_w.rearrange("(kc p) n -> p kc n", p=128)
    for nb in range(12):
        a = aw[nb % 2]; p = ps[nb % 2]
        S.dma('sync', lambda e, a=a, nb=nb: e.dma_start(out=a[:], in_=awv[:, :, nb * 512:(nb + 1) * 512]),
              writes=[a])
        for kc in range(8):
            S.op('tensor', lambda e, a=a, p=p, kc=kc: e.matmul(p[:], lhsT=cs[:, kc, :], rhs=a[:, kc, :],
                                                                start=(kc == 0), stop=(kc == 7)),
                 reads=[cs, a], writes=[p])
        S.op('vector', lambda e, p=p, nb=nb: e.tensor_tensor(out=msb[:, nb * 512:(nb + 1) * 512], in0=p[:],
                                                             in1=ab[:, nb * 512:(nb + 1) * 512], op=ALU.add),
             reads=[p, ab], pwrites=[msb])
    S.dma('sync', lambda e: e.dma_start(out=mods_d[:, :], in_=msb[:]), reads=[msb], writes=[modsT])


def load_bcast(S, st, name, src_row, modsT=None, eng='sync'):
    t = S.sbuf(name, [128, DM], F32, st)
    S.dma(eng, lambda e: e.dma_start(out=t[:], in_=src_row.partition_broadcast(128)),
          reads=([modsT] if modsT is not None else []), writes=[t])
    return t


def attn_inputs(nc):
    EI = "ExternalInput"
    return dict(
        xk=_dram(nc, "xk", [DM, NK], F32, EI), xtm=_dram(nc, "xtm", [NQ, DM], F32, EI),
        ropeC=_dram(nc, "ropeC", [128, NK], F32, EI), ropeS=_dram(nc, "ropeS", [128, NK], F32, EI),
        cT=_dram(nc, "cT", [DM, 2], F32, EI),
        ada_w=_dram(nc, "ada_w", [DM, 6 * DM], F32, EI), ada_b=_dram(nc, "ada_b", [1, 6 * DM], F32, EI),
        w_in=_dram(nc, "w_in", [DM, 3 * DM], F32, EI), w_perm=_dram(nc, "w_perm", [DM, 2 * DM], F32, EI),
        w_out=_dram(nc, "w_out", [DM, DM], F32, EI), lamv=_dram(nc, "lamv", [1, 256], F32, EI),
        subln=_dram(nc, "subln", [1, 128], F32, EI), ln_g=_dram(nc, "ln_g", [1, DM], F32, EI),
        ln_b=_dram(nc, "ln_b", [1, DM], F32, EI), ident=_dram(nc, "ident", [128, 128], F32, EI))


def build_attn(lam_init):
    nc = bass.Bass("TRN2", target_bir_lowering=False)
    I = attn_inputs(nc)
    x1 = _dram(nc, "x1", [NQ, DM], F32, "ExternalOutput")
    mods_d = _dram(nc, "mods", [2, 6 * DM], F32, "ExternalOutput")
    with ExitStack() as st0:
        S = Sched(nc, st0)
        emit_attn(S, nc, I, lam_init, x1, T(x1, "x1"), mods_d, T(mods_d, "mods"))
    return nc


def emit_attn(S, nc, I, lam_init, x1, x1T, mods_d, modsT):
    xk, xtm, ropeC, ropeS, cT = I['xk'], I['xtm'], I['ropeC'], I['ropeS'], I['cT']
    ada_w, ada_b, w_in, w_perm, w_out = I['ada_w'], I['ada_b'], I['w_in'], I['w_perm'], I['w_out']
    lamv, subln, ln_g, ln_b, ident_d = I['lamv'], I['subln'], I['ln_g'], I['ln_b'], I['ident']
    QT = _dram(nc, "QT", [8, 128, NQ], BF16)
    KT = _dram(nc, "KT", [8, 128, NK], BF16)
    Vd = _dram(nc, "Vd", [8, NK, 129], BF16)
    with ExitStack() as st0:
        QTt = [T(QT[h], f"QT{h}") for h in range(8)]
        KTt = [T(KT[h], f"KT{h}") for h in range(8)]
        Vt = [T(Vd[h], f"V{h}") for h in range(8)]
        with ExitStack() as st:
            emit_mods(S, nc, st, cT, ada_w, ada_b, mods_d, modsT)
            S.barrier()
        modp = S.sbuf("modp", [128, 2, 6, 8], F32, st0)
        with nc.allow_non_contiguous_dma(reason="tiny per-partition mod vectors"):
            for m in range(2):
                S.dma('sync', lambda e, m=m: e.dma_start(
                    out=modp[:, m], in_=mods_d[m:m + 1, :].rearrange("o (j kc p) -> p (o j) kc", j=6, kc=8, p=128)),
                    reads=[modsT], pwrites=[modp])
        onep = S.sbuf("onep", [128, 2, 8], F32, st0)
        S.op('vector', lambda e: e.tensor_scalar_add(out=onep[:], in0=modp[:, :, 1, :], scalar1=1.0),
             reads=[modp], writes=[onep])

        with ExitStack() as st:
            wi = S.sbuf("wi", [128, 8, 3 * DM], BF16, st)
            wp = S.sbuf("wp", [128, 8, 2 * DM], BF16, st)
            wiv = w_in.rearrange("(kc p) n -> p kc n", p=128)
            wpv = w_perm.rearrange("(kc p) n -> p kc n", p=128)
            for kc in range(8):
                for c0 in range(0, 3 * DM, 1024):
                    S.dma('gpsimd', lambda e, kc=kc, c0=c0: e.dma_start(out=wi[:, kc, c0:c0 + 1024],
                                                                        in_=wiv[:, kc, c0:c0 + 1024]), pwrites=[wi])
                for c0 in range(0, 2 * DM, 1024):
                    S.dma('gpsimd', lambda e, kc=kc, c0=c0: e.dma_start(out=wp[:, kc, c0:c0 + 1024],
                                                                        in_=wpv[:, kc, c0:c0 + 1024]), pwrites=[wp])
            xb = [S.sbuf(f"xb{i}", [128, 8, 512], F32, st) for i in range(2)]
            tb = [S.sbuf(f"tb{i}", [128, 8, 512], BF16, st) for i in range(2)]
            rc = [S.sbuf(f"rc{i}", [128, 512], F32, st) for i in range(2)]
            rs = [S.sbuf(f"rs{i}", [128, 512], F32, st) for i in range(2)]
            psA = [S.psum(f"psA{i}", [128, 512], F32, st) for i in range(2)]
            psB = [S.psum(f"psB{i}", [128, 512], F32, st) for i in range(2)]
            psV = [S.psum(f"psV{i}", [128, 512], F32, st) for i in range(2)]
            t1 = [S.sbuf(f"t1_{i}", [128, 512], F32, st) for i in range(2)]
            t2 = [S.sbuf(f"t2_{i}", [128, 512], F32, st) for i in range(2)]
            qk = [S.sbuf(f"qk{i}", [128, 512], BF16, st) for i in range(4)]
            vs = [S.sbuf(f"vs{i}", [128, 8, 129], BF16, st) for i in range(3)]
            for v in vs:
                S.op('gpsimd', lambda e, v=v: e.memset(v[:], 1.0), writes=[v])
            xkv = xk.rearrange("(kc p) t -> p kc t", p=128)
            blocks = [(0, NCTX)] + [(NCTX + i * 512, 512) for i in range(16)]
            iqk = 0; ivs = 0; ips = 0
            for bi, (t0, nt) in enumerate(blocks):
                X = xb[bi % 2]; TB = tb[bi % 2]; RC = rc[bi % 2]; RS = rs[bi % 2]
                mset = 1 if bi == 0 else 0
                S.dma('sync', lambda e, X=X, t0=t0, nt=nt: e.dma_start(out=X[:, :, :nt], in_=xkv[:, :, t0:t0 + nt]),
                      writes=[X])
                S.dma('sync', lambda e, RC=RC, t0=t0, nt=nt: e.dma_start(out=RC[:, :nt], in_=ropeC[:, t0:t0 + nt]),
                      writes=[RC])
                S.dma('sync', lambda e, RS=RS, t0=t0, nt=nt: e.dma_start(out=RS[:, :nt], in_=ropeS[:, t0:t0 + nt]),
                      writes=[RS])
                for kc in range(8):
                    eng = 'vector' if kc % 2 == 0 else 'gpsimd'
                    S.op(eng, lambda e, X=X, TB=TB, kc=kc, nt=nt, mset=mset: e.tensor_scalar(
                        out=TB[:, kc, :nt], in0=X[:, kc, :nt], scalar1=onep[:, mset, kc:kc + 1],
                        scalar2=modp[:, mset, 0, kc:kc + 1], op0=ALU.mult, op1=ALU.add),
                        reads=[X, onep, modp], pwrites=[TB])
                has_q = t0 < NQ
                ccs = ([('q', h) for h in range(8)] if has_q else []) + [('k', h) for h in range(8)]
                for kind, h in ccs:
                    c0 = (0 if kind == 'q' else DM) + h * 128
                    pa = psA[ips % 2]; pb = psB[ips % 2]; a1 = t1[ips % 2]; a2 = t2[ips % 2]; ips += 1
                    for kc in range(8):
                        S.op('tensor', lambda e, pa=pa, TB=TB, kc=kc, c0=c0, nt=nt: e.matmul(
                            pa[:, :nt], lhsT=wi[:, kc, c0:c0 + 128], rhs=TB[:, kc, :nt], start=(kc == 0), stop=(kc == 7)),
                            reads=[wi, TB], writes=[pa])
                    for kc in range(8):
                        S.op('tensor', lambda e, pb=pb, TB=TB, kc=kc, c0=c0, nt=nt: e.matmul(
                            pb[:, :nt], lhsT=wp[:, kc, c0:c0 + 128], rhs=TB[:, kc, :nt], start=(kc == 0), stop=(kc == 7)),
                            reads=[wp, TB], writes=[pb])
                    S.op('vector', lambda e, pa=pa, a1=a1, RC=RC, nt=nt: e.tensor_tensor(
                        out=a1[:, :nt], in0=pa[:, :nt], in1=RC[:, :nt], op=ALU.mult), reads=[pa, RC], writes=[a1])
                    S.op('vector', lambda e, pb=pb, a2=a2, RS=RS, nt=nt: e.tensor_tensor(
                        out=a2[:, :nt], in0=pb[:, :nt], in1=RS[:, :nt], op=ALU.mult), reads=[pb, RS], writes=[a2])
                    o = qk[iqk % 4]; iqk += 1
                    S.op('gpsimd', lambda e, o=o, a1=a1, a2=a2, nt=nt: e.tensor_tensor(
                        out=o[:, :nt], in0=a1[:, :nt], in1=a2[:, :nt], op=ALU.add), reads=[a1, a2], writes=[o])
                    if kind == 'q':
                        S.dma('sync', lambda e, o=o, h=h, t0=t0, nt=nt: e.dma_start(out=QT[h, :, t0:t0 + nt], in_=o[:, :nt]),
                              reads=[o], pwrites=[QTt[h]])
                    else:
                        S.dma('sync', lambda e, o=o, h=h, t0=t0, nt=nt: e.dma_start(out=KT[h, :, t0:t0 + nt], in_=o[:, :nt]),
                              reads=[o], pwrites=[KTt[h]])
                for ti in range(nt // 128):
                    V = vs[ivs % 3]; ivs += 1
                    for nh in range(2):
                        pv = psV[nh]
                        for kc in range(8):
                            S.op('tensor', lambda e, pv=pv, TB=TB, kc=kc, ti=ti, nh=nh: e.matmul(
                                pv[:], lhsT=TB[:, kc, ti * 128:(ti + 1) * 128],
                                rhs=wi[:, kc, 2 * DM + nh * 512:2 * DM + (nh + 1) * 512], start=(kc == 0), stop=(kc == 7)),
                                reads=[wi, TB], writes=[pv])
                        S.op('scalar', lambda e, pv=pv, V=V, nh=nh: e.activation(
                            out=V[:, nh * 4:(nh + 1) * 4, 0:128], in_=pv[:].rearrange("p (h d) -> p h d", h=4),
                            func=AF.Copy), reads=[pv], pwrites=[V])
                    r0 = t0 + ti * 128
                    S.dma('sync', lambda e, V=V, r0=r0: e.dma_start(
                        out=Vd[:, r0:r0 + 128, :].rearrange("h t d -> t h d"), in_=V[:]),
                        reads=[V], pwrites=Vt)

        S.barrier()
        onT = S.sbuf("onT", [128, 8, NQ], BF16, st0)
        with ExitStack() as st:
            ident = S.sbuf("ident_sb", [128, 128], BF16, st)
            S.dma('gpsimd', lambda e: e.dma_start(out=ident[:], in_=ident_d[:, :]), writes=[ident])
            lv = S.sbuf("lv", [1, 256], F32, st)
            S.dma('sync', lambda e: e.dma_start(out=lv[:], in_=lamv[:, :]), writes=[lv])
            pr = S.sbuf("pr", [1, 2, 64], F32, st)
            lvv = lv[:].rearrange("p (a b c) -> p a b c", a=2, b=2)
            S.op('vector', lambda e: e.tensor_tensor(out=pr[:], in0=lvv[:, :, 0, :], in1=lvv[:, :, 1, :], op=ALU.mult),
                 reads=[lv], writes=[pr])
            sm = S.sbuf("sm", [1, 2], F32, st)
            S.op('vector', lambda e: e.reduce_sum(out=sm[:], in_=pr[:], axis=AX.X), reads=[pr], writes=[sm])
            S.op('scalar', lambda e: e.activation(out=sm[:], in_=sm[:], func=AF.Exp), reads=[sm], writes=[sm])
            lam1 = S.sbuf("lam1", [1, 1], F32, st)
            S.op('vector', lambda e: e.tensor_tensor(out=lam1[:], in0=sm[:, 0:1], in1=sm[:, 1:2], op=ALU.subtract),
                 reads=[sm], writes=[lam1])
            S.op('vector', lambda e: e.tensor_scalar(out=lam1[:], in0=lam1[:], scalar1=float(lam_init), scalar2=-1.0,
                                                     op0=ALU.add, op1=ALU.mult), reads=[lam1], writes=[lam1])
            ones1 = S.sbuf("ones1", [1, 128], F32, st)
            S.op('vector', lambda e: e.memset(ones1[:], 1.0), writes=[ones1])
            psT = S.psum("psT", [128, 1024], BF16, st)
            psl = S.psum("psl", [128, 512], F32, st)
            S.op('tensor', lambda e: e.matmul(psl[:, 0:1], lhsT=ones1[0:1, :], rhs=lam1[0:1, 0:1], start=True, stop=True),
                 reads=[ones1, lam1], writes=[psl])
            nlam = S.sbuf("nlam", [128, 1], F32, st)
            S.op('vector', lambda e: e.tensor_copy(out=nlam[:], in_=psl[:, 0:1]), reads=[psl], writes=[nlam])
            sw = S.sbuf("sw", [128, 128], F32, st)
            S.dma('sync', lambda e: e.dma_start(out=sw[:], in_=subln.partition_broadcast(128)), writes=[sw])
            S.op('vector', lambda e: e.tensor_scalar_mul(out=sw[:], in0=sw[:], scalar1=float(1.0 - lam_init)),
                 reads=[sw], writes=[sw])

            KTs = [S.sbuf(f"KTs{i}", [128, NK], BF16, st) for i in range(2)]
            Vs = [S.sbuf(f"Vs{i}", [128, 66, 129], BF16, st) for i in range(2)]
            QTs = [S.sbuf(f"QTs{i}", [128, NQ], BF16, st) for i in range(2)]
            psS = [S.psum(f"psS{i}", [128, 512], F32, st) for i in range(3)]
            psO = [S.psum(f"psO{i}", [128, 512], F32, st) for i in range(3)]
            pts = [S.sbuf(f"pt{i}", [128, 512], BF16, st) for i in range(3)]
            Om = [S.sbuf(f"Om{i}", [128, 4, 129], F32, st) for i in range(2)]
            rec = S.sbuf("rec", [128, 2, 4], F32, st)
            osb = S.sbuf("osb", [128, 128], F32, st)
            sq = S.sbuf("sq", [128, 128], F32, st)
            ssq = S.sbuf("ssq", [128, 1], F32, st)
            onb = [S.sbuf(f"onb{i}", [128, 128], BF16, st) for i in range(2)]
            qblocks = [(0, NCTX, NCTX // 128)] + [(NCTX + i * 512, 512, NK // 128) for i in range(8)]
            iS = 0; iO = 0; iT = 0
            for h in range(8):
                KS = KTs[h % 2]; VS = Vs[h % 2]; QS = QTs[h % 2]
                S.dma('sync', lambda e, KS=KS, h=h: e.dma_start(out=KS[:], in_=KT[h]), reads=[KTt[h]], writes=[KS])
                S.dma('sync', lambda e, VS=VS, h=h: e.dma_start(out=VS[:], in_=Vd[h].rearrange("(kc p) d -> p kc d", p=128)),
                      reads=[Vt[h]], writes=[VS])
                S.dma('sync', lambda e, QS=QS, h=h: e.dma_start(out=QS[:], in_=QT[h]), reads=[QTt[h]], writes=[QS])
                for (q0, nq, nkc) in qblocks:
                    nqs = nq // 128
                    for m in range(2):
                        pO = [psO[iO % 3], psO[(iO + 1) % 3]] if nqs > 2 else [psO[iO % 3]]
                        iO += len(pO)
                        for kc in range(nkc):
                            pS = psS[iS % 3]; PT = pts[iS % 3]; iS += 1
                            S.op('tensor', lambda e, pS=pS, KS=KS, QS=QS, m=m, kc=kc, q0=q0, nq=nq: e.matmul(
                                pS[:, :nq], lhsT=KS[m * 64:(m + 1) * 64, kc * 128:(kc + 1) * 128],
                                rhs=QS[m * 64:(m + 1) * 64, q0:q0 + nq], start=True, stop=True),
                                reads=[KS, QS], writes=[pS])
                            S.op('scalar', lambda e, pS=pS, PT=PT, nq=nq: e.activation(
                                out=PT[:, :nq], in_=pS[:, :nq], func=AF.Exp, scale=0.125), reads=[pS], writes=[PT])
                            for qs in range(nqs):
                                po = pO[qs // 2]; c0 = (qs % 2) * 129
                                S.op('tensor', lambda e, po=po, c0=c0, PT=PT, VS=VS, qs=qs, kc=kc, nkc=nkc: e.matmul(
                                    po[:, c0:c0 + 129], lhsT=PT[:, qs * 128:(qs + 1) * 128], rhs=VS[:, kc, :],
                                    start=(kc == 0), stop=(kc == nkc - 1)), reads=[PT, VS], writes=[po])
                        for j, po in enumerate(pO):
                            S.op('vector', lambda e, po=po, j=j, m=m: e.tensor_copy(
                                out=Om[m][:, 2 * j:2 * j + 2, :], in_=po[:, 0:258].rearrange("p (a b) -> p a b", a=2)),
                                reads=[po], pwrites=[Om[m]])
                    S.op('vector', lambda e, nqs=nqs: e.reciprocal(out=rec[:, 0, :nqs], in_=Om[0][:, :nqs, 128]),
                         reads=[Om[0]], pwrites=[rec])
                    S.op('vector', lambda e, nqs=nqs: e.reciprocal(out=rec[:, 1, :nqs], in_=Om[1][:, :nqs, 128]),
                         reads=[Om[1]], pwrites=[rec])
                    S.op('vector', lambda e, nqs=nqs: e.tensor_scalar_mul(out=rec[:, 1, :nqs], in0=rec[:, 1, :nqs],
                                                                          scalar1=nlam[:, 0:1]),
                         reads=[rec, nlam], writes=[rec])
                    for qs in range(nqs):
                        S.op('vector', lambda e, qs=qs: e.tensor_scalar_mul(out=osb[:], in0=Om[0][:, qs, 0:128],
                                                                            scalar1=rec[:, 0, qs:qs + 1]),
                             reads=[Om[0], rec], writes=[osb])
                        S.op('vector', lambda e, qs=qs: e.scalar_tensor_tensor(
                            out=osb[:], in0=Om[1][:, qs, 0:128], scalar=rec[:, 1, qs:qs + 1], in1=osb[:],
                            op0=ALU.mult, op1=ALU.add), reads=[Om[1], rec, osb], writes=[osb])
                        S.op('gpsimd', lambda e: e.tensor_tensor(out=sq[:], in0=osb[:], in1=osb[:], op=ALU.mult),
                             reads=[osb], writes=[sq])
                        S.op('vector', lambda e: e.reduce_sum(out=ssq[:], in_=sq[:], axis=AX.X), reads=[sq], writes=[ssq])
                        S.op('vector', lambda e: e.tensor_scalar(out=ssq[:], in0=ssq[:], scalar1=1.0 / 128, scalar2=EPS,
                                                                 op0=ALU.mult, op1=ALU.add), reads=[ssq], writes=[ssq])
                        S.op('scalar', lambda e: e.activation(out=ssq[:], in_=ssq[:], func=AF.Sqrt), reads=[ssq], writes=[ssq])
                        S.op('vector', lambda e: e.reciprocal(out=ssq[:], in_=ssq[:]), reads=[ssq], writes=[ssq])
                        ob = onb[iT % 2]; iT += 1
                        S.op('vector', lambda e, ob=ob: e.scalar_tensor_tensor(
                            out=ob[:], in0=osb[:], scalar=ssq[:, 0:1], in1=sw[:], op0=ALU.mult, op1=ALU.mult),
                            reads=[osb, ssq, sw], writes=[ob])
                        S.op('tensor', lambda e, ob=ob: e.transpose(out=psT[:, 0:128], in_=ob[:], identity=ident[:]),
                             reads=[ob, ident], writes=[psT])
                        tq = q0 + qs * 128
                        S.op('scalar', lambda e, h=h, tq=tq: e.copy(out=onT[:, h, tq:tq + 128], in_=psT[:, 0:128]),
                             reads=[psT], pwrites=[onT])

        S.barrier()
        with ExitStack() as st:
            wo = S.sbuf("wo", [128, 8, DM], BF16, st)
            S.dma('gpsimd', lambda e: e.dma_start(out=wo[:], in_=w_out.rearrange("(kc p) n -> p kc n", p=128)),
                  writes=[wo])
            g1b = [load_bcast(S, st, f"g1b{m}", mods_d[m:m + 1, 2 * DM:3 * DM], modsT) for m in range(2)]
            lngb = load_bcast(S, st, "lngb", ln_g)
            lnbb = load_bcast(S, st, "lnbb", ln_b)
            psY = [S.psum(f"psY{i}", [128, 512], F32, st) for i in range(4)]
            xts = [S.sbuf(f"xts{i}", [128, DM], F32, st) for i in range(2)]
            zs = [S.sbuf(f"zs{i}", [128, DM], F32, st) for i in range(2)]
            x1s = [S.sbuf(f"x1s{i}", [128, DM], F32, st) for i in range(2)]
            lnsc = LNScratch(S, st, "lnc")
            outs = []
            for ti in range(NQ // 128):
                mset = 1 if ti < 2 else 0
                xt = xts[ti % 2]; z = zs[ti % 2]; xo = x1s[ti % 2]
                S.dma('sync', lambda e, xt=xt, ti=ti: e.dma_start(out=xt[:], in_=xtm[ti * 128:(ti + 1) * 128, :]), writes=[xt])
                for nh in range(2):
                    py = psY[(ti % 2) * 2 + nh]
                    for h in range(8):
                        S.op('tensor', lambda e, py=py, h=h, ti=ti, nh=nh: e.matmul(
                            py[:], lhsT=onT[:, h, ti * 128:(ti + 1) * 128], rhs=wo[:, h, nh * 512:(nh + 1) * 512],
                            start=(h == 0), stop=(h == 7)), reads=[onT, wo], writes=[py])
                    S.op('vector', lambda e, py=py, z=z, nh=nh, mset=mset: e.tensor_tensor(
                        out=z[:, nh * 512:(nh + 1) * 512], in0=py[:], in1=g1b[mset][:, nh * 512:(nh + 1) * 512], op=ALU.mult),
                        reads=[py, g1b[mset]], pwrites=[z])
                S.op('vector', lambda e, xt=xt, z=z: e.scalar_tensor_tensor(
                    out=z[:], in0=xt[:], scalar=ALPHA, in1=z[:], op0=ALU.mult, op1=ALU.add), reads=[xt, z], writes=[z])
                emit_ln(S, lnsc, z, xo, lngb, lnbb)
                outs.append(S.dma('sync', lambda e, xo=xo, ti=ti: e.dma_start(out=x1[ti * 128:(ti + 1) * 128, :], in_=xo[:]),
                                  reads=[xo], pwrites=[x1T]))
        S.barrier()


class LNScratch:
    def __init__(self, S, st, pfx):
        self.stats = S.sbuf(pfx + "_st", [128, 2, 6], F32, st)
        self.mv = S.sbuf(pfx + "_mv", [128, 2], F32, st)
        self.rstd = S.sbuf(pfx + "_rs", [128, 1], F32, st)


def emit_ln(S, sc, z, out, lng, lnb, eng2='gpsimd'):
    for i in range(2):
        S.op('vector', lambda e, i=i: e.bn_stats(out=sc.stats[:, i, :], in_=z[:, i * 512:(i + 1) * 512]),
             reads=[z], pwrites=[sc.stats])
    S.op('vector', lambda e: e.bn_aggr(out=sc.mv[:], in_=sc.stats[:].rearrange("p a b -> p (a b)")),
         reads=[sc.stats], writes=[sc.mv])
    S.op('vector', lambda e: e.tensor_scalar_add(out=sc.rstd[:], in0=sc.mv[:, 1:2], scalar1=EPS),
         reads=[sc.mv], writes=[sc.rstd])
    S.op('scalar', lambda e: e.activation(out=sc.rstd[:], in_=sc.rstd[:], func=AF.Sqrt), reads=[sc.rstd], writes=[sc.rstd])
    S.op('vector', lambda e: e.reciprocal(out=sc.rstd[:], in_=sc.rstd[:]), reads=[sc.rstd], writes=[sc.rstd])
    S.op('vector', lambda e: e.tensor_scalar(out=z[:], in0=z[:], scalar1=sc.mv[:, 0:1], scalar2=sc.rstd[:, 0:1],
                                             op0=ALU.subtract, op1=ALU.mult), reads=[z, sc.mv, sc.rstd], writes=[z])
    S.op(eng2, lambda e: e.tensor_tensor(out=z[:], in0=z[:], in1=lng[:], op=ALU.mult),
         reads=[z, lng], writes=[z])
    S.op(eng2, lambda e: e.tensor_tensor(out=out[:], in0=z[:], in1=lnb[:], op=ALU.add),
         reads=[z, lnb], writes=[out])


def _lambda_init(layer_idx):
    import math
    return 0.8 - 0.6 * math.exp(-0.3 * layer_idx)


def _rope_tables(pos, n_ctx):
    pos = np.asarray(pos)
    row = (pos // 64).astype(np.float32); col = (pos % 64).astype(np.float32)
    inv = (np.float32(10000.0) ** (-np.arange(16, dtype=np.float32) / np.float32(16))).astype(np.float32)
    ang = np.concatenate([row[:, None] * inv, col[:, None] * inv], -1).astype(np.float32)
    cos = np.cos(ang).astype(np.float32); sin = np.sin(ang).astype(np.float32)
    C64 = np.concatenate([cos, cos], -1); S64 = np.concatenate([-sin, sin], -1)
    C = np.concatenate([np.ones((n_ctx, 64), np.float32), C64], 0)
    Sg = np.concatenate([np.zeros((n_ctx, 64), np.float32), S64], 0)
    C = np.concatenate([C, C], -1).T; Sg = np.concatenate([Sg, Sg], -1).T
    return np.ascontiguousarray(C), np.ascontiguousarray(Sg)


def _perm_cols():
    idx = np.arange(2 * DM).reshape(2, 8, 2, 64)
    return np.concatenate([idx[..., 32:], idx[..., :32]], -1).reshape(-1)


def prep_attn(inp, core, shared):
    b, hf = divmod(core, 2)
    x = inp['x'][b]; ctx = inp['ctx'][b]
    own = x[hf * NOWN:(hf + 1) * NOWN]; oth = x[(1 - hf) * NOWN:(2 - hf) * NOWN]
    pos_own = np.arange(hf * NOWN, (hf + 1) * NOWN)
    if hf == 1:
        own = own[::-1]; ctx = ctx[::-1]; pos_own = pos_own[::-1]
    pos = np.concatenate([pos_own, np.arange((1 - hf) * NOWN, (2 - hf) * NOWN)])
    C, Sg = _rope_tables(pos, NCTX)
    d = dict(shared)
    d.update(
        xk=np.ascontiguousarray(np.concatenate([ctx, own, oth], 0).T),
        xtm=np.ascontiguousarray(np.concatenate([ctx, own], 0)),
        ropeC=C, ropeS=Sg,
        cT=np.ascontiguousarray(np.stack([inp['c'][b], inp['c_ctx']], 1)),
    )
    return d


def shared_attn(inp):
    w_in = inp['da_w_in'][0]
    return dict(
        ada_w=inp['ada_w'][0], ada_b=inp['ada_b'][0][None, :],
        w_in=w_in, w_perm=np.ascontiguousarray(w_in[:, :2 * DM][:, _perm_cols()]),
        w_out=inp['da_w_out'][0], lamv=inp['da_lambda'][0].reshape(1, 256),
        subln=inp['da_subln_w'][0][None, :], ln_g=inp['ln_g'][0, 0][None, :], ln_b=inp['ln_b'][0, 0][None, :],
        ident=np.eye(128, dtype=np.float32),
    )


def emit_moe(S, nc, xin, xinT, mods_d, modsT, router_w, router_b, w_gu, b_gu, w_down, b_down, ln_g, ln_b,
             ident_d, xout, xoutT, ntiles, nctx_tiles):
    NE = 32
    group = -(-ntiles // 3)
    with ExitStack() as st:
        identb = S.sbuf("mo_identb", [128, 128], BF16, st)
        S.dma('gpsimd', lambda e: e.dma_start(out=identb[:], in_=ident_d[:, :]), writes=[identb])
        identf = S.sbuf("mo_identf", [128, 128], F32, st)
        S.dma('sync', lambda e: e.dma_start(out=identf[:], in_=ident_d[:, :]), writes=[identf])
        nsets = 2 if nctx_tiles else 1
        sc2b = [load_bcast(S, st, f"mo_sc2b{m}", mods_d[m:m + 1, 4 * DM:5 * DM], modsT) for m in range(nsets)]
        sh2b = [load_bcast(S, st, f"mo_sh2b{m}", mods_d[m:m + 1, 3 * DM:4 * DM], modsT) for m in range(nsets)]
        g2b = [load_bcast(S, st, f"mo_g2b{m}", mods_d[m:m + 1, 5 * DM:6 * DM], modsT) for m in range(nsets)]
        for t in sc2b:
            S.op('gpsimd', lambda e, t=t: e.tensor_scalar_add(out=t[:], in0=t[:], scalar1=1.0), reads=[t], writes=[t])
        lngb = load_bcast(S, st, "mo_lngb", ln_g)
        lnbb = load_bcast(S, st, "mo_lnbb", ln_b)
        rw = S.sbuf("mo_rw", [128, 8, NE], BF16, st)
        S.dma('gpsimd', lambda e: e.dma_start(out=rw[:], in_=router_w.rearrange("(kc p) n -> p kc n", p=128)), writes=[rw])
        rbb = S.sbuf("mo_rbb", [128, NE], F32, st)
        S.dma('sync', lambda e: e.dma_start(out=rbb[:], in_=router_b.partition_broadcast(128)), writes=[rbb])
        bdn = S.sbuf("mo_bdn", [NE, DM], F32, st)
        S.dma('sync', lambda e: e.dma_start(out=bdn[:], in_=b_down[:, :]), writes=[bdn])
        bguT = S.sbuf("mo_bguT", [128, 16, NE], F32, st)
        st_tmp = ExitStack()
        bsb = S.sbuf("mo_bsb", [NE, 2 * DM], F32, st_tmp)
        S.dma('sync', lambda e: e.dma_start(out=bsb[:], in_=b_gu[:, :]), writes=[bsb])
        psX = [S.psum(f"mo_psX{i}", [128, 512], F32, st) for i in range(2)]
        psXb = S.psum("mo_psXb", [128, 1024], BF16, st)
        for c in range(16):
            p = psX[c % 2]
            S.op('tensor', lambda e, p=p, c=c: e.transpose(out=p[:, 0:NE], in_=bsb[0:NE, c * 128:(c + 1) * 128],
                                                           identity=identf[0:NE, 0:NE]), reads=[bsb, identf], writes=[p])
            if c < 8:
                S.op('vector', lambda e, p=p, c=c: e.tensor_copy(out=bguT[:, c, :], in_=p[:, 0:NE]), reads=[p], pwrites=[bguT])
            else:
                S.op('vector', lambda e, p=p, c=c: e.tensor_scalar_add(out=bguT[:, c, :], in0=p[:, 0:NE], scalar1=1.0),
                     reads=[p], pwrites=[bguT])
        S.barrier()
        st_tmp.close()
        wgu = S.sbuf("mo_wgu", [128, 8, 2 * DM], BF16, st)
        wdn = S.sbuf("mo_wdn", [128, 8, DM], BF16, st)
        wguT = [T(None) for _ in range(8)]
        wdnT = [T(None) for _ in range(8)]
        uT = S.sbuf("mo_uT", [128, 8, group * 128], BF16, st)
        acc = S.sbuf("mo_acc", [128, group, DM], F32, st)
        accT = [T(None) for _ in range(group)]
        G = S.sbuf("mo_G", [128, group, NE], F32, st)
        GT = S.sbuf("mo_GT", [NE, 128], F32, st)
        xt2 = [S.sbuf(f"mo_xt{i}", [128, DM], F32, st) for i in range(2)]
        ub = [S.sbuf(f"mo_ub{i}", [128, DM], BF16, st) for i in range(2)]
        lg = S.sbuf("mo_lg", [128, NE], F32, st)
        m8 = S.sbuf("mo_m8", [128, 8], F32, st)
        msk = S.sbuf("mo_msk", [128, NE], F32, st)
        ssum = S.sbuf("mo_ssum", [128, 1], F32, st)
        psG = [S.psum(f"mo_psG{i}", [128, 512], F32, st) for i in range(2)]
        psL = [S.psum(f"mo_psL{i}", [128, 512], F32, st) for i in range(2)]
        psY = psX
        gl = [S.sbuf(f"mo_gl{i}", [128, 512], F32, st) for i in range(2)]
        sg = [S.sbuf(f"mo_sg{i}", [128, 512], F32, st) for i in range(2)]
        l1 = [S.sbuf(f"mo_l1{i}", [128, 512], F32, st) for i in range(2)]
        actT = [S.sbuf(f"mo_act{i}", [128, 8, 512], BF16, st) for i in range(2)]
        xo = [S.sbuf(f"mo_xo{i}", [128, DM], F32, st) for i in range(1)]
        lnsc = LNScratch(S, st, "mo_lnc")
        wguv = w_gu.rearrange("e (kc p) n -> e p kc n", p=128)
        wdnv = w_down.rearrange("e (kc p) n -> e p kc n", p=128)
        it = 0
        for g0 in range(0, ntiles, group):
            gt = min(group, ntiles - g0)
            ntok = gt * 128
            for ti in range(gt):
                tg = g0 + ti
                mset = 1 if tg < nctx_tiles else 0
                xt = xt2[ti % 2]; u = ub[ti % 2]
                S.dma('sync', lambda e, xt=xt, tg=tg: e.dma_start(out=xt[:], in_=xin[tg * 128:(tg + 1) * 128, :]),
                      reads=[xinT], writes=[xt])
                S.op('vector', lambda e, xt=xt, mset=mset: e.tensor_tensor(out=xt[:], in0=xt[:], in1=sc2b[mset][:], op=ALU.mult),
                     reads=[xt, sc2b[mset]], writes=[xt])
                S.op('gpsimd', lambda e, xt=xt, u=u, mset=mset: e.tensor_tensor(out=u[:], in0=xt[:], in1=sh2b[mset][:], op=ALU.add),
                     reads=[xt, sh2b[mset]], writes=[u])
                for kc in range(8):
                    S.op('tensor', lambda e, u=u, kc=kc: e.transpose(out=psXb[:, kc * 128:(kc + 1) * 128],
                                                                     in_=u[:, kc * 128:(kc + 1) * 128], identity=identb[:]),
                         reads=[u, identb], pwrites=[psXb])
                S.op('scalar', lambda e, ti=ti: e.copy(out=uT[:, :, ti * 128:(ti + 1) * 128],
                                                       in_=psXb[:].rearrange("p (k t) -> p k t", k=8)),
                     reads=[psXb], pwrites=[uT])
                pr = psX[ti % 2]
                for kc in range(8):
                    S.op('tensor', lambda e, pr=pr, kc=kc, ti=ti: e.matmul(pr[:, 0:NE], lhsT=uT[:, kc, ti * 128:(ti + 1) * 128],
                                                                           rhs=rw[:, kc, :], start=(kc == 0), stop=(kc == 7)),
                         reads=[uT, rw], writes=[pr])
                S.op('vector', lambda e, pr=pr: e.tensor_tensor(out=lg[:], in0=pr[:, 0:NE], in1=rbb[:], op=ALU.add),
                     reads=[pr, rbb], writes=[lg])
                S.op('vector', lambda e: e.max(out=m8[:], in_=lg[:]), reads=[lg], writes=[m8])
                S.op('vector', lambda e: e.tensor_scalar(out=msk[:], in0=lg[:], scalar1=m8[:, 3:4], scalar2=None, op0=ALU.is_ge),
                     reads=[lg, m8], writes=[msk])
                S.op('vector', lambda e: e.tensor_scalar(out=lg[:], in0=lg[:], scalar1=m8[:, 0:1], scalar2=None, op0=ALU.subtract),
                     reads=[lg, m8], writes=[lg])
                S.op('scalar', lambda e: e.activation(out=lg[:], in_=lg[:], func=AF.Exp), reads=[lg], writes=[lg])
                S.op('vector', lambda e: e.tensor_tensor(out=lg[:], in0=lg[:], in1=msk[:], op=ALU.mult), reads=[lg, msk], writes=[lg])
                S.op('vector', lambda e: e.reduce_sum(out=ssum[:], in_=lg[:], axis=AX.X), reads=[lg], writes=[ssum])
                S.op('vector', lambda e: e.reciprocal(out=ssum[:], in_=ssum[:]), reads=[ssum], writes=[ssum])
                S.op('vector', lambda e, ti=ti: e.tensor_scalar_mul(out=G[:, ti, :], in0=lg[:], scalar1=ssum[:, 0:1]),
                     reads=[lg, ssum], pwrites=[G])
                pg = psX[(ti + 1) % 2]
                S.op('tensor', lambda e, pg=pg, ti=ti: e.transpose(out=pg[0:NE, 0:128], in_=G[:, ti, :], identity=identf[:]),
                     reads=[G, identf], writes=[pg])
                S.op('vector', lambda e, pg=pg: e.tensor_copy(out=GT[:], in_=pg[0:NE, 0:128]), reads=[pg], writes=[GT])
                for nh in range(2):
                    pb = psG[nh]
                    S.op('tensor', lambda e, pb=pb, nh=nh: e.matmul(pb[:], lhsT=GT[:], rhs=bdn[:, nh * 512:(nh + 1) * 512],
                                                                    start=True, stop=True), reads=[GT, bdn], writes=[pb])
                    S.op('scalar', lambda e, pb=pb, nh=nh, ti=ti: e.copy(out=acc[:, ti, nh * 512:(nh + 1) * 512], in_=pb[:]),
                         reads=[pb], pwrites=[accT[ti]])
            tblocks = [(t0, min(512, ntok - t0)) for t0 in range(0, ntok, 512)]
            for ex in range(NE):
                for kc in range(8):
                    S.dma('gpsimd', lambda e, ex=ex, kc=kc: e.dma_start(out=wgu[:, kc, :], in_=wguv[ex, :, kc, :]),
                          writes=[wguT[kc]])
                for kc in range(8):
                    S.dma('gpsimd', lambda e, ex=ex, kc=kc: e.dma_start(out=wdn[:, kc, :], in_=wdnv[ex, :, kc, :]),
                          writes=[wdnT[kc]])
                for (t0, nt) in tblocks:
                    A = actT[it % 2]; it += 1
                    for j in range(8):
                        pg = psG[j % 2]; pl = psL[j % 2]
                        for kc in range(8):
                            S.op('tensor', lambda e, pg=pg, kc=kc, j=j, t0=t0, nt=nt: e.matmul(
                                pg[:, :nt], lhsT=wgu[:, kc, j * 128:(j + 1) * 128], rhs=uT[:, kc, t0:t0 + nt],
                                start=(kc == 0), stop=(kc == 7)), reads=[wguT[kc], uT], writes=[pg])
                        for kc in range(8):
                            S.op('tensor', lambda e, pl=pl, kc=kc, j=j, t0=t0, nt=nt: e.matmul(
                                pl[:, :nt], lhsT=wgu[:, kc, DM + j * 128:DM + (j + 1) * 128], rhs=uT[:, kc, t0:t0 + nt],
                                start=(kc == 0), stop=(kc == 7)), reads=[wguT[kc], uT], writes=[pl])
                        g_ = gl[j % 2]; s_ = sg[j % 2]; l_ = l1[j % 2]
                        S.op('vector', lambda e, pg=pg, g_=g_, j=j, ex=ex, nt=nt: e.tensor_scalar(
                            out=g_[:, :nt], in0=pg[:, :nt], scalar1=bguT[:, j, ex:ex + 1], scalar2=7.0, op0=ALU.add, op1=ALU.min),
                            reads=[pg, bguT], writes=[g_])
                        S.op('scalar', lambda e, g_=g_, s_=s_, nt=nt: e.activation(out=s_[:, :nt], in_=g_[:, :nt], func=AF.Sigmoid,
                                                                                   scale=1.702), reads=[g_], writes=[s_])
                        S.op('scalar', lambda e, pl=pl, l_=l_, j=j, ex=ex, nt=nt: e.activation(
                            out=l_[:, :nt], in_=pl[:, :nt], func=AF.Identity, bias=bguT[:, 8 + j, ex:ex + 1]),
                            reads=[pl, bguT], writes=[l_])
                        S.op('vector', lambda e, l_=l_, nt=nt: e.tensor_scalar(out=l_[:, :nt], in0=l_[:, :nt], scalar1=-6.0, scalar2=8.0,
                                                                                op0=ALU.max, op1=ALU.min), reads=[l_], writes=[l_])
                        S.op('vector', lambda e, g_=g_, s_=s_, nt=nt: e.tensor_tensor(out=g_[:, :nt], in0=g_[:, :nt], in1=s_[:, :nt],
                                                                                      op=ALU.mult), reads=[g_, s_], writes=[g_])
                        S.op('vector', lambda e, A=A, g_=g_, l_=l_, j=j, nt=nt: e.tensor_tensor(out=A[:, j, :nt], in0=g_[:, :nt],
                                                                                                  in1=l_[:, :nt], op=ALU.mult),
                             reads=[g_, l_], pwrites=[A])
                    for tt in range(nt // 128):
                        ti = (t0 // 128) + tt
                        for nh in range(2):
                            py = psY[nh]
                            for j in range(8):
                                S.op('tensor', lambda e, py=py, A=A, j=j, tt=tt, nh=nh: e.matmul(
                                    py[:], lhsT=A[:, j, tt * 128:(tt + 1) * 128], rhs=wdn[:, j, nh * 512:(nh + 1) * 512],
                                    start=(j == 0), stop=(j == 7)), reads=[A, wdnT[j]], writes=[py])
                            S.op('vector', lambda e, py=py, ti=ti, nh=nh, ex=ex: e.scalar_tensor_tensor(
                                out=acc[:, ti, nh * 512:(nh + 1) * 512], in0=py[:], scalar=G[:, ti, ex:ex + 1],
                                in1=acc[:, ti, nh * 512:(nh + 1) * 512], op0=ALU.mult, op1=ALU.add),
                                reads=[py, G, accT[ti]], pwrites=[accT[ti]])
            for ti in range(gt):
                tg = g0 + ti
                mset = 1 if tg < nctx_tiles else 0
                xt = xt2[ti % 2]; o = xo[0]
                S.dma('sync', lambda e, xt=xt, tg=tg: e.dma_start(out=xt[:], in_=xin[tg * 128:(tg + 1) * 128, :]),
                      reads=[xinT], writes=[xt])
                S.op('gpsimd', lambda e, ti=ti, mset=mset: e.tensor_tensor(out=acc[:, ti, :], in0=acc[:, ti, :], in1=g2b[mset][:], op=ALU.mult),
                     reads=[accT[ti], g2b[mset]], writes=[accT[ti]])
                S.op('vector', lambda e, xt=xt, ti=ti: e.scalar_tensor_tensor(out=xt[:], in0=xt[:], scalar=ALPHA, in1=acc[:, ti, :],
                                                                             op0=ALU.mult, op1=ALU.add), reads=[xt, accT[ti]], writes=[xt])
                emit_ln(S, lnsc, xt, o, lngb, lnbb)
                S.dma('sync', lambda e, o=o, tg=tg: e.dma_start(out=xout[tg * 128:(tg + 1) * 128, :], in_=o[:]),
                      reads=[o], pwrites=[xoutT])
        S.barrier()


def build_moe(ntiles, nctx_tiles):
    nc = bass.Bass("TRN2", target_bir_lowering=False)
    EI = "ExternalInput"
    NTk = ntiles * 128
    xin = _dram(nc, "xin", [NTk, DM], F32, EI)
    mods_d = _dram(nc, "mods", [2, 6 * DM], F32, EI)
    router_w = _dram(nc, "router_w", [DM, 32], F32, EI)
    router_b = _dram(nc, "router_b", [1, 32], F32, EI)
    w_gu = _dram(nc, "w_gu", [32, DM, 2 * DM], F32, EI)
    b_gu = _dram(nc, "b_gu", [32, 2 * DM], F32, EI)
    w_down = _dram(nc, "w_down", [32, DM, DM], F32, EI)
    b_down = _dram(nc, "b_down", [32, DM], F32, EI)
    ln_g = _dram(nc, "ln_g", [1, DM], F32, EI)
    ln_b = _dram(nc, "ln_b", [1, DM], F32, EI)
    ident_d = _dram(nc, "ident", [128, 128], F32, EI)
    xout = _dram(nc, "xout", [NTk, DM], F32, "ExternalOutput")
    with ExitStack() as st0:
        S = Sched(nc, st0)
        emit_moe(S, nc, xin, T(xin), mods_d, T(mods_d), router_w, router_b, w_gu, b_gu, w_down, b_down, ln_g, ln_b,
                 ident_d, xout, T(xout), ntiles, nctx_tiles)
    return nc


def shared_moe(inp, i):
    return dict(router_w=inp['router_w'][i], router_b=inp['router_b'][i][None, :], w_gu=inp['moe_w_gu'][i],
                b_gu=inp['moe_b_gu'][i], w_down=inp['moe_w_down'][i], b_down=inp['moe_b_down'][i],
                ln_g=inp['ln_g'][i, 1][None, :], ln_b=inp['ln_b'][i, 1][None, :], ident=np.eye(128, dtype=np.float32))


NU = NQ // 128
GIN = 3104


class GlaScratch:
    def __init__(self, nc, kind="Internal", sfx=""):
        self.qT = _dram(nc, "g_qT" + sfx, [NU, 128, 4, 128], F32, kind)
        self.kT = _dram(nc, "g_kT" + sfx, [NU, 128, 4, 128], F32, kind)
        self.k = _dram(nc, "g_k" + sfx, [NQ, 512], F32, kind)
        self.v = _dram(nc, "g_v" + sfx, [NQ, DM], BF16, kind)
        self.LgA = _dram(nc, "g_LgA" + sfx, [NQ, 512], F32, kind)
        self.LgB = _dram(nc, "g_LgB" + sfx, [NQ, 512], F32, kind)
        self.r = _dram(nc, "g_r" + sfx, [NQ, DM], F32, kind)
        self.T = {n: T(getattr(self, n), n) for n in ('qT', 'kT', 'k', 'v', 'LgA', 'LgB', 'r')}


def emit_gla_proj(S, nc, xin, xinT, mods_d, modsT, w_in, wgA, wgB, ident_d, G):
    with ExitStack() as st:
        identb = S.sbuf("gp_identb", [128, 128], BF16, st)
        S.dma('gpsimd', lambda e: e.dma_start(out=identb[:], in_=ident_d[:, :]), writes=[identb])
        sc1b = [load_bcast(S, st, f"gp_sc1b{m}", mods_d[m:m + 1, DM:2 * DM], modsT) for m in range(2)]
        sh1b = [load_bcast(S, st, f"gp_sh1b{m}", mods_d[m:m + 1, 0:DM], modsT) for m in range(2)]
        for t in sc1b:
            S.op('gpsimd', lambda e, t=t: e.tensor_scalar_add(out=t[:], in0=t[:], scalar1=1.0), reads=[t], writes=[t])
        wi = S.sbuf("gp_wi", [128, 8, GIN], BF16, st)
        wiv = w_in.rearrange("(kc p) n -> p kc n", p=128)
        for kc in range(8):
            for c0, c1 in ((0, 1024), (1024, 2048), (2048, GIN)):
                S.dma('gpsimd', lambda e, kc=kc, c0=c0, c1=c1: e.dma_start(out=wi[:, kc, c0:c1], in_=wiv[:, kc, c0:c1]), pwrites=[wi])
        wg = []
        for nm, src in (("A", wgA), ("B", wgB)):
            t = S.sbuf("gp_wg" + nm, [17, 512], F32, st)
            S.dma('sync', lambda e, t=t, src=src: e.dma_start(out=t[:], in_=src[:, :]), writes=[t])
            wg.append(t)
        zaug = [S.sbuf(f"gp_zaug{i}", [32, 512], F32, st) for i in range(2)]
        for z in zaug:
            S.op('vector', lambda e, z=z: e.memset(z[:], 1.0), writes=[z])
        xts = [S.sbuf(f"gp_xt{i}", [128, DM], F32, st) for i in range(2)]
        tbf = [S.sbuf(f"gp_tbf{i}", [128, DM], BF16, st) for i in range(2)]
        tT = [S.sbuf(f"gp_tT{i}", [128, 8, 512], BF16, st) for i in range(2)]
        psXb = S.psum("gp_psXb", [128, 1024], BF16, st)
        psF = [S.psum(f"gp_psF{i}", [128, 512], F32, st) for i in range(2)]
        psZ = S.psum("gp_psZ", [128, 512], F32, st)
        psK = [S.psum(f"gp_psK{i}", [128, 512], F32, st) for i in range(3)]
        fst = [S.sbuf(f"gp_fst{i}", [128, 512], F32, st) for i in range(3)]
        kst = [S.sbuf(f"gp_kst{i}", [128, 512], F32, st) for i in range(2)]
        vst = [S.sbuf(f"gp_vst{i}", [128, DM], BF16, st) for i in range(2)]
        rst = [S.sbuf(f"gp_rst{i}", [128, DM], F32, st) for i in range(2)]
        gex = [S.sbuf(f"gp_gex{i}", [128, 512], F32, st) for i in range(2)]
        gst = [S.sbuf(f"gp_gst{i}", [128, 512], F32, st) for i in range(2)]
        blocks = [(0, NCTX)] + [(NCTX + i * 512, 512) for i in range(8)]
        iF = 0; iK = 0; ig = 0
        for bi, (t0, nt) in enumerate(blocks):
            TT = tT[bi % 2]
            mset = 1 if bi == 0 else 0
            ntl = nt // 128
            for ti in range(ntl):
                r0 = t0 + ti * 128
                xt = xts[ti % 2]; tb = tbf[ti % 2]
                S.dma('sync', lambda e, xt=xt, r0=r0: e.dma_start(out=xt[:], in_=xin[r0:r0 + 128, :]), reads=[xinT], writes=[xt])
                S.op('vector', lambda e, xt=xt, mset=mset: e.tensor_tensor(out=xt[:], in0=xt[:], in1=sc1b[mset][:], op=ALU.mult),
                     reads=[xt, sc1b[mset]], writes=[xt])
                S.op('gpsimd', lambda e, xt=xt, tb=tb, mset=mset: e.tensor_tensor(out=tb[:], in0=xt[:], in1=sh1b[mset][:], op=ALU.add),
                     reads=[xt, sh1b[mset]], writes=[tb])
                for kc in range(8):
                    S.op('tensor', lambda e, tb=tb, kc=kc: e.transpose(out=psXb[:, kc * 128:(kc + 1) * 128],
                                                                       in_=tb[:, kc * 128:(kc + 1) * 128], identity=identb[:]),
                         reads=[tb, identb], pwrites=[psXb])
                S.op('scalar', lambda e, TT=TT, ti=ti: e.copy(out=TT[:, :, ti * 128:(ti + 1) * 128],
                                                              in_=psXb[:].rearrange("p (k t) -> p k t", k=8)),
                     reads=[psXb], pwrites=[TT])
            n0 = t0 // 128
            for kind in ('q', 'k'):
                for h in range(4):
                    c0 = (0 if kind == 'q' else 512) + h * 128
                    pf = psF[iF % 2]; fs = fst[iF % 3]; iF += 1
                    for kc in range(8):
                        S.op('tensor', lambda e, pf=pf, TT=TT, kc=kc, c0=c0, nt=nt: e.matmul(
                            pf[:, :nt], lhsT=wi[:, kc, c0:c0 + 128], rhs=TT[:, kc, :nt], start=(kc == 0), stop=(kc == 7)),
                            reads=[wi, TT], writes=[pf])
                    S.op('scalar', lambda e, pf=pf, fs=fs, nt=nt, kind=kind: e.activation(
                        out=fs[:, :nt], in_=pf[:, :nt], func=AF.Copy, scale=(128.0 ** -0.5 if kind == 'q' else 1.0)),
                        reads=[pf], writes=[fs])
                    dst = G.qT if kind == 'q' else G.kT
                    S.dma('sync', lambda e, fs=fs, dst=dst, n0=n0, ntl=ntl, h=h, nt=nt: e.dma_start(
                        out=dst[n0:n0 + ntl, :, h, :].rearrange("n p t -> p n t"),
                        in_=fs[:, :nt].rearrange("p (n t) -> p n t", t=128)),
                        reads=[fs], pwrites=[G.T['qT' if kind == 'q' else 'kT']])
            for d in range(2):
                for kc in range(8):
                    S.op('tensor', lambda e, kc=kc, d=d, nt=nt: e.matmul(
                        psZ[0:16, :nt], lhsT=wi[:, kc, 3072 + d * 16:3072 + (d + 1) * 16], rhs=TT[:, kc, :nt],
                        start=(kc == 0), stop=(kc == 7)), reads=[wi, TT], writes=[psZ])
                S.op('vector', lambda e, d=d, nt=nt: e.tensor_copy(out=zaug[d][0:16, :nt], in_=psZ[0:16, :nt]),
                     reads=[psZ], pwrites=[zaug[d]])
            for ti in range(ntl):
                r0 = t0 + ti * 128
                tsl = slice(ti * 128, (ti + 1) * 128)
                pk = psK[iK % 3]; iK += 1
                ks = kst[ti % 2]
                for kc in range(8):
                    S.op('tensor', lambda e, pk=pk, kc=kc: e.matmul(pk[:], lhsT=TT[:, kc, tsl], rhs=wi[:, kc, 512:1024],
                                                                    start=(kc == 0), stop=(kc == 7)), reads=[wi, TT], writes=[pk])
                S.op('scalar', lambda e, pk=pk, ks=ks: e.copy(out=ks[:], in_=pk[:]), reads=[pk], writes=[ks])
                S.dma('sync', lambda e, ks=ks, r0=r0: e.dma_start(out=G.k[r0:r0 + 128, :], in_=ks[:]), reads=[ks], pwrites=[G.T['k']])
                vs_ = vst[ti % 2]; rs_ = rst[ti % 2]
                for which, c00 in (('v', 1024), ('r', 2048)):
                    if which == 'r' and bi == 0:
                        continue
                    for nh in range(2):
                        pk = psK[iK % 3]; iK += 1
                        for kc in range(8):
                            S.op('tensor', lambda e, pk=pk, kc=kc, c00=c00, nh=nh: e.matmul(
                                pk[:], lhsT=TT[:, kc, tsl], rhs=wi[:, kc, c00 + nh * 512:c00 + (nh + 1) * 512],
                                start=(kc == 0), stop=(kc == 7)), reads=[wi, TT], writes=[pk])
                        dstt = vs_ if which == 'v' else rs_
                        eng = 'vector' if which == 'v' else 'scalar'
                        if eng == 'vector':
                            S.op('vector', lambda e, pk=pk, dstt=dstt, nh=nh: e.tensor_copy(out=dstt[:, nh * 512:(nh + 1) * 512], in_=pk[:]),
                                 reads=[pk], pwrites=[dstt])
                        else:
                            S.op('scalar', lambda e, pk=pk, dstt=dstt, nh=nh: e.copy(out=dstt[:, nh * 512:(nh + 1) * 512], in_=pk[:]),
                                 reads=[pk], pwrites=[dstt])
                S.dma('sync', lambda e, vs_=vs_, r0=r0: e.dma_start(out=G.v[r0:r0 + 128, :], in_=vs_[:]), reads=[vs_], pwrites=[G.T['v']])
                if bi != 0:
                    S.dma('sync', lambda e, rs_=rs_, r0=r0: e.dma_start(out=G.r[r0:r0 + 128, :], in_=rs_[:]), reads=[rs_], pwrites=[G.T['r']])
                for d in range(2):
                    pk = psK[iK % 3]; iK += 1
                    ge = gex[ig % 2]; gs = gst[ig % 2]; ig += 1
                    S.op('tensor', lambda e, pk=pk, d=d: e.matmul(pk[:], lhsT=zaug[d][0:17, tsl], rhs=wg[d][0:17, :],
                                                                  start=True, stop=True), reads=[zaug[d], wg[d]], writes=[pk])
                    S.op('scalar', lambda e, pk=pk, ge=ge: e.activation(out=ge[:], in_=pk[:], func=AF.Exp, scale=-1.0),
                         reads=[pk], writes=[ge])
                    S.op('scalar', lambda e, ge=ge, gs=gs: e.activation(out=gs[:], in_=ge[:], func=AF.Ln, bias=1.0),
                         reads=[ge], writes=[gs])
                    dst = G.LgA if d == 0 else G.LgB
                    S.dma('sync', lambda e, gs=gs, dst=dst, r0=r0: e.dma_start(out=dst[r0:r0 + 128, :], in_=gs[:]),
                          reads=[gs], pwrites=[G.T['LgA' if d == 0 else 'LgB']])
        S.barrier()


def emit_gla_scan(S, nc, G, direction, cmats, S_init, S_final, oA, oAT, post=None):
    A = direction == 'A'
    Lg = G.LgA if A else G.LgB
    LgT = G.T['LgA' if A else 'LgB']
    col = 127 if A else 0
    with ExitStack() as st:
        cm = {}
        for nm in ('MinclT', 'MafterT', 'maskT'):
            t = S.sbuf("gs_" + nm, [128, 128], F32, st)
            S.dma('sync', lambda e, t=t, nm=nm: e.dma_start(out=t[:], in_=cmats[nm][:, :]), writes=[t])
            cm[nm] = t
        Sf = S.sbuf("gs_Sf", [128, 4, 256], F32, st)
        Sb = S.sbuf("gs_Sb", [128, 4, 256], BF16, st)
        SfT = [T(None) for _ in range(4)]; SbT = [T(None) for _ in range(4)]
        if S_init is None:
            S.op('vector', lambda e: e.memset(Sf[:], 0.0), writes=SfT)
        else:
            S.dma('sync', lambda e: e.dma_start(out=Sf[:], in_=S_init[:, :, :]), writes=SfT)
        for h in range(4):
            S.op('gpsimd', lambda e, h=h: e.tensor_copy(out=Sb[:, h, :], in_=Sf[:, h, :]), reads=[SfT[h]], writes=[SbT[h]])
        NB = 3
        qTu = [S.sbuf(f"gs_qT{i}", [128, 4, 128], F32, st) for i in range(NB)]
        kTu = [S.sbuf(f"gs_kT{i}", [128, 4, 128], F32, st) for i in range(NB)]
        ku = [S.sbuf(f"gs_k{i}", [128, 512], F32, st) for i in range(NB)]
        vu = [S.sbuf(f"gs_v{i}", [128, DM], BF16, st) for i in range(NB)]
        Lgu = [S.sbuf(f"gs_Lg{i}", [128, 512], F32, st) for i in range(NB)]
        bank = [S.psum(f"gs_bank{i}", [128, 512], F32, st) for i in range(5)]
        psB = [T(bank[0].t[:, h * 128:(h + 1) * 128]) for h in range(4)]
        psW = [T(bank[1].t[:, h * 128:(h + 1) * 128]) for h in range(4)]
        psA = [T(bank[2].t[:, h * 128:(h + 1) * 128]) for h in range(4)]
        psO = [T(bank[3].t[:, i * 256:(i + 1) * 256]) for i in range(2)]
        psD = [T(bank[4].t[:, i * 256:(i + 1) * 256]) for i in range(2)]
        Eq = [S.sbuf(f"gs_Eq{h}", [128, 128], F32, st) for h in range(4)]
        Ek = [S.sbuf(f"gs_Ek{h}", [128, 128], F32, st) for h in range(4)]
        Ew = [S.sbuf(f"gs_Ew{h}", [128, 128], F32, st) for h in range(4)]
        qin = [S.sbuf(f"gs_qin{h}", [128, 128], BF16, st) for h in range(4)]
        kin = [S.sbuf(f"gs_kin{h}", [128, 128], BF16, st) for h in range(4)]
        kst = [S.sbuf(f"gs_kst{h}", [128, 128], BF16, st) for h in range(4)]
        atm = [S.sbuf(f"gs_atm{h}", [128, 128], BF16, st) for h in range(4)]
        ou = [S.sbuf(f"gs_ou{i}", [128, DM], F32, st) for i in range(2)]
        if post is not None:
            P = post
            identb = S.sbuf("go_identb", [128, 128], BF16, st)
            S.dma('gpsimd', lambda e: e.dma_start(out=identb[:], in_=P['ident'][:, :]), writes=[identb])
            wo = S.sbuf("go_wo", [128, 8, DM], BF16, st)
            S.dma('gpsimd', lambda e: e.dma_start(out=wo[:], in_=P['w_out'].rearrange("(kc p) n -> p kc n", p=128)), writes=[wo])
            g1b = load_bcast(S, st, "go_g1b", P['mods_d'][0:1, 2 * DM:3 * DM], P['modsT'])
            lngb = load_bcast(S, st, "go_lngb", P['ln_g'])
            lnbb = load_bcast(S, st, "go_lnbb", P['ln_b'])
            nwb = S.sbuf("go_nwb", [128, 256], F32, st)
            S.dma('sync', lambda e: e.dma_start(out=nwb[:], in_=P['norm_w'].partition_broadcast(128)), writes=[nwb])
            oAu = [S.sbuf(f"go_oA{i}", [128, DM], F32, st) for i in range(NB)]
            ru = [S.sbuf(f"go_r{i}", [128, DM], F32, st) for i in range(NB)]
            xu = [S.sbuf(f"go_x{i}", [128, DM], F32, st) for i in range(NB)]
            sqt = S.sbuf("go_sq", [128, DM], F32, st)
            ms = S.sbuf("go_ms", [128, 4], F32, st)
            onb = S.sbuf("go_onb", [128, DM], BF16, st)
            onT = S.sbuf("go_onT", [128, 8, 128], BF16, st)
            psXb = S.psum("go_psXb", [128, 1024], BF16, st)
            psY = [S.psum(f"go_psY{i}", [128, 512], F32, st) for i in range(2)]
            xo = S.sbuf("go_xo", [128, DM], F32, st)
            lnsc = LNScratch(S, st, "go_lnc")
        units = list(range(NU)) if A else list(range(NU - 1, 1, -1))

        def load(i, n):
            r0 = n * 128
            S.dma('sync', lambda e: e.dma_start(out=kTu[i][:], in_=G.kT[n]), reads=[G.T['kT']], writes=[kTu[i]])
            S.dma('sync', lambda e: e.dma_start(out=ku[i][:], in_=G.k[r0:r0 + 128, :]), reads=[G.T['k']], writes=[ku[i]])
            S.dma('sync', lambda e: e.dma_start(out=vu[i][:], in_=G.v[r0:r0 + 128, :]), reads=[G.T['v']], writes=[vu[i]])
            S.dma('sync', lambda e: e.dma_start(out=Lgu[i][:], in_=Lg[r0:r0 + 128, :]), reads=[LgT], writes=[Lgu[i]])
            if n >= 2:
                S.dma('sync', lambda e: e.dma_start(out=qTu[i][:], in_=G.qT[n]), reads=[G.T['qT']], writes=[qTu[i]])
                if post is not None:
                    j = i
                    S.dma('sync', lambda e: e.dma_start(out=oAu[j][:], in_=oA[r0 - NCTX:r0 - NCTX + 128, :]), reads=[oAT], writes=[oAu[j]])
                    S.dma('sync', lambda e: e.dma_start(out=ru[j][:], in_=G.r[r0:r0 + 128, :]), reads=[G.T['r']], writes=[ru[j]])
                    S.dma('sync', lambda e: e.dma_start(out=xu[j][:], in_=P['xin'][r0:r0 + 128, :]), reads=[P['xinT']], writes=[xu[j]])

        load(0, units[0])
        for ui, n in enumerate(units):
            i = ui % NB
            if ui + 1 < len(units):
                load((ui + 1) % NB, units[ui + 1])
            full = n >= 2
            O = ou[ui % 2]
            for h in range(4):
                hs = slice(h * 128, (h + 1) * 128)
                S.op('tensor', lambda e: e.matmul(psB[h][:], lhsT=Lgu[i][:, hs], rhs=cm['MinclT'][:], start=True, stop=True),
                     reads=[Lgu[i], cm['MinclT']], writes=[psB[h]])
                S.op('tensor', lambda e: e.matmul(psW[h][:], lhsT=cm['MafterT'][:], rhs=Lgu[i][:, hs], start=True, stop=True),
                     reads=[Lgu[i], cm['MafterT']], writes=[psW[h]])
                S.op('scalar', lambda e: e.activation(out=Eq[h][:], in_=psB[h][:], func=AF.Exp), reads=[psB[h]], writes=[Eq[h]])
                S.op('scalar', lambda e: e.activation(out=Ew[h][:], in_=psW[h][:], func=AF.Exp), reads=[psW[h]], writes=[Ew[h]])
                S.op('gpsimd', lambda e: e.tensor_tensor(out=kst[h][:], in0=ku[i][:, hs], in1=Ew[h][:], op=ALU.mult),
                     reads=[ku[i], Ew[h]], writes=[kst[h]])
                if full:
                    S.op('scalar', lambda e: e.activation(out=Ek[h][:], in_=psB[h][:], func=AF.Exp, scale=-1.0),
                         reads=[psB[h]], writes=[Ek[h]])
                    S.op('vector', lambda e: e.tensor_tensor(out=qin[h][:], in0=qTu[i][:, h, :], in1=Eq[h][:], op=ALU.mult),
                         reads=[qTu[i], Eq[h]], writes=[qin[h]])
                    S.op('gpsimd', lambda e: e.tensor_tensor(out=kin[h][:], in0=kTu[i][:, h, :], in1=Ek[h][:], op=ALU.mult),
                         reads=[kTu[i], Ek[h]], writes=[kin[h]])
                    S.op('tensor', lambda e: e.matmul(psA[h][:], lhsT=kin[h][:], rhs=qin[h][:], start=True, stop=True),
                         reads=[kin[h], qin[h]], writes=[psA[h]])
                    S.op('vector', lambda e: e.tensor_tensor(out=atm[h][:], in0=psA[h][:], in1=cm['maskT'][:], op=ALU.mult),
                         reads=[psA[h], cm['maskT']], writes=[atm[h]])
                    po = psO[h % 2]
                    S.op('tensor', lambda e: e.matmul(po[:], lhsT=atm[h][:], rhs=vu[i][:, h * 256:(h + 1) * 256], start=True, stop=False),
                         reads=[atm[h], vu[i]], writes=[po])
                    S.op('tensor', lambda e: e.matmul(po[:], lhsT=qin[h][:], rhs=Sb[:, h, :], start=False, stop=True),
                         reads=[qin[h], SbT[h]], writes=[po])
                    if post is None:
                        S.op('scalar', lambda e: e.copy(out=O[:, h * 256:(h + 1) * 256], in_=po[:]), reads=[po], pwrites=[O])
                    else:
                        S.op('vector', lambda e: e.tensor_tensor(out=O[:, h * 256:(h + 1) * 256], in0=po[:],
                                                                 in1=oAu[i][:, h * 256:(h + 1) * 256], op=ALU.add),
                             reads=[po, oAu[i]], pwrites=[O])
                pd = psD[h % 2]
                S.op('tensor', lambda e: e.matmul(pd[:], lhsT=kst[h][:], rhs=vu[i][:, h * 256:(h + 1) * 256], start=True, stop=True),
                     reads=[kst[h], vu[i]], writes=[pd])
                S.op('vector', lambda e: e.scalar_tensor_tensor(out=Sf[:, h, :], in0=Sf[:, h, :], scalar=Eq[h][:, col:col + 1],
                                                                in1=pd[:], op0=ALU.mult, op1=ALU.add),
                     reads=[SfT[h], Eq[h], pd], writes=[SfT[h]])
                S.op('gpsimd', lambda e: e.tensor_copy(out=Sb[:, h, :], in_=Sf[:, h, :]), reads=[SfT[h]], writes=[SbT[h]])
            if not full:
                continue
            r0 = n * 128
            if post is None:
                S.dma('sync', lambda e: e.dma_start(out=oA[r0 - NCTX:r0 - NCTX + 128, :], in_=O[:]), reads=[O], pwrites=[oAT])
                continue
            j = i
            S.op('gpsimd', lambda e: e.tensor_tensor(out=sqt[:], in0=O[:], in1=O[:], op=ALU.mult), reads=[O], writes=[sqt])
            S.op('vector', lambda e: e.reduce_sum(out=ms[:], in_=sqt[:].rearrange("p (h d) -> p h d", h=4), axis=AX.X),
                 reads=[sqt], writes=[ms])
            S.op('vector', lambda e: e.tensor_scalar(out=ms[:], in0=ms[:], scalar1=1.0 / 256, scalar2=EPS, op0=ALU.mult, op1=ALU.add),
                 reads=[ms], writes=[ms])
            S.op('scalar', lambda e: e.activation(out=ms[:], in_=ms[:], func=AF.Sqrt), reads=[ms], writes=[ms])
            S.op('vector', lambda e: e.reciprocal(out=ms[:], in_=ms[:]), reads=[ms], writes=[ms])
            for h in range(4):
                S.op('vector', lambda e: e.scalar_tensor_tensor(out=O[:, h * 256:(h + 1) * 256], in0=O[:, h * 256:(h + 1) * 256],
                                                                scalar=ms[:, h:h + 1], in1=nwb[:], op0=ALU.mult, op1=ALU.mult),
                     reads=[O, ms, nwb], writes=[O])
            S.op('scalar', lambda e: e.activation(out=sqt[:], in_=ru[j][:], func=AF.Silu), reads=[ru[j]], writes=[sqt])
            S.op('gpsimd', lambda e: e.tensor_tensor(out=onb[:], in0=O[:], in1=sqt[:], op=ALU.mult), reads=[O, sqt], writes=[onb])
            for kc in range(8):
                S.op('tensor', lambda e: e.transpose(out=psXb[:, kc * 128:(kc + 1) * 128], in_=onb[:, kc * 128:(kc + 1) * 128],
                                                     identity=identb[:]), reads=[onb, identb], pwrites=[psXb])
            S.op('scalar', lambda e: e.copy(out=onT[:], in_=psXb[:].rearrange("p (k t) -> p k t", k=8)), reads=[psXb], writes=[onT])
            for nh in range(2):
                py = psY[nh]
                for kc in range(8):
                    S.op('tensor', lambda e: e.matmul(py[:], lhsT=onT[:, kc, :], rhs=wo[:, kc, nh * 512:(nh + 1) * 512],
                                                      start=(kc == 0), stop=(kc == 7)), reads=[onT, wo], writes=[py])
                S.op('vector', lambda e: e.tensor_tensor(out=sqt[:, nh * 512:(nh + 1) * 512], in0=py[:],
                                                         in1=g1b[:, nh * 512:(nh + 1) * 512], op=ALU.mult),
                     reads=[py, g1b], pwrites=[sqt])
            S.op('vector', lambda e: e.scalar_tensor_tensor(out=sqt[:], in0=xu[j][:], scalar=ALPHA, in1=sqt[:],
                                                            op0=ALU.mult, op1=ALU.add), reads=[xu[j], sqt], writes=[sqt])
            emit_ln(S, lnsc, sqt, xo, lngb, lnbb)
            S.dma('sync', lambda e: e.dma_start(out=P['xout'][r0 - NCTX:r0 - NCTX + 128, :], in_=xo[:]),
                  reads=[xo], pwrites=[P['xoutT']])
        if S_final is not None:
            S.dma('sync', lambda e: e.dma_start(out=S_final[:, :, :], in_=Sf[:]), reads=SfT)
        S.barrier()


def _gla_common_inputs(nc):
    EI = "ExternalInput"
    d = dict(
        xin=_dram(nc, "xin", [NQ, DM], F32, EI),
        w_in=_dram(nc, "w_in", [DM, GIN], F32, EI),
        wgA=_dram(nc, "wgA", [17, 512], F32, EI),
        wgB=_dram(nc, "wgB", [17, 512], F32, EI),
        ident=_dram(nc, "ident", [128, 128], F32, EI),
    )
    return d


def _cmats(nc, sfx):
    return {nm: _dram(nc, nm + sfx, [128, 128], F32, "ExternalInput") for nm in ('MinclT', 'MafterT', 'maskT')}


def build_gla1():
    nc = bass.Bass("TRN2", target_bir_lowering=False)
    EI = "ExternalInput"
    I = _gla_common_inputs(nc)
    cT = _dram(nc, "cT", [DM, 2], F32, EI)
    ada_w = _dram(nc, "ada_w", [DM, 6 * DM], F32, EI)
    ada_b = _dram(nc, "ada_b", [1, 6 * DM], F32, EI)
    cmA = _cmats(nc, "A")
    mods_d = _dram(nc, "mods", [2, 6 * DM], F32, "ExternalOutput")
    oA = _dram(nc, "oA", [NOWN, DM], F32, "ExternalOutput")
    SA = _dram(nc, "SA", [128, 4, 256], F32, "ExternalOutput")
    G = GlaScratch(nc)
    with ExitStack() as st0:
        S = Sched(nc, st0)
        modsT = T(mods_d)
        with ExitStack() as st:
            emit_mods(S, nc, st, cT, ada_w, ada_b, mods_d, modsT)
            S.barrier()
        xinT = T(I['xin'])
        emit_gla_proj(S, nc, I['xin'], xinT, mods_d, modsT, I['w_in'], I['wgA'], I['wgB'], I['ident'], G)
        emit_gla_scan(S, nc, G, 'A', cmA, None, SA, oA, T(oA))
    return nc


def build_gla2():
    nc = bass.Bass("TRN2", target_bir_lowering=False)
    EI = "ExternalInput"
    I = _gla_common_inputs(nc)
    mods_d = _dram(nc, "mods", [2, 6 * DM], F32, EI)
    cmB = _cmats(nc, "B")
    oA = _dram(nc, "oA", [NOWN, DM], F32, EI)
    SB0 = _dram(nc, "SB0", [128, 4, 256], F32, EI)
    w_out = _dram(nc, "w_out", [DM, DM], F32, EI)
    norm_w = _dram(nc, "norm_w", [1, 256], F32, EI)
    ln_g = _dram(nc, "ln_g", [1, DM], F32, EI)
    ln_b = _dram(nc, "ln_b", [1, DM], F32, EI)
    xout = _dram(nc, "xout", [NOWN, DM], F32, "ExternalOutput")
    G = GlaScratch(nc)
    with ExitStack() as st0:
        S = Sched(nc, st0)
        modsT = T(mods_d); xinT = T(I['xin'])
        emit_gla_proj(S, nc, I['xin'], xinT, mods_d, modsT, I['w_in'], I['wgA'], I['wgB'], I['ident'], G)
        post = dict(ident=I['ident'], w_out=w_out, mods_d=mods_d, modsT=modsT, ln_g=ln_g, ln_b=ln_b, norm_w=norm_w,
                    xin=I['xin'], xinT=xinT, xout=xout, xoutT=T(xout))
        emit_gla_scan(S, nc, G, 'B', cmB, SB0, None, oA, T(oA), post=post)
    return nc


def _gla_cmats():
    s = np.arange(128)[:, None]; t = np.arange(128)[None, :]
    c = np.float32(-1.0 / 16.0)
    return dict(
        MinclTA=(s <= t) * c, MafterTA=(s > t) * c, maskTA=(s <= t) * np.float32(1),
        MinclTB=(s >= t) * c, MafterTB=(s < t) * c, maskTB=(s >= t) * np.float32(1),
    )


def shared_gla(inp):
    cm = {k: np.ascontiguousarray(v.astype(np.float32)) for k, v in _gla_cmats().items()}
    w = inp['gla_w_in'][0]
    wsw = np.ascontiguousarray(np.concatenate([w[:, :3072], w[:, 3088:3104], w[:, 3072:3088]], 1))
    wg = inp['gla_w_gate'][0]; bg = inp['gla_b_gate'][0]
    aug = [np.ascontiguousarray(np.concatenate([wg[d], bg[d][None, :]], 0)) for d in range(2)]
    return dict(cm=cm, w_in=[w, wsw], aug=aug, ada_w=inp['ada_w'][1], ada_b=inp['ada_b'][1][None, :],
                w_out=inp['gla_w_out'][0], norm_w=inp['gla_norm_w'][0][None, :],
                ln_g=inp['ln_g'][1, 0][None, :], ln_b=inp['ln_b'][1, 0][None, :], ident=np.eye(128, dtype=np.float32))


def prep_gla_common(sh, hf, xin):
    return dict(xin=xin, w_in=sh['w_in'][hf], wgA=sh['aug'][hf], wgB=sh['aug'][1 - hf], ident=sh['ident'])


def _moe_inputs(nc, p):
    EI = "ExternalInput"
    return dict(router_w=_dram(nc, p + "router_w", [DM, 32], F32, EI), router_b=_dram(nc, p + "router_b", [1, 32], F32, EI),
                w_gu=_dram(nc, p + "w_gu", [32, DM, 2 * DM], F32, EI), b_gu=_dram(nc, p + "b_gu", [32, 2 * DM], F32, EI),
                w_down=_dram(nc, p + "w_down", [32, DM, DM], F32, EI), b_down=_dram(nc, p + "b_down", [32, DM], F32, EI),
                ln_g=_dram(nc, p + "ln_g", [1, DM], F32, EI), ln_b=_dram(nc, p + "ln_b", [1, DM], F32, EI))


def build_fused():
    nc = bass.Bass("TRN2", target_bir_lowering=False)
    EI = "ExternalInput"
    A = attn_inputs(nc)
    M0 = _moe_inputs(nc, "m0_"); M1 = _moe_inputs(nc, "m1_")
    g_w_in = _dram(nc, "g_w_in", [DM, GIN], F32, EI)
    wgA = _dram(nc, "wgA", [17, 512], F32, EI); wgB = _dram(nc, "wgB", [17, 512], F32, EI)
    ada_w1 = _dram(nc, "ada_w1", [DM, 6 * DM], F32, EI); ada_b1 = _dram(nc, "ada_b1", [1, 6 * DM], F32, EI)
    cmA = _cmats(nc, "A"); cmB = _cmats(nc, "B")
    g_w_out = _dram(nc, "g_w_out", [DM, DM], F32, EI)
    norm_w = _dram(nc, "norm_w", [1, 256], F32, EI)
    g_ln_g = _dram(nc, "g_ln_g", [1, DM], F32, EI); g_ln_b = _dram(nc, "g_ln_b", [1, DM], F32, EI)
    psel = _dram(nc, "psel", [128, 8], F32, EI)
    out = _dram(nc, "out", [NOWN, DM], F32, "ExternalOutput")
    x1 = _dram(nc, "x1", [NQ, DM], F32); mods0 = _dram(nc, "mods0", [2, 6 * DM], F32)
    x2 = _dram(nc, "x2", [NQ, DM], F32); mods1 = _dram(nc, "mods1", [2, 6 * DM], F32)
    oA = _dram(nc, "oA", [NOWN, DM], F32); SA = _dram(nc, "SA", [128, DM], F32)
    SG = _dram(nc, "SG", [8 * 128, DM], F32); SB0 = _dram(nc, "SB0", [128, DM], F32)
    x3 = _dram(nc, "x3", [NOWN, DM], F32)
    with ExitStack() as st0:
        S = Sched(nc, st0)
        x1T = T(x1); m0T = T(mods0); x2T = T(x2); m1T = T(mods1); oAT = T(oA); x3T = T(x3)
        emit_attn(S, nc, A, _lambda_init(0), x1, x1T, mods0, m0T)
        emit_moe(S, nc, x1, x1T, mods0, m0T, M0['router_w'], M0['router_b'], M0['w_gu'], M0['b_gu'], M0['w_down'],
                 M0['b_down'], M0['ln_g'], M0['ln_b'], A['ident'], x2, x2T, NQ // 128, NCTX // 128)
        with ExitStack() as st:
            emit_mods(S, nc, st, A['cT'], ada_w1, ada_b1, mods1, m1T)
            S.barrier()
        G = GlaScratch(nc)
        emit_gla_proj(S, nc, x2, x2T, mods1, m1T, g_w_in, wgA, wgB, A['ident'], G)
        emit_gla_scan(S, nc, G, 'A', cmA, None, SA.rearrange("p (h d) -> p h d", h=4), oA, oAT)
        SGT = T(SG)
        S.special('gpsimd', lambda e: e.collective_compute("AllGather", ALU.bypass, replica_groups=[list(range(8))],
                                                           ins=[SA.opt()], outs=[SG.opt()]), writes=[SGT])
        with ExitStack() as st:
            ps_ = S.sbuf("ex_psel", [128, 8], F32, st)
            S.dma('sync', lambda e: e.dma_start(out=ps_[:], in_=psel[:, :]), writes=[ps_])
            gt = [S.sbuf(f"ex_g{i}", [128, DM], F32, st) for i in range(2)]
            acc = S.sbuf("ex_acc", [128, DM], F32, st)
            for r in range(8):
                g = gt[r % 2]
                S.dma('sync', lambda e: e.dma_start(out=g[:], in_=SG[r * 128:(r + 1) * 128, :]), reads=[SGT], writes=[g])
                if r == 0:
                    S.op('vector', lambda e: e.tensor_scalar_mul(out=acc[:], in0=g[:], scalar1=ps_[:, 0:1]),
                         reads=[g, ps_], writes=[acc])
                else:
                    S.op('vector', lambda e: e.scalar_tensor_tensor(out=acc[:], in0=g[:], scalar=ps_[:, r:r + 1], in1=acc[:],
                                                                    op0=ALU.mult, op1=ALU.add), reads=[g, ps_, acc], writes=[acc])
            S.dma('sync', lambda e: e.dma_start(out=SB0[:, :], in_=acc[:]), reads=[acc])
            S.barrier()
        post = dict(ident=A['ident'], w_out=g_w_out, mods_d=mods1, modsT=m1T, ln_g=g_ln_g, ln_b=g_ln_b, norm_w=norm_w,
                    xin=x2, xinT=x2T, xout=x3, xoutT=x3T)
        emit_gla_scan(S, nc, G, 'B', cmB, SB0.rearrange("p (h d) -> p h d", h=4), None, oA, oAT, post=post)
        emit_moe(S, nc, x3, x3T, mods1, m1T, M1['router_w'], M1['router_b'], M1['w_gu'], M1['b_gu'], M1['w_down'],
                 M1['b_down'], M1['ln_g'], M1['ln_b'], A['ident'], out, T(out), NOWN // 128, 0)
    return nc


def kernel(**inp):
    inp = {k: np.asarray(v) for k, v in inp.items()}
    cores = list(range(8))
    sh = shared_attn(inp)
    shg = shared_gla(inp)
    shared = dict(sh)
    for i, p in ((0, "m0_"), (1, "m1_")):
        sm = shared_moe(inp, i)
        for k in ('router_w', 'router_b', 'w_gu', 'b_gu', 'w_down', 'b_down', 'ln_g', 'ln_b'):
            shared[p + k] = sm[k]
    shared.update(ada_w1=shg['ada_w'], ada_b1=shg['ada_b'], g_w_out=shg['w_out'], norm_w=shg['norm_w'],
                  g_ln_g=shg['ln_g'], g_ln_b=shg['ln_b'])
    for k, v in shg['cm'].items():
        shared[k] = v
    maps = []
    for c in cores:
        b, hf = divmod(c, 2)
        d = prep_attn(inp, c, shared)
        sel = np.zeros((128, 8), np.float32); sel[:, c ^ 1] = 1.0
        d.update(g_w_in=shg['w_in'][hf], wgA=shg['aug'][hf], wgB=shg['aug'][1 - hf], psel=sel)
        maps.append(d)
    if 'fused' not in _NC_CACHE:
        _NC_CACHE['fused'] = build_fused()
    r = run_bass_kernel_spmd(_NC_CACHE['fused'], maps, core_ids=cores).results
    out = np.empty((4, 2 * NOWN, DM), np.float32)
    for c in cores:
        b, hf = divmod(c, 2)
        xo = r[c]['out']
        out[b, hf * NOWN:(hf + 1) * NOWN] = xo[::-1] if hf == 1 else xo
    return out


_NC_CACHE = {}
```

```python
import numpy as np
from contextlib import ExitStack
import concourse.bass as bass
import concourse.mybir as mybir
from concourse.bass_utils import run_bass_kernel_spmd

F32 = mybir.dt.float32
BF16 = mybir.dt.bfloat16
AF = mybir.ActivationFunctionType
ALU = mybir.AluOpType
AX = mybir.AxisListType

NCTX = 256
NOWN = 4096
NQ = NCTX + NOWN
NK = NQ + NOWN
DM = 1024
ALPHA = (2.0 * 2) ** 0.25
EPS = 1e-5


class T:
    __slots__ = ('t', 'w', 'wf', 'r', 'name')

    def __init__(self, t, name=''):
        self.t = t; self.w = []; self.wf = None; self.r = []; self.name = name

    def __getitem__(self, k):
        return self.t[k]


class Sched:
    ENG = ('tensor', 'vector', 'scalar', 'gpsimd', 'sync')
    EPOCH = 16000
    KDMA = 8

    def __init__(self, nc, stack):
        self.nc = nc; self.stack = stack
        self.cnt = {e: 0 for e in self.ENG}
        self.sems = {e: [] for e in self.ENG}
        self.dcnt = {e: 0 for e in self.ENG}
        self.dsems = {e: [] for e in self.ENG}
        self.waited = {e: {} for e in self.ENG}
        self.nwaits = 0

    def _sem(self, name):
        return self.stack.enter_context(self.nc.semaphore(name))

    def _uname(self, name):
        self.uid = getattr(self, 'uid', 0) + 1
        return f"{name}_{self.uid}"

    def sbuf(self, name, shape, dt, stack=None):
        st = stack or self.stack
        return T(st.enter_context(self.nc.sbuf_tensor(self._uname(name), list(shape), dt)), name)

    def psum(self, name, shape, dt=F32, stack=None):
        st = stack or self.stack
        return T(st.enter_context(self.nc.psum_tensor(self._uname(name), list(shape), dt)), name)

    def special(self, eng, fn, reads=(), writes=()):
        deps = self._deps(eng, reads, writes, ())
        sem = self._sem(self._uname('x_' + eng))
        tok = ('x', sem, 1)
        e = self._emit_waits(eng, deps)
        fn(e).then_inc(sem)
        self._commit(tok, reads, writes, ())
        return tok

    def _need(self, issuer, tok, same_ok=False):
        if tok is None:
            return None
        if tok[0] == 'c':
            _, e, ep, n = tok
            if e == issuer and same_ok:
                return None
            key = ('c', e)
            if (ep, n) <= self.waited[issuer].get(key, (-1, 0)):
                return None
            self.waited[issuer][key] = (ep, n)
            return tok
        if tok[0] == 'x':
            key = ('x', id(tok[1]))
            if self.waited[issuer].get(key, 0) >= tok[2]:
                return None
            self.waited[issuer][key] = tok[2]
            return tok
        _, e, slot, val = tok
        key = ('d', e, slot)
        if self.waited[issuer].get(key, 0) >= val:
            return None
        self.waited[issuer][key] = val
        return tok

    def _deps(self, issuer, reads, writes, pwrites):
        out = []

        def add(tk, same_ok):
            k = self._need(issuer, tk, same_ok)
            if k:
                out.append(k)
        for r in reads:
            for tk in r.w:
                add(tk, False)
        for w in writes:
            for tk in w.w:
                add(tk, True)
            for tk in w.r:
                add(tk, True)
        for w in pwrites:
            add(w.wf, True)
            for tk in w.r:
                add(tk, True)
        return out

    @staticmethod
    def _push(lst, tok):
        if tok[0] == 'c':
            lst[:] = [x for x in lst if not (x[0] == 'c' and x[1] == tok[1])]
        lst.append(tok)

    def _commit(self, tok, reads, writes, pwrites):
        for r in reads:
            self._push(r.r, tok)
        for w in writes:
            w.w = [tok]; w.wf = tok; w.r = []
        for w in pwrites:
            self._push(w.w, tok); w.r = []

    def semof(self, tok):
        if tok[0] == 'c':
            return self.sems[tok[1]][tok[2]], tok[3]
        if tok[0] == 'x':
            return tok[1], tok[2]
        return self.dsems[tok[1]][tok[2]], tok[3]

    def _emit_waits(self, eng, deps):
        e = getattr(self.nc, eng)
        for d in deps:
            s, v = self.semof(d)
            e.wait_ge(s, v)
            self.nwaits += 1
        return e

    def op(self, eng, fn, reads=(), writes=(), pwrites=()):
        deps = self._deps(eng, reads, writes, pwrites)
        n = self.cnt[eng]; ep, k = divmod(n, self.EPOCH)
        if k == 0:
            self.sems[eng].append(self._sem(f's_{eng}_{ep}'))
        self.cnt[eng] = n + 1
        tok = ('c', eng, ep, k + 1)
        e = self._emit_waits(eng, deps)
        fn(e).then_inc(self.sems[eng][ep], 1)
        self._commit(tok, reads, writes, pwrites)
        return tok

    def dma(self, eng, fn, reads=(), writes=(), pwrites=()):
        deps = self._deps(eng, reads, writes, pwrites)
        i = self.dcnt[eng]; self.dcnt[eng] = i + 1
        rnd, slot = divmod(i, self.KDMA)
        if rnd == 0:
            self.dsems[eng].append(self._sem(f'd_{eng}_{slot}'))
        else:
            k = self._need(eng, ('d', eng, slot, 16 * rnd))
            if k:
                deps.append(k)
        tok = ('d', eng, slot, 16 * (rnd + 1))
        e = self._emit_waits(eng, deps)
        fn(e).then_inc(self.dsems[eng][slot], 16)
        self._commit(tok, reads, writes, pwrites)
        return tok

    def all_tokens(self):
        toks = []
        for e in self.ENG:
            n = self.cnt[e]
            if n:
                ep, k = divmod(n - 1, self.EPOCH)
                toks.append(('c', e, ep, k + 1))
            for i in range(max(0, self.dcnt[e] - self.KDMA), self.dcnt[e]):
                rnd, slot = divmod(i, self.KDMA)
                toks.append(('d', e, slot, 16 * (rnd + 1)))
        return toks

    def barrier(self, engines=None):
        toks = self.all_tokens()
        for e in (engines or self.ENG):
            deps = [k for k in (self._need(e, t, same_ok=True) for t in toks) if k]
            self._emit_waits(e, deps)


def _dram(nc, name, shape, dt, kind="Internal"):
    return nc.dram_tensor(name, list(shape), dt, kind=kind).ap()


class RR:
    def __init__(self, items):
        self.items = items; self.i = 0

    def __call__(self):
        x = self.items[self.i % len(self.items)]; self.i += 1
        return x


def emit_mods(S, nc, st, cT, ada_w, ada_b, mods_d, modsT):
    cs = S.sbuf("m_cs", [128, 8, 2], F32, st)
    S.dma('sync', lambda e: e.dma_start(out=cs[:], in_=cT.rearrange("(kc p) m -> p kc m", p=128)),
          writes=[cs])
    S.op('scalar', lambda e: e.activation(out=cs[:], in_=cs[:], func=AF.Silu), reads=[cs], writes=[cs])
    ab = S.sbuf("m_ab", [2, 6144], F32, st)
    S.dma('sync', lambda e: e.dma_start(out=ab[:], in_=ada_b.partition_broadcast(2)), writes=[ab])
    msb = S.sbuf("m_sb", [2, 6144], F32, st)
    aw = [S.sbuf(f"m_aw{i}", [128, 8, 512], F32, st) for i in range(2)]
    ps = [S.psum(f"m_ps{i}", [2, 512], F32, st) for i in range(2)]
    awv = ada_w.rearrange("(kc p) n -> p kc n", p=128)
    for nb in range(12):
        a = aw[nb % 2]; p = ps[nb % 2]
        S.dma('sync', lambda e, a=a, nb=nb: e.dma_start(out=a[:], in_=awv[:, :, nb * 512:(nb + 1) * 512]),
              writes=[a])
        for kc in range(8):
            S.op('tensor', lambda e, a=a, p=p, kc=kc: e.matmul(p[:], lhsT=cs[:, kc, :], rhs=a[:, kc, :],
                                                                start=(kc == 0), stop=(kc == 7)),
                 reads=[cs, a], writes=[p])
        S.op('vector', lambda e, p=p, nb=nb: e.tensor_tensor(out=msb[:, nb * 512:(nb + 1) * 512], in0=p[:],
                                                             in1=ab[:, nb * 512:(nb + 1) * 512], op=ALU.add),
             reads=[p, ab], pwrites=[msb])
    S.dma('sync', lambda e: e.dma_start(out=mods_d[:, :], in_=msb[:]), reads=[msb], writes=[modsT])


def load_bcast(S, st, name, src_row, modsT=None, eng='sync'):
    t = S.sbuf(name, [128, DM], F32, st)
    S.dma(eng, lambda e: e.dma_start(out=t[:], in_=src_row.partition_broadcast(128)),
          reads=([modsT] if modsT is not None else []), writes=[t])
    return t


def attn_inputs(nc):
    EI = "ExternalInput"
    return dict(
        xk=_dram(nc, "xk", [DM, NK], F32, EI), xtm=_dram(nc, "xtm", [NQ, DM], F32, EI),
        ropeC=_dram(nc, "ropeC", [128, NK], F32, EI), ropeS=_dram(nc, "ropeS", [128, NK], F32, EI),
        cT=_dram(nc, "cT", [DM, 2], F32, EI),
        ada_w=_dram(nc, "ada_w", [DM, 6 * DM], F32, EI), ada_b=_dram(nc, "ada_b", [1, 6 * DM], F32, EI),
        w_in=_dram(nc, "w_in", [DM, 3 * DM], F32, EI), w_perm=_dram(nc, "w_perm", [DM, 2 * DM], F32, EI),
        w_out=_dram(nc, "w_out", [DM, DM], F32, EI), lamv=_dram(nc, "lamv", [1, 256], F32, EI),
        subln=_dram(nc, "subln", [1, 128], F32, EI), ln_g=_dram(nc, "ln_g", [1, DM], F32, EI),
        ln_b=_dram(nc, "ln_b", [1, DM], F32, EI), ident=_dram(nc, "ident", [128, 128], F32, EI))


def build_attn(lam_init):
    nc = bass.Bass("TRN2", target_bir_lowering=False)
    I = attn_inputs(nc)
    x1 = _dram(nc, "x1", [NQ, DM], F32, "ExternalOutput")
    mods_d = _dram(nc, "mods", [2, 6 * DM], F32, "ExternalOutput")
    with ExitStack() as st0:
        S = Sched(nc, st0)
        emit_attn(S, nc, I, lam_init, x1, T(x1, "x1"), mods_d, T(mods_d, "mods"))
    return nc


def emit_attn(S, nc, I, lam_init, x1, x1T, mods_d, modsT):
    xk, xtm, ropeC, ropeS, cT = I['xk'], I['xtm'], I['ropeC'], I['ropeS'], I['cT']
    ada_w, ada_b, w_in, w_perm, w_out = I['ada_w'], I['ada_b'], I['w_in'], I['w_perm'], I['w_out']
    lamv, subln, ln_g, ln_b, ident_d = I['lamv'], I['subln'], I['ln_g'], I['ln_b'], I['ident']
    QT = _dram(nc, "QT", [8, 128, NQ], BF16)
    KT = _dram(nc, "KT", [8, 128, NK], BF16)
    Vd = _dram(nc, "Vd", [8, NK, 129], BF16)
    with ExitStack() as st0:
        QTt = [T(QT[h], f"QT{h}") for h in range(8)]
        KTt = [T(KT[h], f"KT{h}") for h in range(8)]
        Vt = [T(Vd[h], f"V{h}") for h in range(8)]
        with ExitStack() as st:
            emit_mods(S, nc, st, cT, ada_w, ada_b, mods_d, modsT)
            S.barrier()
        modp = S.sbuf("modp", [128, 2, 6, 8], F32, st0)
        with nc.allow_non_contiguous_dma(reason="tiny per-partition mod vectors"):
            for m in range(2):
                S.dma('sync', lambda e, m=m: e.dma_start(
                    out=modp[:, m], in_=mods_d[m:m + 1, :].rearrange("o (j kc p) -> p (o j) kc", j=6, kc=8, p=128)),
                    reads=[modsT], pwrites=[modp])
        onep = S.sbuf("onep", [128, 2, 8], F32, st0)
        S.op('vector', lambda e: e.tensor_scalar_add(out=onep[:], in0=modp[:, :, 1, :], scalar1=1.0),
             reads=[modp], writes=[onep])

        with ExitStack() as st:
            wi = S.sbuf("wi", [128, 8, 3 * DM], BF16, st)
            wp = S.sbuf("wp", [128, 8, 2 * DM], BF16, st)
            wiv = w_in.rearrange("(kc p) n -> p kc n", p=128)
            wpv = w_perm.rearrange("(kc p) n -> p kc n", p=128)
            for kc in range(8):
                for c0 in range(0, 3 * DM, 1024):
                    S.dma('gpsimd', lambda e, kc=kc, c0=c0: e.dma_start(out=wi[:, kc, c0:c0 + 1024],
                                                                        in_=wiv[:, kc, c0:c0 + 1024]), pwrites=[wi])
                for c0 in range(0, 2 * DM, 1024):
                    S.dma('gpsimd', lambda e, kc=kc, c0=c0: e.dma_start(out=wp[:, kc, c0:c0 + 1024],
                                                                        in_=wpv[:, kc, c0:c0 + 1024]), pwrites=[wp])
            xb = [S.sbuf(f"xb{i}", [128, 8, 512], F32, st) for i in range(2)]
            tb = [S.sbuf(f"tb{i}", [128, 8, 512], BF16, st) for i in range(2)]
            rc = [S.sbuf(f"rc{i}", [128, 512], F32, st) for i in range(2)]
            rs = [S.sbuf(f"rs{i}", [128, 512], F32, st) for i in range(2)]
            psA = [S.psum(f"psA{i}", [128, 512], F32, st) for i in range(2)]
            psB = [S.psum(f"psB{i}", [128, 512], F32, st) for i in range(2)]
            psV = [S.psum(f"psV{i}", [128, 512], F32, st) for i in range(2)]
            t1 = [S.sbuf(f"t1_{i}", [128, 512], F32, st) for i in range(2)]
            t2 = [S.sbuf(f"t2_{i}", [128, 512], F32, st) for i in range(2)]
            qk = [S.sbuf(f"qk{i}", [128, 512], BF16, st) for i in range(4)]
            vs = [S.sbuf(f"vs{i}", [128, 8, 129], BF16, st) for i in range(3)]
            for v in vs:
                S.op('gpsimd', lambda e, v=v: e.memset(v[:], 1.0), writes=[v])
            xkv = xk.rearrange("(kc p) t -> p kc t", p=128)
            blocks = [(0, NCTX)] + [(NCTX + i * 512, 512) for i in range(16)]
            iqk = 0; ivs = 0; ips = 0
            for bi, (t0, nt) in enumerate(blocks):
                X = xb[bi % 2]; TB = tb[bi % 2]; RC = rc[bi % 2]; RS = rs[bi % 2]
                mset = 1 if bi == 0 else 0
                S.dma('sync', lambda e, X=X, t0=t0, nt=nt: e.dma_start(out=X[:, :, :nt], in_=xkv[:, :, t0:t0 + nt]),
                      writes=[X])
                S.dma('sync', lambda e, RC=RC, t0=t0, nt=nt: e.dma_start(out=RC[:, :nt], in_=ropeC[:, t0:t0 + nt]),
                      writes=[RC])
                S.dma('sync', lambda e, RS=RS, t0=t0, nt=nt: e.dma_start(out=RS[:, :nt], in_=ropeS[:, t0:t0 + nt]),
                      writes=[RS])
                for kc in range(8):
                    eng = 'vector' if kc % 2 == 0 else 'gpsimd'
                    S.op(eng, lambda e, X=X, TB=TB, kc=kc, nt=nt, mset=mset: e.tensor_scalar(
                        out=TB[:, kc, :nt], in0=X[:, kc, :nt], scalar1=onep[:, mset, kc:kc + 1],
                        scalar2=modp[:, mset, 0, kc:kc + 1], op0=ALU.mult, op1=ALU.add),
                        reads=[X, onep, modp], pwrites=[TB])
                has_q = t0 < NQ
                ccs = ([('q', h) for h in range(8)] if has_q else []) + [('k', h) for h in range(8)]
                for kind, h in ccs:
                    c0 = (0 if kind == 'q' else DM) + h * 128
                    pa = psA[ips % 2]; pb = psB[ips % 2]; a1 = t1[ips % 2]; a2 = t2[ips % 2]; ips += 1
                    for kc in range(8):
                        S.op('tensor', lambda e, pa=pa, TB=TB, kc=kc, c0=c0, nt=nt: e.matmul(
                            pa[:, :nt], lhsT=wi[:, kc, c0:c0 + 128], rhs=TB[:, kc, :nt], start=(kc == 0), stop=(kc == 7)),
                            reads=[wi, TB], writes=[pa])
                    for kc in range(8):
                        S.op('tensor', lambda e, pb=pb, TB=TB, kc=kc, c0=c0, nt=nt: e.matmul(
                            pb[:, :nt], lhsT=wp[:, kc, c0:c0 + 128], rhs=TB[:, kc, :nt], start=(kc == 0), stop=(kc == 7)),
                            reads=[wp, TB], writes=[pb])
                    S.op('vector', lambda e, pa=pa, a1=a1, RC=RC, nt=nt: e.tensor_tensor(
                        out=a1[:, :nt], in0=pa[:, :nt], in1=RC[:, :nt], op=ALU.mult), reads=[pa, RC], writes=[a1])
                    S.op('vector', lambda e, pb=pb, a2=a2, RS=RS, nt=nt: e.tensor_tensor(
                        out=a2[:, :nt], in0=pb[:, :nt], in1=RS[:, :nt], op=ALU.mult), reads=[pb, RS], writes=[a2])
                    o = qk[iqk % 4]; iqk += 1
                    S.op('gpsimd', lambda e, o=o, a1=a1, a2=a2, nt=nt: e.tensor_tensor(
                        out=o[:, :nt], in0=a1[:, :nt], in1=a2[:, :nt], op=ALU.add), reads=[a1, a2], writes=[o])
                    if kind == 'q':
                        S.dma('sync', lambda e, o=o, h=h, t0=t0, nt=nt: e.dma_start(out=QT[h, :, t0:t0 + nt], in_=o[:, :nt]),
                              reads=[o], pwrites=[QTt[h]])
                    else:
                        S.dma('sync', lambda e, o=o, h=h, t0=t0, nt=nt: e.dma_start(out=KT[h, :, t0:t0 + nt], in_=o[:, :nt]),
                              reads=[o], pwrites=[KTt[h]])
                for ti in range(nt // 128):
                    V = vs[ivs % 3]; ivs += 1
                    for nh in range(2):
                        pv = psV[nh]
                        for kc in range(8):
                            S.op('tensor', lambda e, pv=pv, TB=TB, kc=kc, ti=ti, nh=nh: e.matmul(
                                pv[:], lhsT=TB[:, kc, ti * 128:(ti + 1) * 128],
                                rhs=wi[:, kc, 2 * DM + nh * 512:2 * DM + (nh + 1) * 512], start=(kc == 0), stop=(kc == 7)),
                                reads=[wi, TB], writes=[pv])
                        S.op('scalar', lambda e, pv=pv, V=V, nh=nh: e.activation(
                            out=V[:, nh * 4:(nh + 1) * 4, 0:128], in_=pv[:].rearrange("p (h d) -> p h d", h=4),
                            func=AF.Copy), reads=[pv], pwrites=[V])
                    r0 = t0 + ti * 128
                    S.dma('sync', lambda e, V=V, r0=r0: e.dma_start(
                        out=Vd[:, r0:r0 + 128, :].rearrange("h t d -> t h d"), in_=V[:]),
                        reads=[V], pwrites=Vt)

        S.barrier()
        onT = S.sbuf("onT", [128, 8, NQ], BF16, st0)
        with ExitStack() as st:
            ident = S.sbuf("ident_sb", [128, 128], BF16, st)
            S.dma('gpsimd', lambda e: e.dma_start(out=ident[:], in_=ident_d[:, :]), writes=[ident])
            lv = S.sbuf("lv", [1, 256], F32, st)
            S.dma('sync', lambda e: e.dma_start(out=lv[:], in_=lamv[:, :]), writes=[lv])
            pr = S.sbuf("pr", [1, 2, 64], F32, st)
            lvv = lv[:].rearrange("p (a b c) -> p a b c", a=2, b=2)
            S.op('vector', lambda e: e.tensor_tensor(out=pr[:], in0=lvv[:, :, 0, :], in1=lvv[:, :, 1, :], op=ALU.mult),
                 reads=[lv], writes=[pr])
            sm = S.sbuf("sm", [1, 2], F32, st)
            S.op('vector', lambda e: e.reduce_sum(out=sm[:], in_=pr[:], axis=AX.X), reads=[pr], writes=[sm])
            S.op('scalar', lambda e: e.activation(out=sm[:], in_=sm[:], func=AF.Exp), reads=[sm], writes=[sm])
            lam1 = S.sbuf("lam1", [1, 1], F32, st)
            S.op('vector', lambda e: e.tensor_tensor(out=lam1[:], in0=sm[:, 0:1], in1=sm[:, 1:2], op=ALU.subtract),
                 reads=[sm], writes=[lam1])
            S.op('vector', lambda e: e.tensor_scalar(out=lam1[:], in0=lam1[:], scalar1=float(lam_init), scalar2=-1.0,
                                                     op0=ALU.add, op1=ALU.mult), reads=[lam1], writes=[lam1])
            ones1 = S.sbuf("ones1", [1, 128], F32, st)
            S.op('vector', lambda e: e.memset(ones1[:], 1.0), writes=[ones1])
            psT = S.psum("psT", [128, 1024], BF16, st)
            psl = S.psum("psl", [128, 512], F32, st)
            S.op('tensor', lambda e: e.matmul(psl[:, 0:1], lhsT=ones1[0:1, :], rhs=lam1[0:1, 0:1], start=True, stop=True),
                 reads=[ones1, lam1], writes=[psl])
            nlam = S.sbuf("nlam", [128, 1], F32, st)
            S.op('vector', lambda e: e.tensor_copy(out=nlam[:], in_=psl[:, 0:1]), reads=[psl], writes=[nlam])
            sw = S.sbuf("sw", [128, 128], F32, st)
            S.dma('sync', lambda e: e.dma_start(out=sw[:], in_=subln.partition_broadcast(128)), writes=[sw])
            S.op('vector', lambda e: e.tensor_scalar_mul(out=sw[:], in0=sw[:], scalar1=float(1.0 - lam_init)),
                 reads=[sw], writes=[sw])

            KTs = [S.sbuf(f"KTs{i}", [128, NK], BF16, st) for i in range(2)]
            Vs = [S.sbuf(f"Vs{i}", [128, 66, 129], BF16, st) for i in range(2)]
            QTs = [S.sbuf(f"QTs{i}", [128, NQ], BF16, st) for i in range(2)]
            psS = [S.psum(f"psS{i}", [128, 512], F32, st) for i in range(3)]
            psO = [S.psum(f"psO{i}", [128, 512], F32, st) for i in range(3)]
            pts = [S.sbuf(f"pt{i}", [128, 512], BF16, st) for i in range(3)]
            Om = [S.sbuf(f"Om{i}", [128, 4, 129], F32, st) for i in range(2)]
            rec = S.sbuf("rec", [128, 2, 4], F32, st)
            osb = S.sbuf("osb", [128, 128], F32, st)
            sq = S.sbuf("sq", [128, 128], F32, st)
            ssq = S.sbuf("ssq", [128, 1], F32, st)
            onb = [S.sbuf(f"onb{i}", [128, 128], BF16, st) for i in range(2)]
            qblocks = [(0, NCTX, NCTX // 128)] + [(NCTX + i * 512, 512, NK // 128) for i in range(8)]
            iS = 0; iO = 0; iT = 0
            for h in range(8):
                KS = KTs[h % 2]; VS = Vs[h % 2]; QS = QTs[h % 2]
                S.dma('sync', lambda e, KS=KS, h=h: e.dma_start(out=KS[:], in_=KT[h]), reads=[KTt[h]], writes=[KS])
                S.dma('sync', lambda e, VS=VS, h=h: e.dma_start(out=VS[:], in_=Vd[h].rearrange("(kc p) d -> p kc d", p=128)),
                      reads=[Vt[h]], writes=[VS])
                S.dma('sync', lambda e, QS=QS, h=h: e.dma_start(out=QS[:], in_=QT[h]), reads=[QTt[h]], writes=[QS])
                for (q0, nq, nkc) in qblocks:
                    nqs = nq // 128
                    for m in range(2):
                        pO = [psO[iO % 3], psO[(iO + 1) % 3]] if nqs > 2 else [psO[iO % 3]]
                        iO += len(pO)
                        for kc in range(nkc):
                            pS = psS[iS % 3]; PT = pts[iS % 3]; iS += 1
                            S.op('tensor', lambda e, pS=pS, KS=KS, QS=QS, m=m, kc=kc, q0=q0, nq=nq: e.matmul(
                                pS[:, :nq], lhsT=KS[m * 64:(m + 1) * 64, kc * 128:(kc + 1) * 128],
                                rhs=QS[m * 64:(m + 1) * 64, q0:q0 + nq], start=True, stop=True),
                                reads=[KS, QS], writes=[pS])
                            S.op('scalar', lambda e, pS=pS, PT=PT, nq=nq: e.activation(
                                out=PT[:, :nq], in_=pS[:, :nq], func=AF.Exp, scale=0.125), reads=[pS], writes=[PT])
                            for qs in range(nqs):
                                po = pO[qs // 2]; c0 = (qs % 2) * 129
                                S.op('tensor', lambda e, po=po, c0=c0, PT=PT, VS=VS, qs=qs, kc=kc, nkc=nkc: e.matmul(
                                    po[:, c0:c0 + 129], lhsT=PT[:, qs * 128:(qs + 1) * 128], rhs=VS[:, kc, :],
                                    start=(kc == 0), stop=(kc == nkc - 1)), reads=[PT, VS], writes=[po])
                        for j, po in enumerate(pO):
                            S.op('vector', lambda e, po=po, j=j, m=m: e.tensor_copy(
                                out=Om[m][:, 2 * j:2 * j + 2, :], in_=po[:, 0:258].rearrange("p (a b) -> p a b", a=2)),
                                reads=[po], pwrites=[Om[m]])
                    S.op('vector', lambda e, nqs=nqs: e.reciprocal(out=rec[:, 0, :nqs], in_=Om[0][:, :nqs, 128]),
                         reads=[Om[0]], pwrites=[rec])
                    S.op('vector', lambda e, nqs=nqs: e.reciprocal(out=rec[:, 1, :nqs], in_=Om[1][:, :nqs, 128]),
                         reads=[Om[1]], pwrites=[rec])
                    S.op('vector', lambda e, nqs=nqs: e.tensor_scalar_mul(out=rec[:, 1, :nqs], in0=rec[:, 1, :nqs],
                                                                          scalar1=nlam[:, 0:1]),
                         reads=[rec, nlam], writes=[rec])
                    for qs in range(nqs):
                        S.op('vector', lambda e, qs=qs: e.tensor_scalar_mul(out=osb[:], in0=Om[0][:, qs, 0:128],
                                                                            scalar1=rec[:, 0, qs:qs + 1]),
                             reads=[Om[0], rec], writes=[osb])
                        S.op('vector', lambda e, qs=qs: e.scalar_tensor_tensor(
                            out=osb[:], in0=Om[1][:, qs, 0:128], scalar=rec[:, 1, qs:qs + 1], in1=osb[:],
                            op0=ALU.mult, op1=ALU.add), reads=[Om[1], rec, osb], writes=[osb])
                        S.op('gpsimd', lambda e: e.tensor_tensor(out=sq[:], in0=osb[:], in1=osb[:], op=ALU.mult),
                             reads=[osb], writes=[sq])
                        S.op('vector', lambda e: e.reduce_sum(out=ssq[:], in_=sq[:], axis=AX.X), reads=[sq], writes=[ssq])
                        S.op('vector', lambda e: e.tensor_scalar(out=ssq[:], in0=ssq[:], scalar1=1.0 / 128, scalar2=EPS,
                                                                 op0=ALU.mult, op1=ALU.add), reads=[ssq], writes=[ssq])
                        S.op('scalar', lambda e: e.activation(out=ssq[:], in_=ssq[:], func=AF.Sqrt), reads=[ssq], writes=[ssq])
                        S.op('vector', lambda e: e.reciprocal(out=ssq[:], in_=ssq[:]), reads=[ssq], writes=[ssq])
                        ob = onb[iT % 2]; iT += 1
                        S.op('vector', lambda e, ob=ob: e.scalar_tensor_tensor(
                            out=ob[:], in0=osb[:], scalar=ssq[:, 0:1], in1=sw[:], op0=ALU.mult, op1=ALU.mult),
                            reads=[osb, ssq, sw], writes=[ob])
                        S.op('tensor', lambda e, ob=ob: e.transpose(out=psT[:, 0:128], in_=ob[:], identity=ident[:]),
                             reads=[ob, ident], writes=[psT])
                        tq = q0 + qs * 128
                        S.op('scalar', lambda e, h=h, tq=tq: e.copy(out=onT[:, h, tq:tq + 128], in_=psT[:, 0:128]),
                             reads=[psT], pwrites=[onT])

        S.barrier()
        with ExitStack() as st:
            wo = S.sbuf("wo", [128, 8, DM], BF16, st)
            S.dma('gpsimd', lambda e: e.dma_start(out=wo[:], in_=w_out.rearrange("(kc p) n -> p kc n", p=128)),
                  writes=[wo])
            g1b = [load_bcast(S, st, f"g1b{m}", mods_d[m:m + 1, 2 * DM:3 * DM], modsT) for m in range(2)]
            lngb = load_bcast(S, st, "lngb", ln_g)
            lnbb = load_bcast(S, st, "lnbb", ln_b)
            psY = [S.psum(f"psY{i}", [128, 512], F32, st) for i in range(4)]
            xts = [S.sbuf(f"xts{i}", [128, DM], F32, st) for i in range(2)]
            zs = [S.sbuf(f"zs{i}", [128, DM], F32, st) for i in range(2)]
            x1s = [S.sbuf(f"x1s{i}", [128, DM], F32, st) for i in range(2)]
            lnsc = LNScratch(S, st, "lnc")
            outs = []
            for ti in range(NQ // 128):
                mset = 1 if ti < 2 else 0
                xt = xts[ti % 2]; z = zs[ti % 2]; xo = x1s[ti % 2]
                S.dma('sync', lambda e, xt=xt, ti=ti: e.dma_start(out=xt[:], in_=xtm[ti * 128:(ti + 1) * 128, :]), writes=[xt])
                for nh in range(2):
                    py = psY[(ti % 2) * 2 + nh]
                    for h in range(8):
                        S.op('tensor', lambda e, py=py, h=h, ti=ti, nh=nh: e.matmul(
                            py[:], lhsT=onT[:, h, ti * 128:(ti + 1) * 128], rhs=wo[:, h, nh * 512:(nh + 1) * 512],
                            start=(h == 0), stop=(h == 7)), reads=[onT, wo], writes=[py])
                    S.op('vector', lambda e, py=py, z=z, nh=nh, mset=mset: e.tensor_tensor(
                        out=z[:, nh * 512:(nh + 1) * 512], in0=py[:], in1=g1b[mset][:, nh * 512:(nh + 1) * 512], op=ALU.mult),
                        reads=[py, g1b[mset]], pwrites=[z])
                S.op('vector', lambda e, xt=xt, z=z: e.scalar_tensor_tensor(
                    out=z[:], in0=xt[:], scalar=ALPHA, in1=z[:], op0=ALU.mult, op1=ALU.add), reads=[xt, z], writes=[z])
                emit_ln(S, lnsc, z, xo, lngb, lnbb)
                outs.append(S.dma('sync', lambda e, xo=xo, ti=ti: e.dma_start(out=x1[ti * 128:(ti + 1) * 128, :], in_=xo[:]),
                                  reads=[xo], pwrites=[x1T]))
        S.barrier()


class LNScratch:
    def __init__(self, S, st, pfx):
        self.stats = S.sbuf(pfx + "_st", [128, 2, 6], F32, st)
        self.mv = S.sbuf(pfx + "_mv", [128, 2], F32, st)
        self.rstd = S.sbuf(pfx + "_rs", [128, 1], F32, st)


def emit_ln(S, sc, z, out, lng, lnb, eng2='gpsimd'):
    for i in range(2):
        S.op('vector', lambda e, i=i: e.bn_stats(out=sc.stats[:, i, :], in_=z[:, i * 512:(i + 1) * 512]),
             reads=[z], pwrites=[sc.stats])
    S.op('vector', lambda e: e.bn_aggr(out=sc.mv[:], in_=sc.stats[:].rearrange("p a b -> p (a b)")),
         reads=[sc.stats], writes=[sc.mv])
    S.op('vector', lambda e: e.tensor_scalar_add(out=sc.rstd[:], in0=sc.mv[:, 1:2], scalar1=EPS),
         reads=[sc.mv], writes=[sc.rstd])
    S.op('scalar', lambda e: e.activation(out=sc.rstd[:], in_=sc.rstd[:], func=AF.Sqrt), reads=[sc.rstd], writes=[sc.rstd])
    S.op('vector', lambda e: e.reciprocal(out=sc.rstd[:], in_=sc.rstd[:]), reads=[sc.rstd], writes=[sc.rstd])
    S.op('vector', lambda e: e.tensor_scalar(out=z[:], in0=z[:], scalar1=sc.mv[:, 0:1], scalar2=sc.rstd[:, 0:1],
                                             op0=ALU.subtract, op1=ALU.mult), reads=[z, sc.mv, sc.rstd], writes=[z])
    S.op(eng2, lambda e: e.tensor_tensor(out=z[:], in0=z[:], in1=lng[:], op=ALU.mult),
         reads=[z, lng], writes=[z])
    S.op(eng2, lambda e: e.tensor_tensor(out=out[:], in0=z[:], in1=lnb[:], op=ALU.add),
         reads=[z, lnb], writes=[out])


def _lambda_init(layer_idx):
    import math
    return 0.8 - 0.6 * math.exp(-0.3 * layer_idx)


def _rope_tables(pos, n_ctx):
    pos = np.asarray(pos)
    row = (pos // 64).astype(np.float32); col = (pos % 64).astype(np.float32)
    inv = (np.float32(10000.0) ** (-np.arange(16, dtype=np.float32) / np.float32(16))).astype(np.float32)
    ang = np.concatenate([row[:, None] * inv, col[:, None] * inv], -1).astype(np.float32)
    cos = np.cos(ang).astype(np.float32); sin = np.sin(ang).astype(np.float32)
    C64 = np.concatenate([cos, cos], -1); S64 = np.concatenate([-sin, sin], -1)
    C = np.concatenate([np.ones((n_ctx, 64), np.float32), C64], 0)
    Sg = np.concatenate([np.zeros((n_ctx, 64), np.float32), S64], 0)
    C = np.concatenate([C, C], -1).T; Sg = np.concatenate([Sg, Sg], -1).T
    return np.ascontiguousarray(C), np.ascontiguousarray(Sg)


def _perm_cols():
    idx = np.arange(2 * DM).reshape(2, 8, 2, 64)
    return np.concatenate([idx[..., 32:], idx[..., :32]], -1).reshape(-1)


def prep_attn(inp, core, shared):
    b, hf = divmod(core, 2)
    x = inp['x'][b]; ctx = inp['ctx'][b]
    own = x[hf * NOWN:(hf + 1) * NOWN]; oth = x[(1 - hf) * NOWN:(2 - hf) * NOWN]
    pos_own = np.arange(hf * NOWN, (hf + 1) * NOWN)
    if hf == 1:
        own = own[::-1]; ctx = ctx[::-1]; pos_own = pos_own[::-1]
    pos = np.concatenate([pos_own, np.arange((1 - hf) * NOWN, (2 - hf) * NOWN)])
    C, Sg = _rope_tables(pos, NCTX)
    d = dict(shared)
    d.update(
        xk=np.ascontiguousarray(np.concatenate([ctx, own, oth], 0).T),
        xtm=np.ascontiguousarray(np.concatenate([ctx, own], 0)),
        ropeC=C, ropeS=Sg,
        cT=np.ascontiguousarray(np.stack([inp['c'][b], inp['c_ctx']], 1)),
    )
    return d


def shared_attn(inp):
    w_in = inp['da_w_in'][0]
    return dict(
        ada_w=inp['ada_w'][0], ada_b=inp['ada_b'][0][None, :],
        w_in=w_in, w_perm=np.ascontiguousarray(w_in[:, :2 * DM][:, _perm_cols()]),
        w_out=inp['da_w_out'][0], lamv=inp['da_lambda'][0].reshape(1, 256),
        subln=inp['da_subln_w'][0][None, :], ln_g=inp['ln_g'][0, 0][None, :], ln_b=inp['ln_b'][0, 0][None, :],
        ident=np.eye(128, dtype=np.float32),
    )


def emit_moe(S, nc, xin, xinT, mods_d, modsT, router_w, router_b, w_gu, b_gu, w_down, b_down, ln_g, ln_b,
             ident_d, xout, xoutT, ntiles, nctx_tiles):
    NE = 32
    group = -(-ntiles // 3)
    with ExitStack() as st:
        identb = S.sbuf("mo_identb", [128, 128], BF16, st)
        S.dma('gpsimd', lambda e: e.dma_start(out=identb[:], in_=ident_d[:, :]), writes=[identb])
        identf = S.sbuf("mo_identf", [128, 128], F32, st)
        S.dma('sync', lambda e: e.dma_start(out=identf[:], in_=ident_d[:, :]), writes=[identf])
        nsets = 2 if nctx_tiles else 1
        sc2b = [load_bcast(S, st, f"mo_sc2b{m}", mods_d[m:m + 1, 4 * DM:5 * DM], modsT) for m in range(nsets)]
        sh2b = [load_bcast(S, st, f"mo_sh2b{m}", mods_d[m:m + 1, 3 * DM:4 * DM], modsT) for m in range(nsets)]
        g2b = [load_bcast(S, st, f"mo_g2b{m}", mods_d[m:m + 1, 5 * DM:6 * DM], modsT) for m in range(nsets)]
        for t in sc2b:
            S.op('gpsimd', lambda e, t=t: e.tensor_scalar_add(out=t[:], in0=t[:], scalar1=1.0), reads=[t], writes=[t])
        lngb = load_bcast(S, st, "mo_lngb", ln_g)
        lnbb = load_bcast(S, st, "mo_lnbb", ln_b)
        rw = S.sbuf("mo_rw", [128, 8, NE], BF16, st)
        S.dma('gpsimd', lambda e: e.dma_start(out=rw[:], in_=router_w.rearrange("(kc p) n -> p kc n", p=128)), writes=[rw])
        rbb = S.sbuf("mo_rbb", [128, NE], F32, st)
        S.dma('sync', lambda e: e.dma_start(out=rbb[:], in_=router_b.partition_broadcast(128)), writes=[rbb])
        bdn = S.sbuf("mo_bdn", [NE, DM], F32, st)
        S.dma('sync', lambda e: e.dma_start(out=bdn[:], in_=b_down[:, :]), writes=[bdn])
        bguT = S.sbuf("mo_bguT", [128, 16, NE], F32, st)
        st_tmp = ExitStack()
        bsb = S.sbuf("mo_bsb", [NE, 2 * DM], F32, st_tmp)
        S.dma('sync', lambda e: e.dma_start(out=bsb[:], in_=b_gu[:, :]), writes=[bsb])
        psX = [S.psum(f"mo_psX{i}", [128, 512], F32, st) for i in range(2)]
        psXb = S.psum("mo_psXb", [128, 1024], BF16, st)
        for c in range(16):
            p = psX[c % 2]
            S.op('tensor', lambda e, p=p, c=c: e.transpose(out=p[:, 0:NE], in_=bsb[0:NE, c * 128:(c + 1) * 128],
                                                           identity=identf[0:NE, 0:NE]), reads=[bsb, identf], writes=[p])
            if c < 8:
                S.op('vector', lambda e, p=p, c=c: e.tensor_copy(out=bguT[:, c, :], in_=p[:, 0:NE]), reads=[p], pwrites=[bguT])
            else:
                S.op('vector', lambda e, p=p, c=c: e.tensor_scalar_add(out=bguT[:, c, :], in0=p[:, 0:NE], scalar1=1.0),
                     reads=[p], pwrites=[bguT])
        S.barrier()
        st_tmp.close()
        wgu = S.sbuf("mo_wgu", [128, 8, 2 * DM], BF16, st)
        wdn = S.sbuf("mo_wdn", [128, 8, DM], BF16, st)
        wguT = [T(None) for _ in range(8)]
        wdnT = [T(None) for _ in range(8)]
        uT = S.sbuf("mo_uT", [128, 8, group * 128], BF16, st)
        acc = S.sbuf("mo_acc", [128, group, DM], F32, st)
        accT = [T(None) for _ in range(group)]
        G = S.sbuf("mo_G", [128, group, NE], F32, st)
        GT = S.sbuf("mo_GT", [NE, 128], F32, st)
        xt2 = [S.sbuf(f"mo_xt{i}", [128, DM], F32, st) for i in range(2)]
        ub = [S.sbuf(f"mo_ub{i}", [128, DM], BF16, st) for i in range(2)]
        lg = S.sbuf("mo_lg", [128, NE], F32, st)
        m8 = S.sbuf("mo_m8", [128, 8], F32, st)
        msk = S.sbuf("mo_msk", [128, NE], F32, st)
        ssum = S.sbuf("mo_ssum", [128, 1], F32, st)
        psG = [S.psum(f"mo_psG{i}", [128, 512], F32, st) for i in range(2)]
        psL = [S.psum(f"mo_psL{i}", [128, 512], F32, st) for i in range(2)]
        psY = psX
        gl = [S.sbuf(f"mo_gl{i}", [128, 512], F32, st) for i in range(2)]
        sg = [S.sbuf(f"mo_sg{i}", [128, 512], F32, st) for i in range(2)]
        l1 = [S.sbuf(f"mo_l1{i}", [128, 512], F32, st) for i in range(2)]
        actT = [S.sbuf(f"mo_act{i}", [128, 8, 512], BF16, st) for i in range(2)]
        xo = [S.sbuf(f"mo_xo{i}", [128, DM], F32, st) for i in range(1)]
        lnsc = LNScratch(S, st, "mo_lnc")
        wguv = w_gu.rearrange("e (kc p) n -> e p kc n", p=128)
        wdnv = w_down.rearrange("e (kc p) n -> e p kc n", p=128)
        it = 0
        for g0 in range(0, ntiles, group):
            gt = min(group, ntiles - g0)
            ntok = gt * 128
            for ti in range(gt):
                tg = g0 + ti
                mset = 1 if tg < nctx_tiles else 0
                xt = xt2[ti % 2]; u = ub[ti % 2]
                S.dma('sync', lambda e, xt=xt, tg=tg: e.dma_start(out=xt[:], in_=xin[tg * 128:(tg + 1) * 128, :]),
                      reads=[xinT], writes=[xt])
                S.op('vector', lambda e, xt=xt, mset=mset: e.tensor_tensor(out=xt[:], in0=xt[:], in1=sc2b[mset][:], op=ALU.mult),
                     reads=[xt, sc2b[mset]], writes=[xt])
                S.op('gpsimd', lambda e, xt=xt, u=u, mset=mset: e.tensor_tensor(out=u[:], in0=xt[:], in1=sh2b[mset][:], op=ALU.add),
                     reads=[xt, sh2b[mset]], writes=[u])
                for kc in range(8):
                    S.op('tensor', lambda e, u=u, kc=kc: e.transpose(out=psXb[:, kc * 128:(kc + 1) * 128],
                                                                     in_=u[:, kc * 128:(kc + 1) * 128], identity=identb[:]),
                         reads=[u, identb], pwrites=[psXb])
                S.op('scalar', lambda e, ti=ti: e.copy(out=uT[:, :, ti * 128:(ti + 1) * 128],
                                                       in_=psXb[:].rearrange("p (k t) -> p k t", k=8)),
                     reads=[psXb], pwrites=[uT])
                pr = psX[ti % 2]
                for kc in range(8):
                    S.op('tensor', lambda e, pr=pr, kc=kc, ti=ti: e.matmul(pr[:, 0:NE], lhsT=uT[:, kc, ti * 128:(ti + 1) * 128],
                                                                           rhs=rw[:, kc, :], start=(kc == 0), stop=(kc == 7)),
                         reads=[uT, rw], writes=[pr])
                S.op('vector', lambda e, pr=pr: e.tensor_tensor(out=lg[:], in0=pr[:, 0:NE], in1=rbb[:], op=ALU.add),
                     reads=[pr, rbb], writes=[lg])
                S.op('vector', lambda e: e.max(out=m8[:], in_=lg[:]), reads=[lg], writes=[m8])
                S.op('vector', lambda e: e.tensor_scalar(out=msk[:], in0=lg[:], scalar1=m8[:, 3:4], scalar2=None, op0=ALU.is_ge),
                     reads=[lg, m8], writes=[msk])
                S.op('vector', lambda e: e.tensor_scalar(out=lg[:], in0=lg[:], scalar1=m8[:, 0:1], scalar2=None, op0=ALU.subtract),
                     reads=[lg, m8], writes=[lg])
                S.op('scalar', lambda e: e.activation(out=lg[:], in_=lg[:], func=AF.Exp), reads=[lg], writes=[lg])
                S.op('vector', lambda e: e.tensor_tensor(out=lg[:], in0=lg[:], in1=msk[:], op=ALU.mult), reads=[lg, msk], writes=[lg])
                S.op('vector', lambda e: e.reduce_sum(out=ssum[:], in_=lg[:], axis=AX.X), reads=[lg], writes=[ssum])
                S.op('vector', lambda e: e.reciprocal(out=ssum[:], in_=ssum[:]), reads=[ssum], writes=[ssum])
                S.op('vector', lambda e, ti=ti: e.tensor_scalar_mul(out=G[:, ti, :], in0=lg[:], scalar1=ssum[:, 0:1]),
                     reads=[lg, ssum], pwrites=[G])
                pg = psX[(ti + 1) % 2]
                S.op('tensor', lambda e, pg=pg, ti=ti: e.transpose(out=pg[0:NE, 0:128], in_=G[:, ti, :], identity=identf[:]),
                     reads=[G, identf], writes=[pg])
                S.op('vector', lambda e, pg=pg: e.tensor_copy(out=GT[:], in_=pg[0:NE, 0:128]), reads=[pg], writes=[GT])
                for nh in range(2):
                    pb = psG[nh]
                    S.op('tensor', lambda e, pb=pb, nh=nh: e.matmul(pb[:], lhsT=GT[:], rhs=bdn[:, nh * 512:(nh + 1) * 512],
                                                                    start=True, stop=True), reads=[GT, bdn], writes=[pb])
                    S.op('scalar', lambda e, pb=pb, nh=nh, ti=ti: e.copy(out=acc[:, ti, nh * 512:(nh + 1) * 512], in_=pb[:]),
                         reads=[pb], pwrites=[accT[ti]])
            tblocks = [(t0, min(512, ntok - t0)) for t0 in range(0, ntok, 512)]
            for ex in range(_NEXP):
                for kc in range(8):
                    S.dma('gpsimd', lambda e, ex=ex, kc=kc: e.dma_start(out=wgu[:, kc, :], in_=wguv[ex, :, kc, :]),
                          writes=[wguT[kc]])
                for kc in range(8):
                    S.dma('gpsimd', lambda e, ex=ex, kc=kc: e.dma_start(out=wdn[:, kc, :], in_=wdnv[ex, :, kc, :]),
                          writes=[wdnT[kc]])
                for (t0, nt) in tblocks:
                    A = actT[it % 2]; it += 1
                    for j in range(8):
                        pg = psG[j % 2]; pl = psL[j % 2]
                        for kc in range(8):
                            S.op('tensor', lambda e, pg=pg, kc=kc, j=j, t0=t0, nt=nt: e.matmul(
                                pg[:, :nt], lhsT=wgu[:, kc, j * 128:(j + 1) * 128], rhs=uT[:, kc, t0:t0 + nt],
                                start=(kc == 0), stop=(kc == 7)), reads=[wguT[kc], uT], writes=[pg])
                        for kc in range(8):
                            S.op('tensor', lambda e, pl=pl, kc=kc, j=j, t0=t0, nt=nt: e.matmul(
                                pl[:, :nt], lhsT=wgu[:, kc, DM + j * 128:DM + (j + 1) * 128], rhs=uT[:, kc, t0:t0 + nt],
                                start=(kc == 0), stop=(kc == 7)), reads=[wguT[kc], uT], writes=[pl])
                        g_ = gl[j % 2]; s_ = sg[j % 2]; l_ = l1[j % 2]
                        S.op('vector', lambda e, pg=pg, g_=g_, j=j, ex=ex, nt=nt: e.tensor_scalar(
                            out=g_[:, :nt], in0=pg[:, :nt], scalar1=bguT[:, j, ex:ex + 1], scalar2=7.0, op0=ALU.add, op1=ALU.min),
                            reads=[pg, bguT], writes=[g_])
                        S.op('scalar', lambda e, g_=g_, s_=s_, nt=nt: e.activation(out=s_[:, :nt], in_=g_[:, :nt], func=AF.Sigmoid,
                                                                                   scale=1.702), reads=[g_], writes=[s_])
                        S.op('scalar', lambda e, pl=pl, l_=l_, j=j, ex=ex, nt=nt: e.activation(
                            out=l_[:, :nt], in_=pl[:, :nt], func=AF.Identity, bias=bguT[:, 8 + j, ex:ex + 1]),
                            reads=[pl, bguT], writes=[l_])
                        S.op('vector', lambda e, l_=l_, nt=nt: e.tensor_scalar(out=l_[:, :nt], in0=l_[:, :nt], scalar1=-6.0, scalar2=8.0,
                                                                                op0=ALU.max, op1=ALU.min), reads=[l_], writes=[l_])
                        S.op('vector', lambda e, g_=g_, s_=s_, nt=nt: e.tensor_tensor(out=g_[:, :nt], in0=g_[:, :nt], in1=s_[:, :nt],
                                                                                      op=ALU.mult), reads=[g_, s_], writes=[g_])
                        S.op('vector', lambda e, A=A, g_=g_, l_=l_, j=j, nt=nt: e.tensor_tensor(out=A[:, j, :nt], in0=g_[:, :nt],
                                                                                                  in1=l_[:, :nt], op=ALU.mult),
                             reads=[g_, l_], pwrites=[A])
                    for tt in range(nt // 128):
                        ti = (t0 // 128) + tt
                        for nh in range(2):
                            py = psY[nh]
                            for j in range(8):
                                S.op('tensor', lambda e, py=py, A=A, j=j, tt=tt, nh=nh: e.matmul(
                                    py[:], lhsT=A[:, j, tt * 128:(tt + 1) * 128], rhs=wdn[:, j, nh * 512:(nh + 1) * 512],
                                    start=(j == 0), stop=(j == 7)), reads=[A, wdnT[j]], writes=[py])
                            S.op('vector', lambda e, py=py, ti=ti, nh=nh, ex=ex: e.scalar_tensor_tensor(
                                out=acc[:, ti, nh * 512:(nh + 1) * 512], in0=py[:], scalar=G[:, ti, ex:ex + 1],
                                in1=acc[:, ti, nh * 512:(nh + 1) * 512], op0=ALU.mult, op1=ALU.add),
                                reads=[py, G, accT[ti]], pwrites=[accT[ti]])
            for ti in range(gt):
                tg = g0 + ti
                mset = 1 if tg < nctx_tiles else 0
                xt = xt2[ti % 2]; o = xo[0]
                S.dma('sync', lambda e, xt=xt, tg=tg: e.dma_start(out=xt[:], in_=xin[tg * 128:(tg + 1) * 128, :]),
                      reads=[xinT], writes=[xt])
                S.op('gpsimd', lambda e, ti=ti, mset=mset: e.tensor_tensor(out=acc[:, ti, :], in0=acc[:, ti, :], in1=g2b[mset][:], op=ALU.mult),
                     reads=[accT[ti], g2b[mset]], writes=[accT[ti]])
                S.op('vector', lambda e, xt=xt, ti=ti: e.scalar_tensor_tensor(out=xt[:], in0=xt[:], scalar=ALPHA, in1=acc[:, ti, :],
                                                                             op0=ALU.mult, op1=ALU.add), reads=[xt, accT[ti]], writes=[xt])
                emit_ln(S, lnsc, xt, o, lngb, lnbb)
                S.dma('sync', lambda e, o=o, tg=tg: e.dma_start(out=xout[tg * 128:(tg + 1) * 128, :], in_=o[:]),
                      reads=[o], pwrites=[xoutT])
        S.barrier()


def build_moe(ntiles, nctx_tiles):
    nc = bass.Bass("TRN2", target_bir_lowering=False)
    EI = "ExternalInput"
    NTk = ntiles * 128
    xin = _dram(nc, "xin", [NTk, DM], F32, EI)
    mods_d = _dram(nc, "mods", [2, 6 * DM], F32, EI)
    router_w = _dram(nc, "router_w", [DM, 32], F32, EI)
    router_b = _dram(nc, "router_b", [1, 32], F32, EI)
    w_gu = _dram(nc, "w_gu", [32, DM, 2 * DM], F32, EI)
    b_gu = _dram(nc, "b_gu", [32, 2 * DM], F32, EI)
    w_down = _dram(nc, "w_down", [32, DM, DM], F32, EI)
    b_down = _dram(nc, "b_down", [32, DM], F32, EI)
    ln_g = _dram(nc, "ln_g", [1, DM], F32, EI)
    ln_b = _dram(nc, "ln_b", [1, DM], F32, EI)
    ident_d = _dram(nc, "ident", [128, 128], F32, EI)
    xout = _dram(nc, "xout", [NTk, DM], F32, "ExternalOutput")
    with ExitStack() as st0:
        S = Sched(nc, st0)
        emit_moe(S, nc, xin, T(xin), mods_d, T(mods_d), router_w, router_b, w_gu, b_gu, w_down, b_down, ln_g, ln_b,
                 ident_d, xout, T(xout), ntiles, nctx_tiles)
    return nc


def shared_moe(inp, i):
    return dict(router_w=inp['router_w'][i], router_b=inp['router_b'][i][None, :], w_gu=inp['moe_w_gu'][i],
                b_gu=inp['moe_b_gu'][i], w_down=inp['moe_w_down'][i], b_down=inp['moe_b_down'][i],
                ln_g=inp['ln_g'][i, 1][None, :], ln_b=inp['ln_b'][i, 1][None, :], ident=np.eye(128, dtype=np.float32))


_NEXP = 32
NU = NQ // 128
GIN = 3104


class GlaScratch:
    def __init__(self, nc, kind="Internal", sfx=""):
        self.qT = _dram(nc, "g_qT" + sfx, [NU, 128, 4, 128], F32, kind)
        self.kT = _dram(nc, "g_kT" + sfx, [NU, 128, 4, 128], F32, kind)
        self.k = _dram(nc, "g_k" + sfx, [NQ, 512], F32, kind)
        self.v = _dram(nc, "g_v" + sfx, [NQ, DM], BF16, kind)
        self.LgA = _dram(nc, "g_LgA" + sfx, [NQ, 512], F32, kind)
        self.LgB = _dram(nc, "g_LgB" + sfx, [NQ, 512], F32, kind)
        self.r = _dram(nc, "g_r" + sfx, [NQ, DM], F32, kind)
        self.T = {n: T(getattr(self, n), n) for n in ('qT', 'kT', 'k', 'v', 'LgA', 'LgB', 'r')}


def emit_gla_proj(S, nc, xin, xinT, mods_d, modsT, w_in, wgA, wgB, ident_d, G):
    with ExitStack() as st:
        identb = S.sbuf("gp_identb", [128, 128], BF16, st)
        S.dma('gpsimd', lambda e: e.dma_start(out=identb[:], in_=ident_d[:, :]), writes=[identb])
        sc1b = [load_bcast(S, st, f"gp_sc1b{m}", mods_d[m:m + 1, DM:2 * DM], modsT) for m in range(2)]
        sh1b = [load_bcast(S, st, f"gp_sh1b{m}", mods_d[m:m + 1, 0:DM], modsT) for m in range(2)]
        for t in sc1b:
            S.op('gpsimd', lambda e, t=t: e.tensor_scalar_add(out=t[:], in0=t[:], scalar1=1.0), reads=[t], writes=[t])
        wi = S.sbuf("gp_wi", [128, 8, GIN], BF16, st)
        wiv = w_in.rearrange("(kc p) n -> p kc n", p=128)
        for kc in range(8):
            for c0, c1 in ((0, 1024), (1024, 2048), (2048, GIN)):
                S.dma('gpsimd', lambda e, kc=kc, c0=c0, c1=c1: e.dma_start(out=wi[:, kc, c0:c1], in_=wiv[:, kc, c0:c1]), pwrites=[wi])
        wg = []
        for nm, src in (("A", wgA), ("B", wgB)):
            t = S.sbuf("gp_wg" + nm, [17, 512], F32, st)
            S.dma('sync', lambda e, t=t, src=src: e.dma_start(out=t[:], in_=src[:, :]), writes=[t])
            wg.append(t)
        zaug = [S.sbuf(f"gp_zaug{i}", [32, 512], F32, st) for i in range(2)]
        for z in zaug:
            S.op('vector', lambda e, z=z: e.memset(z[:], 1.0), writes=[z])
        xts = [S.sbuf(f"gp_xt{i}", [128, DM], F32, st) for i in range(2)]
        tbf = [S.sbuf(f"gp_tbf{i}", [128, DM], BF16, st) for i in range(2)]
        tT = [S.sbuf(f"gp_tT{i}", [128, 8, 512], BF16, st) for i in range(2)]
        psXb = S.psum("gp_psXb", [128, 1024], BF16, st)
        psF = [S.psum(f"gp_psF{i}", [128, 512], F32, st) for i in range(2)]
        psZ = S.psum("gp_psZ", [128, 512], F32, st)
        psK = [S.psum(f"gp_psK{i}", [128, 512], F32, st) for i in range(3)]
        fst = [S.sbuf(f"gp_fst{i}", [128, 512], F32, st) for i in range(3)]
        kst = [S.sbuf(f"gp_kst{i}", [128, 512], F32, st) for i in range(2)]
        vst = [S.sbuf(f"gp_vst{i}", [128, DM], BF16, st) for i in range(2)]
        rst = [S.sbuf(f"gp_rst{i}", [128, DM], F32, st) for i in range(2)]
        gex = [S.sbuf(f"gp_gex{i}", [128, 512], F32, st) for i in range(2)]
        gst = [S.sbuf(f"gp_gst{i}", [128, 512], F32, st) for i in range(2)]
        blocks = [(0, NCTX)] + [(NCTX + i * 512, 512) for i in range(8)]
        iF = 0; iK = 0; ig = 0
        for bi, (t0, nt) in enumerate(blocks):
            TT = tT[bi % 2]
            mset = 1 if bi == 0 else 0
            ntl = nt // 128
            for ti in range(ntl):
                r0 = t0 + ti * 128
                xt = xts[ti % 2]; tb = tbf[ti % 2]
                S.dma('sync', lambda e, xt=xt, r0=r0: e.dma_start(out=xt[:], in_=xin[r0:r0 + 128, :]), reads=[xinT], writes=[xt])
                S.op('vector', lambda e, xt=xt, mset=mset: e.tensor_tensor(out=xt[:], in0=xt[:], in1=sc1b[mset][:], op=ALU.mult),
                     reads=[xt, sc1b[mset]], writes=[xt])
                S.op('gpsimd', lambda e, xt=xt, tb=tb, mset=mset: e.tensor_tensor(out=tb[:], in0=xt[:], in1=sh1b[mset][:], op=ALU.add),
                     reads=[xt, sh1b[mset]], writes=[tb])
                for kc in range(8):
                    S.op('tensor', lambda e, tb=tb, kc=kc: e.transpose(out=psXb[:, kc * 128:(kc + 1) * 128],
                                                                       in_=tb[:, kc * 128:(kc + 1) * 128], identity=identb[:]),
                         reads=[tb, identb], pwrites=[psXb])
                S.op('scalar', lambda e, TT=TT, ti=ti: e.copy(out=TT[:, :, ti * 128:(ti + 1) * 128],
                                                              in_=psXb[:].rearrange("p (k t) -> p k t", k=8)),
                     reads=[psXb], pwrites=[TT])
            n0 = t0 // 128
            for kind in ('q', 'k'):
                for h in range(4):
                    c0 = (0 if kind == 'q' else 512) + h * 128
                    pf = psF[iF % 2]; fs = fst[iF % 3]; iF += 1
                    for kc in range(8):
                        S.op('tensor', lambda e, pf=pf, TT=TT, kc=kc, c0=c0, nt=nt: e.matmul(
                            pf[:, :nt], lhsT=wi[:, kc, c0:c0 + 128], rhs=TT[:, kc, :nt], start=(kc == 0), stop=(kc == 7)),
                            reads=[wi, TT], writes=[pf])
                    S.op('scalar', lambda e, pf=pf, fs=fs, nt=nt, kind=kind: e.activation(
                        out=fs[:, :nt], in_=pf[:, :nt], func=AF.Copy, scale=(128.0 ** -0.5 if kind == 'q' else 1.0)),
                        reads=[pf], writes=[fs])
                    dst = G.qT if kind == 'q' else G.kT
                    S.dma('sync', lambda e, fs=fs, dst=dst, n0=n0, ntl=ntl, h=h, nt=nt: e.dma_start(
                        out=dst[n0:n0 + ntl, :, h, :].rearrange("n p t -> p n t"),
                        in_=fs[:, :nt].rearrange("p (n t) -> p n t", t=128)),
                        reads=[fs], pwrites=[G.T['qT' if kind == 'q' else 'kT']])
            for d in range(2):
                for kc in range(8):
                    S.op('tensor', lambda e, kc=kc, d=d, nt=nt: e.matmul(
                        psZ[0:16, :nt], lhsT=wi[:, kc, 3072 + d * 16:3072 + (d + 1) * 16], rhs=TT[:, kc, :nt],
                        start=(kc == 0), stop=(kc == 7)), reads=[wi, TT], writes=[psZ])
                S.op('vector', lambda e, d=d, nt=nt: e.tensor_copy(out=zaug[d][0:16, :nt], in_=psZ[0:16, :nt]),
                     reads=[psZ], pwrites=[zaug[d]])
            for ti in range(ntl):
                r0 = t0 + ti * 128
                tsl = slice(ti * 128, (ti + 1) * 128)
                pk = psK[iK % 3]; iK += 1
                ks = kst[ti % 2]
                for kc in range(8):
                    S.op('tensor', lambda e, pk=pk, kc=kc: e.matmul(pk[:], lhsT=TT[:, kc, tsl], rhs=wi[:, kc, 512:1024],
                                                                    start=(kc == 0), stop=(kc == 7)), reads=[wi, TT], writes=[pk])
                S.op('scalar', lambda e, pk=pk, ks=ks: e.copy(out=ks[:], in_=pk[:]), reads=[pk], writes=[ks])
                S.dma('sync', lambda e, ks=ks, r0=r0: e.dma_start(out=G.k[r0:r0 + 128, :], in_=ks[:]), reads=[ks], pwrites=[G.T['k']])
                vs_ = vst[ti % 2]; rs_ = rst[ti % 2]
                for which, c00 in (('v', 1024), ('r', 2048)):
                    if which == 'r' and bi == 0:
                        continue
                    for nh in range(2):
                        pk = psK[iK % 3]; iK += 1
                        for kc in range(8):
                            S.op('tensor', lambda e, pk=pk, kc=kc, c00=c00, nh=nh: e.matmul(
                                pk[:], lhsT=TT[:, kc, tsl], rhs=wi[:, kc, c00 + nh * 512:c00 + (nh + 1) * 512],
                                start=(kc == 0), stop=(kc == 7)), reads=[wi, TT], writes=[pk])
                        dstt = vs_ if which == 'v' else rs_
                        eng = 'vector' if which == 'v' else 'scalar'
                        if eng == 'vector':
                            S.op('vector', lambda e, pk=pk, dstt=dstt, nh=nh: e.tensor_copy(out=dstt[:, nh * 512:(nh + 1) * 512], in_=pk[:]),
                                 reads=[pk], pwrites=[dstt])
                        else:
                            S.op('scalar', lambda e, pk=pk, dstt=dstt, nh=nh: e.copy(out=dstt[:, nh * 512:(nh + 1) * 512], in_=pk[:]),
                                 reads=[pk], pwrites=[dstt])
                S.dma('sync', lambda e, vs_=vs_, r0=r0: e.dma_start(out=G.v[r0:r0 + 128, :], in_=vs_[:]), reads=[vs_], pwrites=[G.T['v']])
                if bi != 0:
                    S.dma('sync', lambda e, rs_=rs_, r0=r0: e.dma_start(out=G.r[r0:r0 + 128, :], in_=rs_[:]), reads=[rs_], pwrites=[G.T['r']])
                for d in range(2):
                    pk = psK[iK % 3]; iK += 1
                    ge = gex[ig % 2]; gs = gst[ig % 2]; ig += 1
                    S.op('tensor', lambda e, pk=pk, d=d: e.matmul(pk[:], lhsT=zaug[d][0:17, tsl], rhs=wg[d][0:17, :],
                                                                  start=True, stop=True), reads=[zaug[d], wg[d]], writes=[pk])
                    S.op('scalar', lambda e, pk=pk, ge=ge: e.activation(out=ge[:], in_=pk[:], func=AF.Exp, scale=-1.0),
                         reads=[pk], writes=[ge])
                    S.op('scalar', lambda e, ge=ge, gs=gs: e.activation(out=gs[:], in_=ge[:], func=AF.Ln, bias=1.0),
                         reads=[ge], writes=[gs])
                    dst = G.LgA if d == 0 else G.LgB
                    S.dma('sync', lambda e, gs=gs, dst=dst, r0=r0: e.dma_start(out=dst[r0:r0 + 128, :], in_=gs[:]),
                          reads=[gs], pwrites=[G.T['LgA' if d == 0 else 'LgB']])
        S.barrier()


def emit_gla_scan(S, nc, G, direction, cmats, S_init, S_final, oA, oAT, post=None):
    A = direction == 'A'
    Lg = G.LgA if A else G.LgB
    LgT = G.T['LgA' if A else 'LgB']
    col = 127 if A else 0
    with ExitStack() as st:
        cm = {}
        for nm in ('MinclT', 'MafterT', 'maskT'):
            t = S.sbuf("gs_" + nm, [128, 128], F32, st)
            S.dma('sync', lambda e, t=t, nm=nm: e.dma_start(out=t[:], in_=cmats[nm][:, :]), writes=[t])
            cm[nm] = t
        Sf = S.sbuf("gs_Sf", [128, 4, 256], F32, st)
        Sb = S.sbuf("gs_Sb", [128, 4, 256], BF16, st)
        SfT = [T(None) for _ in range(4)]; SbT = [T(None) for _ in range(4)]
        if S_init is None:
            S.op('vector', lambda e: e.memset(Sf[:], 0.0), writes=SfT)
        else:
            S.dma('sync', lambda e: e.dma_start(out=Sf[:], in_=S_init[:, :, :]), writes=SfT)
        for h in range(4):
            S.op('gpsimd', lambda e, h=h: e.tensor_copy(out=Sb[:, h, :], in_=Sf[:, h, :]), reads=[SfT[h]], writes=[SbT[h]])
        NB = 3
        qTu = [S.sbuf(f"gs_qT{i}", [128, 4, 128], F32, st) for i in range(NB)]
        kTu = [S.sbuf(f"gs_kT{i}", [128, 4, 128], F32, st) for i in range(NB)]
        ku = [S.sbuf(f"gs_k{i}", [128, 512], F32, st) for i in range(NB)]
        vu = [S.sbuf(f"gs_v{i}", [128, DM], BF16, st) for i in range(NB)]
        Lgu = [S.sbuf(f"gs_Lg{i}", [128, 512], F32, st) for i in range(NB)]
        bank = [S.psum(f"gs_bank{i}", [128, 512], F32, st) for i in range(5)]
        psB = [T(bank[0].t[:, h * 128:(h + 1) * 128]) for h in range(4)]
        psW = [T(bank[1].t[:, h * 128:(h + 1) * 128]) for h in range(4)]
        psA = [T(bank[2].t[:, h * 128:(h + 1) * 128]) for h in range(4)]
        psO = [T(bank[3].t[:, i * 256:(i + 1) * 256]) for i in range(2)]
        psD = [T(bank[4].t[:, i * 256:(i + 1) * 256]) for i in range(2)]
        Eq = [S.sbuf(f"gs_Eq{h}", [128, 128], F32, st) for h in range(4)]
        Ek = [S.sbuf(f"gs_Ek{h}", [128, 128], F32, st) for h in range(4)]
        Ew = [S.sbuf(f"gs_Ew{h}", [128, 128], F32, st) for h in range(4)]
        qin = [S.sbuf(f"gs_qin{h}", [128, 128], BF16, st) for h in range(4)]
        kin = [S.sbuf(f"gs_kin{h}", [128, 128], BF16, st) for h in range(4)]
        kst = [S.sbuf(f"gs_kst{h}", [128, 128], BF16, st) for h in range(4)]
        atm = [S.sbuf(f"gs_atm{h}", [128, 128], BF16, st) for h in range(4)]
        ou = [S.sbuf(f"gs_ou{i}", [128, DM], F32, st) for i in range(2)]
        if post is not None:
            P = post
            identb = S.sbuf("go_identb", [128, 128], BF16, st)
            S.dma('gpsimd', lambda e: e.dma_start(out=identb[:], in_=P['ident'][:, :]), writes=[identb])
            wo = S.sbuf("go_wo", [128, 8, DM], BF16, st)
            S.dma('gpsimd', lambda e: e.dma_start(out=wo[:], in_=P['w_out'].rearrange("(kc p) n -> p kc n", p=128)), writes=[wo])
            g1b = load_bcast(S, st, "go_g1b", P['mods_d'][0:1, 2 * DM:3 * DM], P['modsT'])
            lngb = load_bcast(S, st, "go_lngb", P['ln_g'])
            lnbb = load_bcast(S, st, "go_lnbb", P['ln_b'])
            nwb = S.sbuf("go_nwb", [128, 256], F32, st)
            S.dma('sync', lambda e: e.dma_start(out=nwb[:], in_=P['norm_w'].partition_broadcast(128)), writes=[nwb])
            oAu = [S.sbuf(f"go_oA{i}", [128, DM], F32, st) for i in range(NB)]
            ru = [S.sbuf(f"go_r{i}", [128, DM], F32, st) for i in range(NB)]
            xu = [S.sbuf(f"go_x{i}", [128, DM], F32, st) for i in range(NB)]
            sqt = S.sbuf("go_sq", [128, DM], F32, st)
            ms = S.sbuf("go_ms", [128, 4], F32, st)
            onb = S.sbuf("go_onb", [128, DM], BF16, st)
            onT = S.sbuf("go_onT", [128, 8, 128], BF16, st)
            psXb = S.psum("go_psXb", [128, 1024], BF16, st)
            psY = [S.psum(f"go_psY{i}", [128, 512], F32, st) for i in range(2)]
            xo = S.sbuf("go_xo", [128, DM], F32, st)
            lnsc = LNScratch(S, st, "go_lnc")
        units = list(range(NU)) if A else list(range(NU - 1, 1, -1))

        def load(i, n):
            r0 = n * 128
            S.dma('sync', lambda e: e.dma_start(out=kTu[i][:], in_=G.kT[n]), reads=[G.T['kT']], writes=[kTu[i]])
            S.dma('sync', lambda e: e.dma_start(out=ku[i][:], in_=G.k[r0:r0 + 128, :]), reads=[G.T['k']], writes=[ku[i]])
            S.dma('sync', lambda e: e.dma_start(out=vu[i][:], in_=G.v[r0:r0 + 128, :]), reads=[G.T['v']], writes=[vu[i]])
            S.dma('sync', lambda e: e.dma_start(out=Lgu[i][:], in_=Lg[r0:r0 + 128, :]), reads=[LgT], writes=[Lgu[i]])
            if n >= 2:
                S.dma('sync', lambda e: e.dma_start(out=qTu[i][:], in_=G.qT[n]), reads=[G.T['qT']], writes=[qTu[i]])
                if post is not None:
                    j = i
                    S.dma('sync', lambda e: e.dma_start(out=oAu[j][:], in_=oA[r0 - NCTX:r0 - NCTX + 128, :]), reads=[oAT], writes=[oAu[j]])
                    S.dma('sync', lambda e: e.dma_start(out=ru[j][:], in_=G.r[r0:r0 + 128, :]), reads=[G.T['r']], writes=[ru[j]])
                    S.dma('sync', lambda e: e.dma_start(out=xu[j][:], in_=P['xin'][r0:r0 + 128, :]), reads=[P['xinT']], writes=[xu[j]])

        load(0, units[0])
        for ui, n in enumerate(units):
            i = ui % NB
            if ui + 1 < len(units):
                load((ui + 1) % NB, units[ui + 1])
            full = n >= 2
            O = ou[ui % 2]
            for h in range(4):
                hs = slice(h * 128, (h + 1) * 128)
                S.op('tensor', lambda e: e.matmul(psB[h][:], lhsT=Lgu[i][:, hs], rhs=cm['MinclT'][:], start=True, stop=True),
                     reads=[Lgu[i], cm['MinclT']], writes=[psB[h]])
                S.op('tensor', lambda e: e.matmul(psW[h][:], lhsT=cm['MafterT'][:], rhs=Lgu[i][:, hs], start=True, stop=True),
                     reads=[Lgu[i], cm['MafterT']], writes=[psW[h]])
                S.op('scalar', lambda e: e.activation(out=Eq[h][:], in_=psB[h][:], func=AF.Exp), reads=[psB[h]], writes=[Eq[h]])
                S.op('scalar', lambda e: e.activation(out=Ew[h][:], in_=psW[h][:], func=AF.Exp), reads=[psW[h]], writes=[Ew[h]])
                S.op('gpsimd', lambda e: e.tensor_tensor(out=kst[h][:], in0=ku[i][:, hs], in1=Ew[h][:], op=ALU.mult),
                     reads=[ku[i], Ew[h]], writes=[kst[h]])
                if full:
                    S.op('scalar', lambda e: e.activation(out=Ek[h][:], in_=psB[h][:], func=AF.Exp, scale=-1.0),
                         reads=[psB[h]], writes=[Ek[h]])
                    S.op('vector', lambda e: e.tensor_tensor(out=qin[h][:], in0=qTu[i][:, h, :], in1=Eq[h][:], op=ALU.mult),
                         reads=[qTu[i], Eq[h]], writes=[qin[h]])
                    S.op('gpsimd', lambda e: e.tensor_tensor(out=kin[h][:], in0=kTu[i][:, h, :], in1=Ek[h][:], op=ALU.mult),
                         reads=[kTu[i], Ek[h]], writes=[kin[h]])
                    S.op('tensor', lambda e: e.matmul(psA[h][:], lhsT=kin[h][:], rhs=qin[h][:], start=True, stop=True),
                         reads=[kin[h], qin[h]], writes=[psA[h]])
                    S.op('vector', lambda e: e.tensor_tensor(out=atm[h][:], in0=psA[h][:], in1=cm['maskT'][:], op=ALU.mult),
                         reads=[psA[h], cm['maskT']], writes=[atm[h]])
                    po = psO[h % 2]
                    S.op('tensor', lambda e: e.matmul(po[:], lhsT=atm[h][:], rhs=vu[i][:, h * 256:(h + 1) * 256], start=True, stop=False),
                         reads=[atm[h], vu[i]], writes=[po])
                    S.op('tensor', lambda e: e.matmul(po[:], lhsT=qin[h][:], rhs=Sb[:, h, :], start=False, stop=True),
                         reads=[qin[h], SbT[h]], writes=[po])
                    if post is None:
                        S.op('scalar', lambda e: e.copy(out=O[:, h * 256:(h + 1) * 256], in_=po[:]), reads=[po], pwrites=[O])
                    else:
                        S.op('vector', lambda e: e.tensor_tensor(out=O[:, h * 256:(h + 1) * 256], in0=po[:],
                                                                 in1=oAu[i][:, h * 256:(h + 1) * 256], op=ALU.add),
                             reads=[po, oAu[i]], pwrites=[O])
                pd = psD[h % 2]
                S.op('tensor', lambda e: e.matmul(pd[:], lhsT=kst[h][:], rhs=vu[i][:, h * 256:(h + 1) * 256], start=True, stop=True),
                     reads=[kst[h], vu[i]], writes=[pd])
                S.op('vector', lambda e: e.scalar_tensor_tensor(out=Sf[:, h, :], in0=Sf[:, h, :], scalar=Eq[h][:, col:col + 1],
                                                                in1=pd[:], op0=ALU.mult, op1=ALU.add),
                     reads=[SfT[h], Eq[h], pd], writes=[SfT[h]])
                S.op('gpsimd', lambda e: e.tensor_copy(out=Sb[:, h, :], in_=Sf[:, h, :]), reads=[SfT[h]], writes=[SbT[h]])
            if not full:
                continue
            r0 = n * 128
            if post is None:
                S.dma('sync', lambda e: e.dma_start(out=oA[r0 - NCTX:r0 - NCTX + 128, :], in_=O[:]), reads=[O], pwrites=[oAT])
                continue
            j = i
            S.op('gpsimd', lambda e: e.tensor_tensor(out=sqt[:], in0=O[:], in1=O[:], op=ALU.mult), reads=[O], writes=[sqt])
            S.op('vector', lambda e: e.reduce_sum(out=ms[:], in_=sqt[:].rearrange("p (h d) -> p h d", h=4), axis=AX.X),
                 reads=[sqt], writes=[ms])
            S.op('vector', lambda e: e.tensor_scalar(out=ms[:], in0=ms[:], scalar1=1.0 / 256, scalar2=EPS, op0=ALU.mult, op1=ALU.add),
                 reads=[ms], writes=[ms])
            S.op('scalar', lambda e: e.activation(out=ms[:], in_=ms[:], func=AF.Sqrt), reads=[ms], writes=[ms])
            S.op('vector', lambda e: e.reciprocal(out=ms[:], in_=ms[:]), reads=[ms], writes=[ms])
            for h in range(4):
                S.op('vector', lambda e: e.scalar_tensor_tensor(out=O[:, h * 256:(h + 1) * 256], in0=O[:, h * 256:(h + 1) * 256],
                                                                scalar=ms[:, h:h + 1], in1=nwb[:], op0=ALU.mult, op1=ALU.mult),
                     reads=[O, ms, nwb], writes=[O])
            S.op('scalar', lambda e: e.activation(out=sqt[:], in_=ru[j][:], func=AF.Silu), reads=[ru[j]], writes=[sqt])
            S.op('gpsimd', lambda e: e.tensor_tensor(out=onb[:], in0=O[:], in1=sqt[:], op=ALU.mult), reads=[O, sqt], writes=[onb])
            for kc in range(8):
                S.op('tensor', lambda e: e.transpose(out=psXb[:, kc * 128:(kc + 1) * 128], in_=onb[:, kc * 128:(kc + 1) * 128],
                                                     identity=identb[:]), reads=[onb, identb], pwrites=[psXb])
            S.op('scalar', lambda e: e.copy(out=onT[:], in_=psXb[:].rearrange("p (k t) -> p k t", k=8)), reads=[psXb], writes=[onT])
            for nh in range(2):
                py = psY[nh]
                for kc in range(8):
                    S.op('tensor', lambda e: e.matmul(py[:], lhsT=onT[:, kc, :], rhs=wo[:, kc, nh * 512:(nh + 1) * 512],
                                                      start=(kc == 0), stop=(kc == 7)), reads=[onT, wo], writes=[py])
                S.op('vector', lambda e: e.tensor_tensor(out=sqt[:, nh * 512:(nh + 1) * 512], in0=py[:],
                                                         in1=g1b[:, nh * 512:(nh + 1) * 512], op=ALU.mult),
                     reads=[py, g1b], pwrites=[sqt])
            S.op('vector', lambda e: e.scalar_tensor_tensor(out=sqt[:], in0=xu[j][:], scalar=ALPHA, in1=sqt[:],
                                                            op0=ALU.mult, op1=ALU.add), reads=[xu[j], sqt], writes=[sqt])
            emit_ln(S, lnsc, sqt, xo, lngb, lnbb)
            S.dma('sync', lambda e: e.dma_start(out=P['xout'][r0 - NCTX:r0 - NCTX + 128, :], in_=xo[:]),
                  reads=[xo], pwrites=[P['xoutT']])
        if S_final is not None:
            S.dma('sync', lambda e: e.dma_start(out=S_final[:, :, :], in_=Sf[:]), reads=SfT)
        S.barrier()


def _gla_common_inputs(nc):
    EI = "ExternalInput"
    d = dict(
        xin=_dram(nc, "xin", [NQ, DM], F32, EI),
        w_in=_dram(nc, "w_in", [DM, GIN], F32, EI),
        wgA=_dram(nc, "wgA", [17, 512], F32, EI),
        wgB=_dram(nc, "wgB", [17, 512], F32, EI),
        ident=_dram(nc, "ident", [128, 128], F32, EI),
    )
    return d


def _cmats(nc, sfx):
    return {nm: _dram(nc, nm + sfx, [128, 128], F32, "ExternalInput") for nm in ('MinclT', 'MafterT', 'maskT')}


def build_gla1():
    nc = bass.Bass("TRN2", target_bir_lowering=False)
    EI = "ExternalInput"
    I = _gla_common_inputs(nc)
    cT = _dram(nc, "cT", [DM, 2], F32, EI)
    ada_w = _dram(nc, "ada_w", [DM, 6 * DM], F32, EI)
    ada_b = _dram(nc, "ada_b", [1, 6 * DM], F32, EI)
    cmA = _cmats(nc, "A")
    mods_d = _dram(nc, "mods", [2, 6 * DM], F32, "ExternalOutput")
    oA = _dram(nc, "oA", [NOWN, DM], F32, "ExternalOutput")
    SA = _dram(nc, "SA", [128, 4, 256], F32, "ExternalOutput")
    G = GlaScratch(nc)
    with ExitStack() as st0:
        S = Sched(nc, st0)
        modsT = T(mods_d)
        with ExitStack() as st:
            emit_mods(S, nc, st, cT, ada_w, ada_b, mods_d, modsT)
            S.barrier()
        xinT = T(I['xin'])
        emit_gla_proj(S, nc, I['xin'], xinT, mods_d, modsT, I['w_in'], I['wgA'], I['wgB'], I['ident'], G)
        emit_gla_scan(S, nc, G, 'A', cmA, None, SA, oA, T(oA))
    return nc


def build_gla2():
    nc = bass.Bass("TRN2", target_bir_lowering=False)
    EI = "ExternalInput"
    I = _gla_common_inputs(nc)
    mods_d = _dram(nc, "mods", [2, 6 * DM], F32, EI)
    cmB = _cmats(nc, "B")
    oA = _dram(nc, "oA", [NOWN, DM], F32, EI)
    SB0 = _dram(nc, "SB0", [128, 4, 256], F32, EI)
    w_out = _dram(nc, "w_out", [DM, DM], F32, EI)
    norm_w = _dram(nc, "norm_w", [1, 256], F32, EI)
    ln_g = _dram(nc, "ln_g", [1, DM], F32, EI)
    ln_b = _dram(nc, "ln_b", [1, DM], F32, EI)
    xout = _dram(nc, "xout", [NOWN, DM], F32, "ExternalOutput")
    G = GlaScratch(nc)
    with ExitStack() as st0:
        S = Sched(nc, st0)
        modsT = T(mods_d); xinT = T(I['xin'])
        emit_gla_proj(S, nc, I['xin'], xinT, mods_d, modsT, I['w_in'], I['wgA'], I['wgB'], I['ident'], G)
        post = dict(ident=I['ident'], w_out=w_out, mods_d=mods_d, modsT=modsT, ln_g=ln_g, ln_b=ln_b, norm_w=norm_w,
                    xin=I['xin'], xinT=xinT, xout=xout, xoutT=T(xout))
        emit_gla_scan(S, nc, G, 'B', cmB, SB0, None, oA, T(oA), post=post)
    return nc


def _gla_cmats():
    s = np.arange(128)[:, None]; t = np.arange(128)[None, :]
    c = np.float32(-1.0 / 16.0)
    return dict(
        MinclTA=(s <= t) * c, MafterTA=(s > t) * c, maskTA=(s <= t) * np.float32(1),
        MinclTB=(s >= t) * c, MafterTB=(s < t) * c, maskTB=(s >= t) * np.float32(1),
    )


def shared_gla(inp):
    cm = {k: np.ascontiguousarray(v.astype(np.float32)) for k, v in _gla_cmats().items()}
    w = inp['gla_w_in'][0]
    wsw = np.ascontiguousarray(np.concatenate([w[:, :3072], w[:, 3088:3104], w[:, 3072:3088]], 1))
    wg = inp['gla_w_gate'][0]; bg = inp['gla_b_gate'][0]
    aug = [np.ascontiguousarray(np.concatenate([wg[d], bg[d][None, :]], 0)) for d in range(2)]
    return dict(cm=cm, w_in=[w, wsw], aug=aug, ada_w=inp['ada_w'][1], ada_b=inp['ada_b'][1][None, :],
                w_out=inp['gla_w_out'][0], norm_w=inp['gla_norm_w'][0][None, :],
                ln_g=inp['ln_g'][1, 0][None, :], ln_b=inp['ln_b'][1, 0][None, :], ident=np.eye(128, dtype=np.float32))


def prep_gla_common(sh, hf, xin):
    return dict(xin=xin, w_in=sh['w_in'][hf], wgA=sh['aug'][hf], wgB=sh['aug'][1 - hf], ident=sh['ident'])


def _moe_inputs(nc, p):
    EI = "ExternalInput"
    return dict(router_w=_dram(nc, p + "router_w", [DM, 32], F32, EI), router_b=_dram(nc, p + "router_b", [1, 32], F32, EI),
                w_gu=_dram(nc, p + "w_gu", [32, DM, 2 * DM], F32, EI), b_gu=_dram(nc, p + "b_gu", [32, 2 * DM], F32, EI),
                w_down=_dram(nc, p + "w_down", [32, DM, DM], F32, EI), b_down=_dram(nc, p + "b_down", [32, DM], F32, EI),
                ln_g=_dram(nc, p + "ln_g", [1, DM], F32, EI), ln_b=_dram(nc, p + "ln_b", [1, DM], F32, EI))


def build_fused():
    nc = bass.Bass("TRN2", target_bir_lowering=False)
    EI = "ExternalInput"
    A = attn_inputs(nc)
    M0 = _moe_inputs(nc, "m0_"); M1 = _moe_inputs(nc, "m1_")
    g_w_in = _dram(nc, "g_w_in", [DM, GIN], F32, EI)
    wgA = _dram(nc, "wgA", [17, 512], F32, EI); wgB = _dram(nc, "wgB", [17, 512], F32, EI)
    ada_w1 = _dram(nc, "ada_w1", [DM, 6 * DM], F32, EI); ada_b1 = _dram(nc, "ada_b1", [1, 6 * DM], F32, EI)
    cmA = _cmats(nc, "A"); cmB = _cmats(nc, "B")
    g_w_out = _dram(nc, "g_w_out", [DM, DM], F32, EI)
    norm_w = _dram(nc, "norm_w", [1, 256], F32, EI)
    g_ln_g = _dram(nc, "g_ln_g", [1, DM], F32, EI); g_ln_b = _dram(nc, "g_ln_b", [1, DM], F32, EI)
    psel = _dram(nc, "psel", [128, 8], F32, EI)
    out = _dram(nc, "out", [NOWN, DM], F32, "ExternalOutput")
    x1 = _dram(nc, "x1", [NQ, DM], F32); mods0 = _dram(nc, "mods0", [2, 6 * DM], F32)
    x2 = _dram(nc, "x2", [NQ, DM], F32); mods1 = _dram(nc, "mods1", [2, 6 * DM], F32)
    oA = _dram(nc, "oA", [NOWN, DM], F32); SA = _dram(nc, "SA", [128, DM], F32)
    SG = _dram(nc, "SG", [8 * 128, DM], F32); SB0 = _dram(nc, "SB0", [128, DM], F32)
    x3 = _dram(nc, "x3", [NOWN, DM], F32)
    with ExitStack() as st0:
        S = Sched(nc, st0)
        x1T = T(x1); m0T = T(mods0); x2T = T(x2); m1T = T(mods1); oAT = T(oA); x3T = T(x3)
        emit_attn(S, nc, A, _lambda_init(0), x1, x1T, mods0, m0T)
        emit_moe(S, nc, x1, x1T, mods0, m0T, M0['router_w'], M0['router_b'], M0['w_gu'], M0['b_gu'], M0['w_down'],
                 M0['b_down'], M0['ln_g'], M0['ln_b'], A['ident'], x2, x2T, NQ // 128, NCTX // 128)
        with ExitStack() as st:
            emit_mods(S, nc, st, A['cT'], ada_w1, ada_b1, mods1, m1T)
            S.barrier()
        G = GlaScratch(nc)
        emit_gla_proj(S, nc, x2, x2T, mods1, m1T, g_w_in, wgA, wgB, A['ident'], G)
        emit_gla_scan(S, nc, G, 'A', cmA, None, SA.rearrange("p (h d) -> p h d", h=4), oA, oAT)
        SGT = T(SG)
        S.special('gpsimd', lambda e: e.collective_compute("AllGather", ALU.bypass, replica_groups=[list(range(8))],
                                                           ins=[SA.opt()], outs=[SG.opt()]), writes=[SGT])
        with ExitStack() as st:
            ps_ = S.sbuf("ex_psel", [128, 8], F32, st)
            S.dma('sync', lambda e: e.dma_start(out=ps_[:], in_=psel[:, :]), writes=[ps_])
            gt = [S.sbuf(f"ex_g{i}", [128, DM], F32, st) for i in range(2)]
            acc = S.sbuf("ex_acc", [128, DM], F32, st)
            for r in range(8):
                g = gt[r % 2]
                S.dma('sync', lambda e: e.dma_start(out=g[:], in_=SG[r * 128:(r + 1) * 128, :]), reads=[SGT], writes=[g])
                if r == 0:
                    S.op('vector', lambda e: e.tensor_scalar_mul(out=acc[:], in0=g[:], scalar1=ps_[:, 0:1]),
                         reads=[g, ps_], writes=[acc])
                else:
                    S.op('vector', lambda e: e.scalar_tensor_tensor(out=acc[:], in0=g[:], scalar=ps_[:, r:r + 1], in1=acc[:],
                                                                    op0=ALU.mult, op1=ALU.add), reads=[g, ps_, acc], writes=[acc])
            S.dma('sync', lambda e: e.dma_start(out=SB0[:, :], in_=acc[:]), reads=[acc])
            S.barrier()
        post = dict(ident=A['ident'], w_out=g_w_out, mods_d=mods1, modsT=m1T, ln_g=g_ln_g, ln_b=g_ln_b, norm_w=norm_w,
                    xin=x2, xinT=x2T, xout=x3, xoutT=x3T)
        emit_gla_scan(S, nc, G, 'B', cmB, SB0.rearrange("p (h d) -> p h d", h=4), None, oA, oAT, post=post)
        emit_moe(S, nc, x3, x3T, mods1, m1T, M1['router_w'], M1['router_b'], M1['w_gu'], M1['b_gu'], M1['w_down'],
                 M1['b_down'], M1['ln_g'], M1['ln_b'], A['ident'], out, T(out), NOWN // 128, 0)
    return nc


def kernel(**inp):
    inp = {k: np.asarray(v) for k, v in inp.items()}
    cores = list(range(8))
    sh = shared_attn(inp)
    shg = shared_gla(inp)
    shared = dict(sh)
    for i, p in ((0, "m0_"), (1, "m1_")):
        sm = shared_moe(inp, i)
        for k in ('router_w', 'router_b', 'w_gu', 'b_gu', 'w_down', 'b_down', 'ln_g', 'ln_b'):
            shared[p + k] = sm[k]
    shared.update(ada_w1=shg['ada_w'], ada_b1=shg['ada_b'], g_w_out=shg['w_out'], norm_w=shg['norm_w'],
                  g_ln_g=shg['ln_g'], g_ln_b=shg['ln_b'])
    for k, v in shg['cm'].items():
        shared[k] = v
    maps = []
    for c in cores:
        b, hf = divmod(c, 2)
        d = prep_attn(inp, c, shared)
        sel = np.zeros((128, 8), np.float32); sel[:, c ^ 1] = 1.0
        d.update(g_w_in=shg['w_in'][hf], wgA=shg['aug'][hf], wgB=shg['aug'][1 - hf], psel=sel)
        maps.append(d)
    if 'fused' not in _NC_CACHE:
        _NC_CACHE['fused'] = build_fused()
    r = run_bass_kernel_spmd(_NC_CACHE['fused'], maps, core_ids=cores).results
    out = np.empty((4, 2 * NOWN, DM), np.float32)
    for c in cores:
        b, hf = divmod(c, 2)
        xo = r[c]['out']
        out[b, hf * NOWN:(hf + 1) * NOWN] = xo[::-1] if hf == 1 else xo
    return out


_NC_CACHE = {}


def kernel_unfused(**inp):
    inp = {k: np.asarray(v) for k, v in inp.items()}
    cores = list(range(8))
    run = lambda nc, maps: run_bass_kernel_spmd(nc, maps, core_ids=cores).results
    get = lambda name, fn: _NC_CACHE.setdefault(name, None) or _NC_CACHE.__setitem__(name, fn()) or _NC_CACHE[name]
    sh = shared_attn(inp)
    r = run(get('attn', lambda: build_attn(_lambda_init(0))), [prep_attn(inp, c, sh) for c in cores])
    mods0 = [r[c]['mods'] for c in cores]; x1 = [r[c]['x1'] for c in cores]
    shm = shared_moe(inp, 0)
    maps = []
    for c in cores:
        d = dict(shm); d['mods'] = mods0[c]; d['xin'] = x1[c]; maps.append(d)
    r = run(get('moe0', lambda: build_moe(NQ // 128, NCTX // 128)), maps)
    x2 = [r[c]['xout'] for c in cores]
    shg = shared_gla(inp)
    maps = []
    for c in cores:
        b, hf = divmod(c, 2)
        d = prep_gla_common(shg, hf, x2[c])
        d.update(cT=np.ascontiguousarray(np.stack([inp['c'][b], inp['c_ctx']], 1)), ada_w=shg['ada_w'], ada_b=shg['ada_b'],
                 MinclTA=shg['cm']['MinclTA'], MafterTA=shg['cm']['MafterTA'], maskTA=shg['cm']['maskTA'])
        maps.append(d)
    r1 = run(get('gla1', build_gla1), maps)
    maps = []
    for c in cores:
        b, hf = divmod(c, 2)
        d = prep_gla_common(shg, hf, x2[c])
        d.update(mods=r1[c]['mods'], oA=r1[c]['oA'], SB0=r1[c ^ 1]['SA'], w_out=shg['w_out'], norm_w=shg['norm_w'],
                 ln_g=shg['ln_g'], ln_b=shg['ln_b'],
                 MinclTB=shg['cm']['MinclTB'], MafterTB=shg['cm']['MafterTB'], maskTB=shg['cm']['maskTB'])
        maps.append(d)
    r2 = run(get('gla2', build_gla2), maps)
    shm = shared_moe(inp, 1)
    maps = []
    for c in cores:
        d = dict(shm); d['mods'] = r1[c]['mods']; d['xin'] = r2[c]['xout']; maps.append(d)
    r3 = run(get('moe1', lambda: build_moe(NOWN // 128, 0)), maps)
    out = np.empty((4, 2 * NOWN, DM), np.float32)
    for c in cores:
        b, hf = divmod(c, 2)
        xo = r3[c]['xout']
        out[b, hf * NOWN:(hf + 1) * NOWN] = xo[::-1] if hf == 1 else xo
    return out


kernel_fused = kernel
kernel = kernel_unfused
```

```python
import numpy as np
from contextlib import ExitStack
import concourse.bass as bass
import concourse.mybir as mybir
from concourse.bass_utils import run_bass_kernel_spmd

F32 = mybir.dt.float32
BF16 = mybir.dt.bfloat16
AF = mybir.ActivationFunctionType
ALU = mybir.AluOpType
AX = mybir.AxisListType

NCTX = 256
NOWN = 4096
NQ = NCTX + NOWN
NK = NQ + NOWN
DM = 1024
ALPHA = (2.0 * 2) ** 0.25
EPS = 1e-5


class T:
    __slots__ = ('t', 'w', 'wf', 'r', 'name')

    def __init__(self, t, name=''):
        self.t = t; self.w = []; self.wf = None; self.r = []; self.name = name

    def __getitem__(self, k):
        return self.t[k]


class Sched:
    ENG = ('tensor', 'vector', 'scalar', 'gpsimd', 'sync')
    EPOCH = 16000
    KDMA = 8

    def __init__(self, nc, stack):
        self.nc = nc; self.stack = stack
        self.cnt = {e: 0 for e in self.ENG}
        self.sems = {e: [] for e in self.ENG}
        self.dcnt = {e: 0 for e in self.ENG}
        self.dsems = {e: [] for e in self.ENG}
        self.waited = {e: {} for e in self.ENG}
        self.nwaits = 0

    def _sem(self, name):
        return self.stack.enter_context(self.nc.semaphore(name))

    def _uname(self, name):
        self.uid = getattr(self, 'uid', 0) + 1
        return f"{name}_{self.uid}"

    def sbuf(self, name, shape, dt, stack=None):
        st = stack or self.stack
        return T(st.enter_context(self.nc.sbuf_tensor(self._uname(name), list(shape), dt)), name)

    def psum(self, name, shape, dt=F32, stack=None):
        st = stack or self.stack
        return T(st.enter_context(self.nc.psum_tensor(self._uname(name), list(shape), dt)), name)

    def special(self, eng, fn, reads=(), writes=()):
        deps = self._deps(eng, reads, writes, ())
        sem = self._sem(self._uname('x_' + eng))
        tok = ('x', sem, 1)
        e = self._emit_waits(eng, deps)
        fn(e).then_inc(sem)
        self._commit(tok, reads, writes, ())
        return tok

    def _need(self, issuer, tok, same_ok=False):
        if tok is None:
            return None
        if tok[0] == 'c':
            _, e, ep, n = tok
            if e == issuer and same_ok:
                return None
            key = ('c', e)
            if (ep, n) <= self.waited[issuer].get(key, (-1, 0)):
                return None
            self.waited[issuer][key] = (ep, n)
            return tok
        if tok[0] == 'x':
            key = ('x', id(tok[1]))
            if self.waited[issuer].get(key, 0) >= tok[2]:
                return None
            self.waited[issuer][key] = tok[2]
            return tok
        _, e, slot, val = tok
        key = ('d', e, slot)
        if self.waited[issuer].get(key, 0) >= val:
            return None
        self.waited[issuer][key] = val
        return tok

    def _deps(self, issuer, reads, writes, pwrites):
        out = []

        def add(tk, same_ok):
            k = self._need(issuer, tk, same_ok)
            if k:
                out.append(k)
        for r in reads:
            for tk in r.w:
                add(tk, False)
        for w in writes:
            for tk in w.w:
                add(tk, True)
            for tk in w.r:
                add(tk, True)
        for w in pwrites:
            add(w.wf, True)
            for tk in w.r:
                add(tk, True)
        return out

    @staticmethod
    def _push(lst, tok):
        if tok[0] == 'c':
            lst[:] = [x for x in lst if not (x[0] == 'c' and x[1] == tok[1])]
        lst.append(tok)

    def _commit(self, tok, reads, writes, pwrites):
        for r in reads:
            self._push(r.r, tok)
        for w in writes:
            w.w = [tok]; w.wf = tok; w.r = []
        for w in pwrites:
            self._push(w.w, tok); w.r = []

    def semof(self, tok):
        if tok[0] == 'c':
            return self.sems[tok[1]][tok[2]], tok[3]
        if tok[0] == 'x':
            return tok[1], tok[2]
        return self.dsems[tok[1]][tok[2]], tok[3]

    def _emit_waits(self, eng, deps):
        e = getattr(self.nc, eng)
        for d in deps:
            s, v = self.semof(d)
            e.wait_ge(s, v)
            self.nwaits += 1
        return e

    def op(self, eng, fn, reads=(), writes=(), pwrites=()):
        deps = self._deps(eng, reads, writes, pwrites)
        n = self.cnt[eng]; ep, k = divmod(n, self.EPOCH)
        if k == 0:
            self.sems[eng].append(self._sem(f's_{eng}_{ep}'))
        self.cnt[eng] = n + 1
        tok = ('c', eng, ep, k + 1)
        e = self._emit_waits(eng, deps)
        fn(e).then_inc(self.sems[eng][ep], 1)
        self._commit(tok, reads, writes, pwrites)
        return tok

    def dma(self, eng, fn, reads=(), writes=(), pwrites=()):
        deps = self._deps(eng, reads, writes, pwrites)
        i = self.dcnt[eng]; self.dcnt[eng] = i + 1
        rnd, slot = divmod(i, self.KDMA)
        if rnd == 0:
            self.dsems[eng].append(self._sem(f'd_{eng}_{slot}'))
        else:
            k = self._need(eng, ('d', eng, slot, 16 * rnd))
            if k:
                deps.append(k)
        tok = ('d', eng, slot, 16 * (rnd + 1))
        e = self._emit_waits(eng, deps)
        fn(e).then_inc(self.dsems[eng][slot], 16)
        self._commit(tok, reads, writes, pwrites)
        return tok

    def all_tokens(self):
        toks = []
        for e in self.ENG:
            n = self.cnt[e]
            if n:
                ep, k = divmod(n - 1, self.EPOCH)
                toks.append(('c', e, ep, k + 1))
            for i in range(max(0, self.dcnt[e] - self.KDMA), self.dcnt[e]):
                rnd, slot = divmod(i, self.KDMA)
                toks.append(('d', e, slot, 16 * (rnd + 1)))
        return toks

    def barrier(self, engines=None):
        toks = self.all_tokens()
        for e in (engines or self.ENG):
            deps = [k for k in (self._need(e, t, same_ok=True) for t in toks) if k]
            self._emit_waits(e, deps)


def _dram(nc, name, shape, dt, kind="Internal"):
    return nc.dram_tensor(name, list(shape), dt, kind=kind).ap()


class RR:
    def __init__(self, items):
        self.items = items; self.i = 0

    def __call__(self):
        x = self.items[self.i % len(self.items)]; self.i += 1
        return x


def emit_mods(S, nc, st, cT, ada_w, ada_b, mods_d, modsT):
    cs = S.sbuf("m_cs", [128, 8, 2], F32, st)
    S.dma('sync', lambda e: e.dma_start(out=cs[:], in_=cT.rearrange("(kc p) m -> p kc m", p=128)),
          writes=[cs])
    S.op('scalar', lambda e: e.activation(out=cs[:], in_=cs[:], func=AF.Silu), reads=[cs], writes=[cs])
    ab = S.sbuf("m_ab", [2, 6144], F32, st)
    S.dma('sync', lambda e: e.dma_start(out=ab[:], in_=ada_b.partition_broadcast(2)), writes=[ab])
    msb = S.sbuf("m_sb", [2, 6144], F32, st)
    aw = [S.sbuf(f"m_aw{i}", [128, 8, 512], F32, st) for i in range(2)]
    ps = [S.psum(f"m_ps{i}", [2, 512], F32, st) for i in range(2)]
    awv = ada_w.rearrange("(kc p) n -> p kc n", p=128)
    for nb in range(12):
        a = aw[nb % 2]; p = ps[nb % 2]
        S.dma('sync', lambda e, a=a, nb=nb: e.dma_start(out=a[:], in_=awv[:, :, nb * 512:(nb + 1) * 512]),
              writes=[a])
        for kc in range(8):
            S.op('tensor', lambda e, a=a, p=p, kc=kc: e.matmul(p[:], lhsT=cs[:, kc, :], rhs=a[:, kc, :],
                                                                start=(kc == 0), stop=(kc == 7)),
                 reads=[cs, a], writes=[p])
        S.op('vector', lambda e, p=p, nb=nb: e.tensor_tensor(out=msb[:, nb * 512:(nb + 1) * 512], in0=p[:],
                                                             in1=ab[:, nb * 512:(nb + 1) * 512], op=ALU.add),
             reads=[p, ab], pwrites=[msb])
    S.dma('sync', lambda e: e.dma_start(out=mods_d[:, :], in_=msb[:]), reads=[msb], writes=[modsT])


def load_bcast(S, st, name, src_row, modsT=None, eng='sync'):
    t = S.sbuf(name, [128, DM], F32, st)
    S.dma(eng, lambda e: e.dma_start(out=t[:], in_=src_row.partition_broadcast(128)),
          reads=([modsT] if modsT is not None else []), writes=[t])
    return t


def attn_inputs(nc):
    EI = "ExternalInput"
    return dict(
        xk=_dram(nc, "xk", [DM, NK], F32, EI), xtm=_dram(nc, "xtm", [NQ, DM], F32, EI),
        ropeC=_dram(nc, "ropeC", [128, NK], F32, EI), ropeS=_dram(nc, "ropeS", [128, NK], F32, EI),
        cT=_dram(nc, "cT", [DM, 2], F32, EI),
        ada_w=_dram(nc, "ada_w", [DM, 6 * DM], F32, EI), ada_b=_dram(nc, "ada_b", [1, 6 * DM], F32, EI),
        w_in=_dram(nc, "w_in", [DM, 3 * DM], F32, EI), w_perm=_dram(nc, "w_perm", [DM, 2 * DM], F32, EI),
        w_out=_dram(nc, "w_out", [DM, DM], F32, EI), lamv=_dram(nc, "lamv", [1, 256], F32, EI),
        subln=_dram(nc, "subln", [1, 128], F32, EI), ln_g=_dram(nc, "ln_g", [1, DM], F32, EI),
        ln_b=_dram(nc, "ln_b", [1, DM], F32, EI), ident=_dram(nc, "ident", [128, 128], F32, EI))


def build_attn(lam_init):
    nc = bass.Bass("TRN2", target_bir_lowering=False)
    I = attn_inputs(nc)
    x1 = _dram(nc, "x1", [NQ, DM], F32, "ExternalOutput")
    mods_d = _dram(nc, "mods", [2, 6 * DM], F32, "ExternalOutput")
    with ExitStack() as st0:
        S = Sched(nc, st0)
        emit_attn(S, nc, I, lam_init, x1, T(x1, "x1"), mods_d, T(mods_d, "mods"))
    return nc


def emit_attn(S, nc, I, lam_init, x1, x1T, mods_d, modsT):
    xk, xtm, ropeC, ropeS, cT = I['xk'], I['xtm'], I['ropeC'], I['ropeS'], I['cT']
    ada_w, ada_b, w_in, w_perm, w_out = I['ada_w'], I['ada_b'], I['w_in'], I['w_perm'], I['w_out']
    lamv, subln, ln_g, ln_b, ident_d = I['lamv'], I['subln'], I['ln_g'], I['ln_b'], I['ident']
    QT = _dram(nc, "QT", [8, 128, NQ], BF16)
    KT = _dram(nc, "KT", [8, 128, NK], BF16)
    Vd = _dram(nc, "Vd", [8, NK, 129], BF16)
    with ExitStack() as st0:
        QTt = [T(QT[h], f"QT{h}") for h in range(8)]
        KTt = [T(KT[h], f"KT{h}") for h in range(8)]
        Vt = [T(Vd[h], f"V{h}") for h in range(8)]
        with ExitStack() as st:
            emit_mods(S, nc, st, cT, ada_w, ada_b, mods_d, modsT)
            S.barrier()
        modp = S.sbuf("modp", [128, 2, 6, 8], F32, st0)
        with nc.allow_non_contiguous_dma(reason="tiny per-partition mod vectors"):
            for m in range(2):
                S.dma('sync', lambda e, m=m: e.dma_start(
                    out=modp[:, m], in_=mods_d[m:m + 1, :].rearrange("o (j kc p) -> p (o j) kc", j=6, kc=8, p=128)),
                    reads=[modsT], pwrites=[modp])
        onep = S.sbuf("onep", [128, 2, 8], F32, st0)
        S.op('vector', lambda e: e.tensor_scalar_add(out=onep[:], in0=modp[:, :, 1, :], scalar1=1.0),
             reads=[modp], writes=[onep])

        with ExitStack() as st:
            wi = S.sbuf("wi", [128, 8, 3 * DM], BF16, st)
            wp = S.sbuf("wp", [128, 8, 2 * DM], BF16, st)
            wiv = w_in.rearrange("(kc p) n -> p kc n", p=128)
            wpv = w_perm.rearrange("(kc p) n -> p kc n", p=128)
            for kc in range(8):
                for c0 in range(0, 3 * DM, 1024):
                    S.dma('gpsimd', lambda e, kc=kc, c0=c0: e.dma_start(out=wi[:, kc, c0:c0 + 1024],
                                                                        in_=wiv[:, kc, c0:c0 + 1024]), pwrites=[wi])
                for c0 in range(0, 2 * DM, 1024):
                    S.dma('gpsimd', lambda e, kc=kc, c0=c0: e.dma_start(out=wp[:, kc, c0:c0 + 1024],
                                                                        in_=wpv[:, kc, c0:c0 + 1024]), pwrites=[wp])
            xb = [S.sbuf(f"xb{i}", [128, 8, 512], F32, st) for i in range(2)]
            tb = [S.sbuf(f"tb{i}", [128, 8, 512], BF16, st) for i in range(2)]
            rc = [S.sbuf(f"rc{i}", [128, 512], F32, st) for i in range(2)]
            rs = [S.sbuf(f"rs{i}", [128, 512], F32, st) for i in range(2)]
            psA = [S.psum(f"psA{i}", [128, 512], F32, st) for i in range(2)]
            psB = [S.psum(f"psB{i}", [128, 512], F32, st) for i in range(2)]
            psV = [S.psum(f"psV{i}", [128, 512], F32, st) for i in range(2)]
            t1 = [S.sbuf(f"t1_{i}", [128, 512], F32, st) for i in range(2)]
            t2 = [S.sbuf(f"t2_{i}", [128, 512], F32, st) for i in range(2)]
            qk = [S.sbuf(f"qk{i}", [128, 512], BF16, st) for i in range(4)]
            vs = [S.sbuf(f"vs{i}", [128, 8, 129], BF16, st) for i in range(3)]
            for v in vs:
                S.op('gpsimd', lambda e, v=v: e.memset(v[:], 1.0), writes=[v])
            xkv = xk.rearrange("(kc p) t -> p kc t", p=128)
            blocks = [(0, NCTX)] + [(NCTX + i * 512, 512) for i in range(16)]
            iqk = 0; ivs = 0; ips = 0
            for bi, (t0, nt) in enumerate(blocks):
                X = xb[bi % 2]; TB = tb[bi % 2]; RC = rc[bi % 2]; RS = rs[bi % 2]
                mset = 1 if bi == 0 else 0
                S.dma('sync', lambda e, X=X, t0=t0, nt=nt: e.dma_start(out=X[:, :, :nt], in_=xkv[:, :, t0:t0 + nt]),
                      writes=[X])
                S.dma('sync', lambda e, RC=RC, t0=t0, nt=nt: e.dma_start(out=RC[:, :nt], in_=ropeC[:, t0:t0 + nt]),
                      writes=[RC])
                S.dma('sync', lambda e, RS=RS, t0=t0, nt=nt: e.dma_start(out=RS[:, :nt], in_=ropeS[:, t0:t0 + nt]),
                      writes=[RS])
                for kc in range(8):
                    eng = 'vector' if kc % 2 == 0 else 'gpsimd'
                    S.op(eng, lambda e, X=X, TB=TB, kc=kc, nt=nt, mset=mset: e.tensor_scalar(
                        out=TB[:, kc, :nt], in0=X[:, kc, :nt], scalar1=onep[:, mset, kc:kc + 1],
                        scalar2=modp[:, mset, 0, kc:kc + 1], op0=ALU.mult, op1=ALU.add),
                        reads=[X, onep, modp], pwrites=[TB])
                has_q = t0 < NQ
                ccs = ([('q', h) for h in range(8)] if has_q else []) + [('k', h) for h in range(8)]
                for kind, h in ccs:
                    c0 = (0 if kind == 'q' else DM) + h * 128
                    pa = psA[ips % 2]; pb = psB[ips % 2]; a1 = t1[ips % 2]; a2 = t2[ips % 2]; ips += 1
                    for kc in range(8):
                        S.op('tensor', lambda e, pa=pa, TB=TB, kc=kc, c0=c0, nt=nt: e.matmul(
                            pa[:, :nt], lhsT=wi[:, kc, c0:c0 + 128], rhs=TB[:, kc, :nt], start=(kc == 0), stop=(kc == 7)),
                            reads=[wi, TB], writes=[pa])
                    for kc in range(8):
                        S.op('tensor', lambda e, pb=pb, TB=TB, kc=kc, c0=c0, nt=nt: e.matmul(
                            pb[:, :nt], lhsT=wp[:, kc, c0:c0 + 128], rhs=TB[:, kc, :nt], start=(kc == 0), stop=(kc == 7)),
                            reads=[wp, TB], writes=[pb])
                    S.op('vector', lambda e, pa=pa, a1=a1, RC=RC, nt=nt: e.tensor_tensor(
                        out=a1[:, :nt], in0=pa[:, :nt], in1=RC[:, :nt], op=ALU.mult), reads=[pa, RC], writes=[a1])
                    S.op('vector', lambda e, pb=pb, a2=a2, RS=RS, nt=nt: e.tensor_tensor(
                        out=a2[:, :nt], in0=pb[:, :nt], in1=RS[:, :nt], op=ALU.mult), reads=[pb, RS], writes=[a2])
                    o = qk[iqk % 4]; iqk += 1
                    S.op('gpsimd', lambda e, o=o, a1=a1, a2=a2, nt=nt: e.tensor_tensor(
                        out=o[:, :nt], in0=a1[:, :nt], in1=a2[:, :nt], op=ALU.add), reads=[a1, a2], writes=[o])
                    if kind == 'q':
                        S.dma('sync', lambda e, o=o, h=h, t0=t0, nt=nt: e.dma_start(out=QT[h, :, t0:t0 + nt], in_=o[:, :nt]),
                              reads=[o], pwrites=[QTt[h]])
                    else:
                        S.dma('sync', lambda e, o=o, h=h, t0=t0, nt=nt: e.dma_start(out=KT[h, :, t0:t0 + nt], in_=o[:, :nt]),
                              reads=[o], pwrites=[KTt[h]])
                for ti in range(nt // 128):
                    V = vs[ivs % 3]; ivs += 1
                    for nh in range(2):
                        pv = psV[nh]
                        for kc in range(8):
                            S.op('tensor', lambda e, pv=pv, TB=TB, kc=kc, ti=ti, nh=nh: e.matmul(
                                pv[:], lhsT=TB[:, kc, ti * 128:(ti + 1) * 128],
                                rhs=wi[:, kc, 2 * DM + nh * 512:2 * DM + (nh + 1) * 512], start=(kc == 0), stop=(kc == 7)),
                                reads=[wi, TB], writes=[pv])
                        S.op('scalar', lambda e, pv=pv, V=V, nh=nh: e.activation(
                            out=V[:, nh * 4:(nh + 1) * 4, 0:128], in_=pv[:].rearrange("p (h d) -> p h d", h=4),
                            func=AF.Copy), reads=[pv], pwrites=[V])
                    r0 = t0 + ti * 128
                    S.dma('sync', lambda e, V=V, r0=r0: e.dma_start(
                        out=Vd[:, r0:r0 + 128, :].rearrange("h t d -> t h d"), in_=V[:]),
                        reads=[V], pwrites=Vt)

        S.barrier()
        onT = S.sbuf("onT", [128, 8, NQ], BF16, st0)
        with ExitStack() as st:
            ident = S.sbuf("ident_sb", [128, 128], BF16, st)
            S.dma('gpsimd', lambda e: e.dma_start(out=ident[:], in_=ident_d[:, :]), writes=[ident])
            lv = S.sbuf("lv", [1, 256], F32, st)
            S.dma('sync', lambda e: e.dma_start(out=lv[:], in_=lamv[:, :]), writes=[lv])
            pr = S.sbuf("pr", [1, 2, 64], F32, st)
            lvv = lv[:].rearrange("p (a b c) -> p a b c", a=2, b=2)
            S.op('vector', lambda e: e.tensor_tensor(out=pr[:], in0=lvv[:, :, 0, :], in1=lvv[:, :, 1, :], op=ALU.mult),
                 reads=[lv], writes=[pr])
            sm = S.sbuf("sm", [1, 2], F32, st)
            S.op('vector', lambda e: e.reduce_sum(out=sm[:], in_=pr[:], axis=AX.X), reads=[pr], writes=[sm])
            S.op('scalar', lambda e: e.activation(out=sm[:], in_=sm[:], func=AF.Exp), reads=[sm], writes=[sm])
            lam1 = S.sbuf("lam1", [1, 1], F32, st)
            S.op('vector', lambda e: e.tensor_tensor(out=lam1[:], in0=sm[:, 0:1], in1=sm[:, 1:2], op=ALU.subtract),
                 reads=[sm], writes=[lam1])
            S.op('vector', lambda e: e.tensor_scalar(out=lam1[:], in0=lam1[:], scalar1=float(lam_init), scalar2=-1.0,
                                                     op0=ALU.add, op1=ALU.mult), reads=[lam1], writes=[lam1])
            ones1 = S.sbuf("ones1", [1, 128], F32, st)
            S.op('vector', lambda e: e.memset(ones1[:], 1.0), writes=[ones1])
            psl = S.psum("psl", [128, 512], F32, st)
            S.op('tensor', lambda e: e.matmul(psl[:, 0:1], lhsT=ones1[0:1, :], rhs=lam1[0:1, 0:1], start=True, stop=True),
                 reads=[ones1, lam1], writes=[psl])
            nlam = S.sbuf("nlam", [128, 1], F32, st)
            S.op('vector', lambda e: e.tensor_copy(out=nlam[:], in_=psl[:, 0:1]), reads=[psl], writes=[nlam])
            sw = S.sbuf("sw", [128, 128], F32, st)
            S.dma('sync', lambda e: e.dma_start(out=sw[:], in_=subln.partition_broadcast(128)), writes=[sw])
            S.op('vector', lambda e: e.tensor_scalar_mul(out=sw[:], in0=sw[:], scalar1=float(1.0 - lam_init)),
                 reads=[sw], writes=[sw])

            swp = S.sbuf("swp", [128, 1], F32, st)
            with nc.allow_non_contiguous_dma(reason="tiny per-partition vector"):
                S.dma('sync', lambda e: e.dma_start(out=swp[:], in_=subln.rearrange("o d -> d o")), writes=[swp])
            S.op('vector', lambda e: e.tensor_scalar_mul(out=swp[:], in0=swp[:], scalar1=float(1.0 - lam_init)),
                 reads=[swp], writes=[swp])
            onesf = S.sbuf("onesf", [128, 128], F32, st)
            S.op('vector', lambda e: e.memset(onesf[:], 1.0), writes=[onesf])
            KTs = [S.sbuf(f"KTs{i}", [128, NK], BF16, st) for i in range(2)]
            Vs = [S.sbuf(f"Vs{i}", [128, 66, 129], BF16, st) for i in range(2)]
            QTs = [S.sbuf(f"QTs{i}", [128, NQ], BF16, st) for i in range(2)]
            psS = [S.psum(f"psS{i}", [128, 512], F32, st) for i in range(4)]
            psO = [S.psum(f"psO{i}", [128, 512], F32, st) for i in range(3)]
            pts = [S.sbuf(f"pt{i}", [128, 512], BF16, st) for i in range(8)]
            accP = [S.sbuf(f"accP{i}", [128, 512], F32, st) for i in range(2)]
            Rr = [S.sbuf(f"Rr{i}", [128, 512], F32, st) for i in range(2)]
            OT = [S.sbuf(f"OT{i}", [128, 512], F32, st) for i in range(2)]
            osb = S.sbuf("osb", [128, 512], F32, st)
            sq = S.sbuf("sq", [128, 512], F32, st)
            rstd = S.sbuf("rstd", [128, 512], F32, st)
            psM = psl
            qblocks = [(0, NCTX, NCTX // 128)] + [(NCTX + i * 512, 512, NK // 128) for i in range(8)]
            iS = 0; iO = 0
            for h in range(8):
                KS = KTs[h % 2]; VS = Vs[h % 2]; QS = QTs[h % 2]
                S.dma('sync', lambda e: e.dma_start(out=KS[:], in_=KT[h]), reads=[KTt[h]], writes=[KS])
                S.dma('sync', lambda e: e.dma_start(out=VS[:], in_=Vd[h].rearrange("(kc p) d -> p kc d", p=128)),
                      reads=[Vt[h]], writes=[VS])
                S.dma('sync', lambda e: e.dma_start(out=QS[:], in_=QT[h]), reads=[QTt[h]], writes=[QS])
                for (q0, nq, nkc) in qblocks:
                    for m in range(2):
                        pO = psO[iO % 3]; iO += 1
                        ap_ = accP[m]
                        slots = {}
                        LA = 3
                        for kk in range(nkc + LA):
                            if kk < nkc:
                                kc = kk
                                pS = psS[iS % 4]; PT = pts[iS % 8]; iS += 1
                                slots[kc] = PT
                                S.op('tensor', lambda e: e.matmul(
                                    pS[:, :nq], lhsT=KS[m * 64:(m + 1) * 64, kc * 128:(kc + 1) * 128],
                                    rhs=QS[m * 64:(m + 1) * 64, q0:q0 + nq], start=True, stop=True),
                                    reads=[KS, QS], writes=[pS])
                                S.op('scalar', lambda e: e.activation(out=PT[:, :nq], in_=pS[:, :nq], func=AF.Exp, scale=0.125),
                                     reads=[pS], writes=[PT])
                                if kc == 0:
                                    S.op('vector', lambda e: e.tensor_copy(out=ap_[:, :nq], in_=PT[:, :nq]), reads=[PT], writes=[ap_])
                                else:
                                    S.op('vector', lambda e: e.tensor_tensor(out=ap_[:, :nq], in0=ap_[:, :nq], in1=PT[:, :nq], op=ALU.add),
                                         reads=[PT, ap_], writes=[ap_])
                            if kk >= LA:
                                kc = kk - LA
                                PT = slots.pop(kc)
                                S.op('tensor', lambda e: e.matmul(pO[:, :nq], lhsT=VS[:, kc, 0:128], rhs=PT[:, :nq],
                                                                  start=(kc == 0), stop=(kc == nkc - 1)),
                                     reads=[PT, VS], writes=[pO])
                        S.op('tensor', lambda e: e.matmul(psM[:, :nq], lhsT=onesf[:], rhs=ap_[:, :nq], start=True, stop=True),
                             reads=[onesf, ap_], writes=[psM])
                        S.op('vector', lambda e: e.reciprocal(out=Rr[m][:, :nq], in_=psM[:, :nq]), reads=[psM], writes=[Rr[m]])
                        S.op('vector', lambda e: e.tensor_tensor(out=OT[m][:, :nq], in0=pO[:, :nq], in1=Rr[m][:, :nq], op=ALU.mult),
                             reads=[pO, Rr[m]], writes=[OT[m]])
                    S.op('vector', lambda e: e.scalar_tensor_tensor(out=osb[:, :nq], in0=OT[1][:, :nq], scalar=nlam[:, 0:1],
                                                                    in1=OT[0][:, :nq], op0=ALU.mult, op1=ALU.add),
                         reads=[OT[0], OT[1], nlam], writes=[osb])
                    S.op('gpsimd', lambda e: e.tensor_tensor(out=sq[:, :nq], in0=osb[:, :nq], in1=osb[:, :nq], op=ALU.mult),
                         reads=[osb], writes=[sq])
                    S.op('tensor', lambda e: e.matmul(psM[:, :nq], lhsT=onesf[:], rhs=sq[:, :nq], start=True, stop=True),
                         reads=[onesf, sq], writes=[psM])
                    S.op('vector', lambda e: e.tensor_scalar(out=rstd[:, :nq], in0=psM[:, :nq], scalar1=1.0 / 128, scalar2=EPS,
                                                             op0=ALU.mult, op1=ALU.add), reads=[psM], writes=[rstd])
                    S.op('scalar', lambda e: e.activation(out=rstd[:, :nq], in_=rstd[:, :nq], func=AF.Sqrt), reads=[rstd], writes=[rstd])
                    S.op('vector', lambda e: e.reciprocal(out=rstd[:, :nq], in_=rstd[:, :nq]), reads=[rstd], writes=[rstd])
                    S.op('vector', lambda e: e.scalar_tensor_tensor(out=onT[:, h, q0:q0 + nq], in0=osb[:, :nq], scalar=swp[:, 0:1],
                                                                    in1=rstd[:, :nq], op0=ALU.mult, op1=ALU.mult),
                         reads=[osb, swp, rstd], pwrites=[onT])

        S.barrier()
        with ExitStack() as st:
            wo = S.sbuf("wo", [128, 8, DM], BF16, st)
            S.dma('gpsimd', lambda e: e.dma_start(out=wo[:], in_=w_out.rearrange("(kc p) n -> p kc n", p=128)),
                  writes=[wo])
            g1b = [load_bcast(S, st, f"g1b{m}", mods_d[m:m + 1, 2 * DM:3 * DM], modsT) for m in range(2)]
            lngb = load_bcast(S, st, "lngb", ln_g)
            lnbb = load_bcast(S, st, "lnbb", ln_b)
            psY = [S.psum(f"psY{i}", [128, 512], F32, st) for i in range(4)]
            xts = [S.sbuf(f"xts{i}", [128, DM], F32, st) for i in range(2)]
            zs = [S.sbuf(f"zs{i}", [128, DM], F32, st) for i in range(2)]
            x1s = [S.sbuf(f"x1s{i}", [128, DM], F32, st) for i in range(2)]
            lnsc = LNScratch(S, st, "lnc")
            outs = []
            for ti in range(NQ // 128):
                mset = 1 if ti < 2 else 0
                xt = xts[ti % 2]; z = zs[ti % 2]; xo = x1s[ti % 2]
                S.dma('sync', lambda e, xt=xt, ti=ti: e.dma_start(out=xt[:], in_=xtm[ti * 128:(ti + 1) * 128, :]), writes=[xt])
                for nh in range(2):
                    py = psY[(ti % 2) * 2 + nh]
                    for h in range(8):
                        S.op('tensor', lambda e, py=py, h=h, ti=ti, nh=nh: e.matmul(
                            py[:], lhsT=onT[:, h, ti * 128:(ti + 1) * 128], rhs=wo[:, h, nh * 512:(nh + 1) * 512],
                            start=(h == 0), stop=(h == 7)), reads=[onT, wo], writes=[py])
                    S.op('vector', lambda e, py=py, z=z, nh=nh, mset=mset: e.tensor_tensor(
                        out=z[:, nh * 512:(nh + 1) * 512], in0=py[:], in1=g1b[mset][:, nh * 512:(nh + 1) * 512], op=ALU.mult),
                        reads=[py, g1b[mset]], pwrites=[z])
                S.op('vector', lambda e, xt=xt, z=z: e.scalar_tensor_tensor(
                    out=z[:], in0=xt[:], scalar=ALPHA, in1=z[:], op0=ALU.mult, op1=ALU.add), reads=[xt, z], writes=[z])
                emit_ln(S, lnsc, z, xo, lngb, lnbb)
                outs.append(S.dma('sync', lambda e, xo=xo, ti=ti: e.dma_start(out=x1[ti * 128:(ti + 1) * 128, :], in_=xo[:]),
                                  reads=[xo], pwrites=[x1T]))
        S.barrier()


class LNScratch:
    def __init__(self, S, st, pfx):
        self.stats = S.sbuf(pfx + "_st", [128, 2, 6], F32, st)
        self.mv = S.sbuf(pfx + "_mv", [128, 2], F32, st)
        self.rstd = S.sbuf(pfx + "_rs", [128, 1], F32, st)


def emit_ln(S, sc, z, out, lng, lnb, eng2='gpsimd'):
    for i in range(2):
        S.op('vector', lambda e, i=i: e.bn_stats(out=sc.stats[:, i, :], in_=z[:, i * 512:(i + 1) * 512]),
             reads=[z], pwrites=[sc.stats])
    S.op('vector', lambda e: e.bn_aggr(out=sc.mv[:], in_=sc.stats[:].rearrange("p a b -> p (a b)")),
         reads=[sc.stats], writes=[sc.mv])
    S.op('vector', lambda e: e.tensor_scalar_add(out=sc.rstd[:], in0=sc.mv[:, 1:2], scalar1=EPS),
         reads=[sc.mv], writes=[sc.rstd])
    S.op('scalar', lambda e: e.activation(out=sc.rstd[:], in_=sc.rstd[:], func=AF.Sqrt), reads=[sc.rstd], writes=[sc.rstd])
    S.op('vector', lambda e: e.reciprocal(out=sc.rstd[:], in_=sc.rstd[:]), reads=[sc.rstd], writes=[sc.rstd])
    S.op('vector', lambda e: e.tensor_scalar(out=z[:], in0=z[:], scalar1=sc.mv[:, 0:1], scalar2=sc.rstd[:, 0:1],
                                             op0=ALU.subtract, op1=ALU.mult), reads=[z, sc.mv, sc.rstd], writes=[z])
    S.op(eng2, lambda e: e.tensor_tensor(out=z[:], in0=z[:], in1=lng[:], op=ALU.mult),
         reads=[z, lng], writes=[z])
    S.op(eng2, lambda e: e.tensor_tensor(out=out[:], in0=z[:], in1=lnb[:], op=ALU.add),
         reads=[z, lnb], writes=[out])


def _lambda_init(layer_idx):
    import math
    return 0.8 - 0.6 * math.exp(-0.3 * layer_idx)


def _rope_tables(pos, n_ctx):
    pos = np.asarray(pos)
    row = (pos // 64).astype(np.float32); col = (pos % 64).astype(np.float32)
    inv = (np.float32(10000.0) ** (-np.arange(16, dtype=np.float32) / np.float32(16))).astype(np.float32)
    ang = np.concatenate([row[:, None] * inv, col[:, None] * inv], -1).astype(np.float32)
    cos = np.cos(ang).astype(np.float32); sin = np.sin(ang).astype(np.float32)
    C64 = np.concatenate([cos, cos], -1); S64 = np.concatenate([-sin, sin], -1)
    C = np.concatenate([np.ones((n_ctx, 64), np.float32), C64], 0)
    Sg = np.concatenate([np.zeros((n_ctx, 64), np.float32), S64], 0)
    C = np.concatenate([C, C], -1).T; Sg = np.concatenate([Sg, Sg], -1).T
    return np.ascontiguousarray(C), np.ascontiguousarray(Sg)


def _perm_cols():
    idx = np.arange(2 * DM).reshape(2, 8, 2, 64)
    return np.concatenate([idx[..., 32:], idx[..., :32]], -1).reshape(-1)


def prep_attn(inp, core, shared):
    b, hf = divmod(core, 2)
    x = inp['x'][b]; ctx = inp['ctx'][b]
    own = x[hf * NOWN:(hf + 1) * NOWN]; oth = x[(1 - hf) * NOWN:(2 - hf) * NOWN]
    pos_own = np.arange(hf * NOWN, (hf + 1) * NOWN)
    if hf == 1:
        own = own[::-1]; ctx = ctx[::-1]; pos_own = pos_own[::-1]
    pos = np.concatenate([pos_own, np.arange((1 - hf) * NOWN, (2 - hf) * NOWN)])
    C, Sg = _rope_tables(pos, NCTX)
    d = dict(shared)
    d.update(
        xk=np.ascontiguousarray(np.concatenate([ctx, own, oth], 0).T),
        xtm=np.ascontiguousarray(np.concatenate([ctx, own], 0)),
        ropeC=C, ropeS=Sg,
        cT=np.ascontiguousarray(np.stack([inp['c'][b], inp['c_ctx']], 1)),
    )
    return d


def shared_attn(inp):
    w_in = inp['da_w_in'][0]
    return dict(
        ada_w=inp['ada_w'][0], ada_b=inp['ada_b'][0][None, :],
        w_in=w_in, w_perm=np.ascontiguousarray(w_in[:, :2 * DM][:, _perm_cols()]),
        w_out=inp['da_w_out'][0], lamv=inp['da_lambda'][0].reshape(1, 256),
        subln=inp['da_subln_w'][0][None, :], ln_g=inp['ln_g'][0, 0][None, :], ln_b=inp['ln_b'][0, 0][None, :],
        ident=np.eye(128, dtype=np.float32),
    )


def emit_moe(S, nc, xin, xinT, mods_d, modsT, router_w, router_b, w_gu, b_gu, w_down, b_down, ln_g, ln_b,
             ident_d, xout, xoutT, ntiles, nctx_tiles):
    NE = 32
    group = -(-ntiles // 3)
    with ExitStack() as st:
        identb = S.sbuf("mo_identb", [128, 128], BF16, st)
        S.dma('gpsimd', lambda e: e.dma_start(out=identb[:], in_=ident_d[:, :]), writes=[identb])
        identf = S.sbuf("mo_identf", [128, 128], F32, st)
        S.dma('sync', lambda e: e.dma_start(out=identf[:], in_=ident_d[:, :]), writes=[identf])
        nsets = 2 if nctx_tiles else 1
        sc2b = [load_bcast(S, st, f"mo_sc2b{m}", mods_d[m:m + 1, 4 * DM:5 * DM], modsT) for m in range(nsets)]
        sh2b = [load_bcast(S, st, f"mo_sh2b{m}", mods_d[m:m + 1, 3 * DM:4 * DM], modsT) for m in range(nsets)]
        g2b = [load_bcast(S, st, f"mo_g2b{m}", mods_d[m:m + 1, 5 * DM:6 * DM], modsT) for m in range(nsets)]
        for t in sc2b:
            S.op('gpsimd', lambda e, t=t: e.tensor_scalar_add(out=t[:], in0=t[:], scalar1=1.0), reads=[t], writes=[t])
        lngb = load_bcast(S, st, "mo_lngb", ln_g)
        lnbb = load_bcast(S, st, "mo_lnbb", ln_b)
        rw = S.sbuf("mo_rw", [128, 8, NE], BF16, st)
        S.dma('gpsimd', lambda e: e.dma_start(out=rw[:], in_=router_w.rearrange("(kc p) n -> p kc n", p=128)), writes=[rw])
        rbb = S.sbuf("mo_rbb", [128, NE], F32, st)
        S.dma('sync', lambda e: e.dma_start(out=rbb[:], in_=router_b.partition_broadcast(128)), writes=[rbb])
        bdn = S.sbuf("mo_bdn", [NE, DM], F32, st)
        S.dma('sync', lambda e: e.dma_start(out=bdn[:], in_=b_down[:, :]), writes=[bdn])
        bguT = S.sbuf("mo_bguT", [128, 16, NE], F32, st)
        st_tmp = ExitStack()
        bsb = S.sbuf("mo_bsb", [NE, 2 * DM], F32, st_tmp)
        S.dma('sync', lambda e: e.dma_start(out=bsb[:], in_=b_gu[:, :]), writes=[bsb])
        psX = [S.psum(f"mo_psX{i}", [128, 512], F32, st) for i in range(2)]
        psXb = S.psum("mo_psXb", [128, 1024], BF16, st)
        for c in range(16):
            p = psX[c % 2]
            S.op('tensor', lambda e, p=p, c=c: e.transpose(out=p[:, 0:NE], in_=bsb[0:NE, c * 128:(c + 1) * 128],
                                                           identity=identf[0:NE, 0:NE]), reads=[bsb, identf], writes=[p])
            if c < 8:
                S.op('vector', lambda e, p=p, c=c: e.tensor_copy(out=bguT[:, c, :], in_=p[:, 0:NE]), reads=[p], pwrites=[bguT])
            else:
                S.op('vector', lambda e, p=p, c=c: e.tensor_scalar_add(out=bguT[:, c, :], in0=p[:, 0:NE], scalar1=1.0),
                     reads=[p], pwrites=[bguT])
        S.barrier()
        st_tmp.close()
        wgu = S.sbuf("mo_wgu", [128, 8, 2 * DM], BF16, st)
        wdn = S.sbuf("mo_wdn", [128, 8, DM], BF16, st)
        wguT = [T(None) for _ in range(8)]
        wdnT = [T(None) for _ in range(8)]
        uT = S.sbuf("mo_uT", [128, 8, group * 128], BF16, st)
        acc = S.sbuf("mo_acc", [128, group, DM], F32, st)
        accT = [T(None) for _ in range(group)]
        G = S.sbuf("mo_G", [128, group, NE], F32, st)
        GT = S.sbuf("mo_GT", [NE, 128], F32, st)
        xt2 = [S.sbuf(f"mo_xt{i}", [128, DM], F32, st) for i in range(2)]
        ub = [S.sbuf(f"mo_ub{i}", [128, DM], BF16, st) for i in range(2)]
        lg = S.sbuf("mo_lg", [128, NE], F32, st)
        m8 = S.sbuf("mo_m8", [128, 8], F32, st)
        msk = S.sbuf("mo_msk", [128, NE], F32, st)
        ssum = S.sbuf("mo_ssum", [128, 1], F32, st)
        psG = [S.psum(f"mo_psG{i}", [128, 512], F32, st) for i in range(2)]
        psL = [S.psum(f"mo_psL{i}", [128, 512], F32, st) for i in range(2)]
        psY = psX
        gl = [S.sbuf(f"mo_gl{i}", [128, 512], F32, st) for i in range(2)]
        sg = [S.sbuf(f"mo_sg{i}", [128, 512], F32, st) for i in range(2)]
        l1 = [S.sbuf(f"mo_l1{i}", [128, 512], F32, st) for i in range(2)]
        actT = [S.sbuf(f"mo_act{i}", [128, 8, 512], BF16, st) for i in range(2)]
        xo = [S.sbuf(f"mo_xo{i}", [128, DM], F32, st) for i in range(1)]
        lnsc = LNScratch(S, st, "mo_lnc")
        wguv = w_gu.rearrange("e (kc p) n -> e p kc n", p=128)
        wdnv = w_down.rearrange("e (kc p) n -> e p kc n", p=128)
        it = 0
        for g0 in range(0, ntiles, group):
            gt = min(group, ntiles - g0)
            ntok = gt * 128
            for ti in range(gt):
                tg = g0 + ti
                mset = 1 if tg < nctx_tiles else 0
                xt = xt2[ti % 2]; u = ub[ti % 2]
                S.dma('sync', lambda e, xt=xt, tg=tg: e.dma_start(out=xt[:], in_=xin[tg * 128:(tg + 1) * 128, :]),
                      reads=[xinT], writes=[xt])
                S.op('vector', lambda e, xt=xt, mset=mset: e.tensor_tensor(out=xt[:], in0=xt[:], in1=sc2b[mset][:], op=ALU.mult),
                     reads=[xt, sc2b[mset]], writes=[xt])
                S.op('gpsimd', lambda e, xt=xt, u=u, mset=mset: e.tensor_tensor(out=u[:], in0=xt[:], in1=sh2b[mset][:], op=ALU.add),
                     reads=[xt, sh2b[mset]], writes=[u])
                for kc in range(8):
                    S.op('tensor', lambda e, u=u, kc=kc: e.transpose(out=psXb[:, kc * 128:(kc + 1) * 128],
                                                                     in_=u[:, kc * 128:(kc + 1) * 128], identity=identb[:]),
                         reads=[u, identb], pwrites=[psXb])
                S.op('scalar', lambda e, ti=ti: e.copy(out=uT[:, :, ti * 128:(ti + 1) * 128],
                                                       in_=psXb[:].rearrange("p (k t) -> p k t", k=8)),
                     reads=[psXb], pwrites=[uT])
                pr = psX[ti % 2]
                for kc in range(8):
                    S.op('tensor', lambda e, pr=pr, kc=kc, ti=ti: e.matmul(pr[:, 0:NE], lhsT=uT[:, kc, ti * 128:(ti + 1) * 128],
                                                                           rhs=rw[:, kc, :], start=(kc == 0), stop=(kc == 7)),
                         reads=[uT, rw], writes=[pr])
                S.op('vector', lambda e, pr=pr: e.tensor_tensor(out=lg[:], in0=pr[:, 0:NE], in1=rbb[:], op=ALU.add),
                     reads=[pr, rbb], writes=[lg])
                S.op('vector', lambda e: e.max(out=m8[:], in_=lg[:]), reads=[lg], writes=[m8])
                S.op('vector', lambda e: e.tensor_scalar(out=msk[:], in0=lg[:], scalar1=m8[:, 3:4], scalar2=None, op0=ALU.is_ge),
                     reads=[lg, m8], writes=[msk])
                S.op('vector', lambda e: e.tensor_scalar(out=lg[:], in0=lg[:], scalar1=m8[:, 0:1], scalar2=None, op0=ALU.subtract),
                     reads=[lg, m8], writes=[lg])
                S.op('scalar', lambda e: e.activation(out=lg[:], in_=lg[:], func=AF.Exp), reads=[lg], writes=[lg])
                S.op('vector', lambda e: e.tensor_tensor(out=lg[:], in0=lg[:], in1=msk[:], op=ALU.mult), reads=[lg, msk], writes=[lg])
                S.op('vector', lambda e: e.reduce_sum(out=ssum[:], in_=lg[:], axis=AX.X), reads=[lg], writes=[ssum])
                S.op('vector', lambda e: e.reciprocal(out=ssum[:], in_=ssum[:]), reads=[ssum], writes=[ssum])
                S.op('vector', lambda e, ti=ti: e.tensor_scalar_mul(out=G[:, ti, :], in0=lg[:], scalar1=ssum[:, 0:1]),
                     reads=[lg, ssum], pwrites=[G])
                pg = psX[(ti + 1) % 2]
                S.op('tensor', lambda e, pg=pg, ti=ti: e.transpose(out=pg[0:NE, 0:128], in_=G[:, ti, :], identity=identf[:]),
                     reads=[G, identf], writes=[pg])
                S.op('vector', lambda e, pg=pg: e.tensor_copy(out=GT[:], in_=pg[0:NE, 0:128]), reads=[pg], writes=[GT])
                for nh in range(2):
                    pb = psG[nh]
                    S.op('tensor', lambda e, pb=pb, nh=nh: e.matmul(pb[:], lhsT=GT[:], rhs=bdn[:, nh * 512:(nh + 1) * 512],
                                                                    start=True, stop=True), reads=[GT, bdn], writes=[pb])
                    S.op('scalar', lambda e, pb=pb, nh=nh, ti=ti: e.copy(out=acc[:, ti, nh * 512:(nh + 1) * 512], in_=pb[:]),
                         reads=[pb], pwrites=[accT[ti]])
            tblocks = [(t0, min(512, ntok - t0)) for t0 in range(0, ntok, 512)]
            for ex in range(_NEXP):
                for kc in range(8):
                    S.dma('gpsimd', lambda e, ex=ex, kc=kc: e.dma_start(out=wgu[:, kc, :], in_=wguv[ex, :, kc, :]),
                          writes=[wguT[kc]])
                for kc in range(8):
                    S.dma('gpsimd', lambda e, ex=ex, kc=kc: e.dma_start(out=wdn[:, kc, :], in_=wdnv[ex, :, kc, :]),
                          writes=[wdnT[kc]])
                for (t0, nt) in tblocks:
                    A = actT[it % 2]; it += 1
                    for j in range(8):
                        pg = psG[j % 2]; pl = psL[j % 2]
                        for kc in range(8):
                            S.op('tensor', lambda e, pg=pg, kc=kc, j=j, t0=t0, nt=nt: e.matmul(
                                pg[:, :nt], lhsT=wgu[:, kc, j * 128:(j + 1) * 128], rhs=uT[:, kc, t0:t0 + nt],
                                start=(kc == 0), stop=(kc == 7)), reads=[wguT[kc], uT], writes=[pg])
                        for kc in range(8):
                            S.op('tensor', lambda e, pl=pl, kc=kc, j=j, t0=t0, nt=nt: e.matmul(
                                pl[:, :nt], lhsT=wgu[:, kc, DM + j * 128:DM + (j + 1) * 128], rhs=uT[:, kc, t0:t0 + nt],
                                start=(kc == 0), stop=(kc == 7)), reads=[wguT[kc], uT], writes=[pl])
                        g_ = gl[j % 2]; s_ = sg[j % 2]; l_ = l1[j % 2]
                        S.op('vector', lambda e, pg=pg, g_=g_, j=j, ex=ex, nt=nt: e.tensor_scalar(
                            out=g_[:, :nt], in0=pg[:, :nt], scalar1=bguT[:, j, ex:ex + 1], scalar2=7.0, op0=ALU.add, op1=ALU.min),
                            reads=[pg, bguT], writes=[g_])
                        S.op('scalar', lambda e, g_=g_, s_=s_, nt=nt: e.activation(out=s_[:, :nt], in_=g_[:, :nt], func=AF.Sigmoid,
                                                                                   scale=1.702), reads=[g_], writes=[s_])
                        S.op('scalar', lambda e, pl=pl, l_=l_, j=j, ex=ex, nt=nt: e.activation(
                            out=l_[:, :nt], in_=pl[:, :nt], func=AF.Identity, bias=bguT[:, 8 + j, ex:ex + 1]),
                            reads=[pl, bguT], writes=[l_])
                        S.op('vector', lambda e, l_=l_, nt=nt: e.tensor_scalar(out=l_[:, :nt], in0=l_[:, :nt], scalar1=-6.0, scalar2=8.0,
                                                                                op0=ALU.max, op1=ALU.min), reads=[l_], writes=[l_])
                        S.op('vector', lambda e, g_=g_, s_=s_, nt=nt: e.tensor_tensor(out=g_[:, :nt], in0=g_[:, :nt], in1=s_[:, :nt],
                                                                                      op=ALU.mult), reads=[g_, s_], writes=[g_])
                        S.op('vector', lambda e, A=A, g_=g_, l_=l_, j=j, nt=nt: e.tensor_tensor(out=A[:, j, :nt], in0=g_[:, :nt],
                                                                                                  in1=l_[:, :nt], op=ALU.mult),
                             reads=[g_, l_], pwrites=[A])
                    for tt in range(nt // 128):
                        ti = (t0 // 128) + tt
                        for nh in range(2):
                            py = psY[nh]
                            for j in range(8):
                                S.op('tensor', lambda e, py=py, A=A, j=j, tt=tt, nh=nh: e.matmul(
                                    py[:], lhsT=A[:, j, tt * 128:(tt + 1) * 128], rhs=wdn[:, j, nh * 512:(nh + 1) * 512],
                                    start=(j == 0), stop=(j == 7)), reads=[A, wdnT[j]], writes=[py])
                            S.op('vector', lambda e, py=py, ti=ti, nh=nh, ex=ex: e.scalar_tensor_tensor(
                                out=acc[:, ti, nh * 512:(nh + 1) * 512], in0=py[:], scalar=G[:, ti, ex:ex + 1],
                                in1=acc[:, ti, nh * 512:(nh + 1) * 512], op0=ALU.mult, op1=ALU.add),
                                reads=[py, G, accT[ti]], pwrites=[accT[ti]])
            for ti in range(gt):
                tg = g0 + ti
                mset = 1 if tg < nctx_tiles else 0
                xt = xt2[ti % 2]; o = xo[0]
                S.dma('sync', lambda e, xt=xt, tg=tg: e.dma_start(out=xt[:], in_=xin[tg * 128:(tg + 1) * 128, :]),
                      reads=[xinT], writes=[xt])
                S.op('gpsimd', lambda e, ti=ti, mset=mset: e.tensor_tensor(out=acc[:, ti, :], in0=acc[:, ti, :], in1=g2b[mset][:], op=ALU.mult),
                     reads=[accT[ti], g2b[mset]], writes=[accT[ti]])
                S.op('vector', lambda e, xt=xt, ti=ti: e.scalar_tensor_tensor(out=xt[:], in0=xt[:], scalar=ALPHA, in1=acc[:, ti, :],
                                                                             op0=ALU.mult, op1=ALU.add), reads=[xt, accT[ti]], writes=[xt])
                emit_ln(S, lnsc, xt, o, lngb, lnbb)
                S.dma('sync', lambda e, o=o, tg=tg: e.dma_start(out=xout[tg * 128:(tg + 1) * 128, :], in_=o[:]),
                      reads=[o], pwrites=[xoutT])
        S.barrier()


def build_moe(ntiles, nctx_tiles):
    nc = bass.Bass("TRN2", target_bir_lowering=False)
    EI = "ExternalInput"
    NTk = ntiles * 128
    xin = _dram(nc, "xin", [NTk, DM], F32, EI)
    mods_d = _dram(nc, "mods", [2, 6 * DM], F32, EI)
    router_w = _dram(nc, "router_w", [DM, 32], F32, EI)
    router_b = _dram(nc, "router_b", [1, 32], F32, EI)
    w_gu = _dram(nc, "w_gu", [32, DM, 2 * DM], F32, EI)
    b_gu = _dram(nc, "b_gu", [32, 2 * DM], F32, EI)
    w_down = _dram(nc, "w_down", [32, DM, DM], F32, EI)
    b_down = _dram(nc, "b_down", [32, DM], F32, EI)
    ln_g = _dram(nc, "ln_g", [1, DM], F32, EI)
    ln_b = _dram(nc, "ln_b", [1, DM], F32, EI)
    ident_d = _dram(nc, "ident", [128, 128], F32, EI)
    xout = _dram(nc, "xout", [NTk, DM], F32, "ExternalOutput")
    with ExitStack() as st0:
        S = Sched(nc, st0)
        emit_moe(S, nc, xin, T(xin), mods_d, T(mods_d), router_w, router_b, w_gu, b_gu, w_down, b_down, ln_g, ln_b,
                 ident_d, xout, T(xout), ntiles, nctx_tiles)
    return nc


def shared_moe(inp, i):
    return dict(router_w=inp['router_w'][i], router_b=inp['router_b'][i][None, :], w_gu=inp['moe_w_gu'][i],
                b_gu=inp['moe_b_gu'][i], w_down=inp['moe_w_down'][i], b_down=inp['moe_b_down'][i],
                ln_g=inp['ln_g'][i, 1][None, :], ln_b=inp['ln_b'][i, 1][None, :], ident=np.eye(128, dtype=np.float32))


_NEXP = 32
NU = NQ // 128
GIN = 3104


class GlaScratch:
    def __init__(self, nc, kind="Internal", sfx=""):
        self.qT = _dram(nc, "g_qT" + sfx, [NU, 128, 4, 128], F32, kind)
        self.kT = _dram(nc, "g_kT" + sfx, [NU, 128, 4, 128], F32, kind)
        self.k = _dram(nc, "g_k" + sfx, [NQ, 512], F32, kind)
        self.v = _dram(nc, "g_v" + sfx, [NQ, DM], BF16, kind)
        self.LgA = _dram(nc, "g_LgA" + sfx, [NQ, 512], F32, kind)
        self.LgB = _dram(nc, "g_LgB" + sfx, [NQ, 512], F32, kind)
        self.r = _dram(nc, "g_r" + sfx, [NQ, DM], F32, kind)
        self.T = {n: T(getattr(self, n), n) for n in ('qT', 'kT', 'k', 'v', 'LgA', 'LgB', 'r')}


def emit_gla_proj(S, nc, xin, xinT, mods_d, modsT, w_in, wgA, wgB, ident_d, G):
    with ExitStack() as st:
        identb = S.sbuf("gp_identb", [128, 128], BF16, st)
        S.dma('gpsimd', lambda e: e.dma_start(out=identb[:], in_=ident_d[:, :]), writes=[identb])
        sc1b = [load_bcast(S, st, f"gp_sc1b{m}", mods_d[m:m + 1, DM:2 * DM], modsT) for m in range(2)]
        sh1b = [load_bcast(S, st, f"gp_sh1b{m}", mods_d[m:m + 1, 0:DM], modsT) for m in range(2)]
        for t in sc1b:
            S.op('gpsimd', lambda e, t=t: e.tensor_scalar_add(out=t[:], in0=t[:], scalar1=1.0), reads=[t], writes=[t])
        wi = S.sbuf("gp_wi", [128, 8, GIN], BF16, st)
        wiv = w_in.rearrange("(kc p) n -> p kc n", p=128)
        for kc in range(8):
            for c0, c1 in ((0, 1024), (1024, 2048), (2048, GIN)):
                S.dma('gpsimd', lambda e, kc=kc, c0=c0, c1=c1: e.dma_start(out=wi[:, kc, c0:c1], in_=wiv[:, kc, c0:c1]), pwrites=[wi])
        wg = []
        for nm, src in (("A", wgA), ("B", wgB)):
            t = S.sbuf("gp_wg" + nm, [17, 512], F32, st)
            S.dma('sync', lambda e, t=t, src=src: e.dma_start(out=t[:], in_=src[:, :]), writes=[t])
            wg.append(t)
        zaug = [S.sbuf(f"gp_zaug{i}", [32, 512], F32, st) for i in range(2)]
        for z in zaug:
            S.op('vector', lambda e, z=z: e.memset(z[:], 1.0), writes=[z])
        xts = [S.sbuf(f"gp_xt{i}", [128, DM], F32, st) for i in range(2)]
        tbf = [S.sbuf(f"gp_tbf{i}", [128, DM], BF16, st) for i in range(2)]
        tT = [S.sbuf(f"gp_tT{i}", [128, 8, 512], BF16, st) for i in range(2)]
        psXb = S.psum("gp_psXb", [128, 1024], BF16, st)
        psF = [S.psum(f"gp_psF{i}", [128, 512], F32, st) for i in range(2)]
        psZ = S.psum("gp_psZ", [128, 512], F32, st)
        psK = [S.psum(f"gp_psK{i}", [128, 512], F32, st) for i in range(3)]
        fst = [S.sbuf(f"gp_fst{i}", [128, 512], F32, st) for i in range(3)]
        kst = [S.sbuf(f"gp_kst{i}", [128, 512], F32, st) for i in range(2)]
        vst = [S.sbuf(f"gp_vst{i}", [128, DM], BF16, st) for i in range(2)]
        rst = [S.sbuf(f"gp_rst{i}", [128, DM], F32, st) for i in range(2)]
        gex = [S.sbuf(f"gp_gex{i}", [128, 512], F32, st) for i in range(2)]
        gst = [S.sbuf(f"gp_gst{i}", [128, 512], F32, st) for i in range(2)]
        blocks = [(0, NCTX)] + [(NCTX + i * 512, 512) for i in range(8)]
        iF = 0; iK = 0; ig = 0
        for bi, (t0, nt) in enumerate(blocks):
            TT = tT[bi % 2]
            mset = 1 if bi == 0 else 0
            ntl = nt // 128
            for ti in range(ntl):
                r0 = t0 + ti * 128
                xt = xts[ti % 2]; tb = tbf[ti % 2]
                S.dma('sync', lambda e, xt=xt, r0=r0: e.dma_start(out=xt[:], in_=xin[r0:r0 + 128, :]), reads=[xinT], writes=[xt])
                S.op('vector', lambda e, xt=xt, mset=mset: e.tensor_tensor(out=xt[:], in0=xt[:], in1=sc1b[mset][:], op=ALU.mult),
                     reads=[xt, sc1b[mset]], writes=[xt])
                S.op('gpsimd', lambda e, xt=xt, tb=tb, mset=mset: e.tensor_tensor(out=tb[:], in0=xt[:], in1=sh1b[mset][:], op=ALU.add),
                     reads=[xt, sh1b[mset]], writes=[tb])
                for kc in range(8):
                    S.op('tensor', lambda e, tb=tb, kc=kc: e.transpose(out=psXb[:, kc * 128:(kc + 1) * 128],
                                                                       in_=tb[:, kc * 128:(kc + 1) * 128], identity=identb[:]),
                         reads=[tb, identb], pwrites=[psXb])
                S.op('scalar', lambda e, TT=TT, ti=ti: e.copy(out=TT[:, :, ti * 128:(ti + 1) * 128],
                                                              in_=psXb[:].rearrange("p (k t) -> p k t", k=8)),
                     reads=[psXb], pwrites=[TT])
            n0 = t0 // 128
            for kind in ('q', 'k'):
                for h in range(4):
                    c0 = (0 if kind == 'q' else 512) + h * 128
                    pf = psF[iF % 2]; fs = fst[iF % 3]; iF += 1
                    for kc in range(8):
                        S.op('tensor', lambda e, pf=pf, TT=TT, kc=kc, c0=c0, nt=nt: e.matmul(
                            pf[:, :nt], lhsT=wi[:, kc, c0:c0 + 128], rhs=TT[:, kc, :nt], start=(kc == 0), stop=(kc == 7)),
                            reads=[wi, TT], writes=[pf])
                    S.op('scalar', lambda e, pf=pf, fs=fs, nt=nt, kind=kind: e.activation(
                        out=fs[:, :nt], in_=pf[:, :nt], func=AF.Copy, scale=(128.0 ** -0.5 if kind == 'q' else 1.0)),
                        reads=[pf], writes=[fs])
                    dst = G.qT if kind == 'q' else G.kT
                    S.dma('sync', lambda e, fs=fs, dst=dst, n0=n0, ntl=ntl, h=h, nt=nt: e.dma_start(
                        out=dst[n0:n0 + ntl, :, h, :].rearrange("n p t -> p n t"),
                        in_=fs[:, :nt].rearrange("p (n t) -> p n t", t=128)),
                        reads=[fs], pwrites=[G.T['qT' if kind == 'q' else 'kT']])
            for d in range(2):
                for kc in range(8):
                    S.op('tensor', lambda e, kc=kc, d=d, nt=nt: e.matmul(
                        psZ[0:16, :nt], lhsT=wi[:, kc, 3072 + d * 16:3072 + (d + 1) * 16], rhs=TT[:, kc, :nt],
                        start=(kc == 0), stop=(kc == 7)), reads=[wi, TT], writes=[psZ])
                S.op('vector', lambda e, d=d, nt=nt: e.tensor_copy(out=zaug[d][0:16, :nt], in_=psZ[0:16, :nt]),
                     reads=[psZ], pwrites=[zaug[d]])
            for ti in range(ntl):
                r0 = t0 + ti * 128
                tsl = slice(ti * 128, (ti + 1) * 128)
                pk = psK[iK % 3]; iK += 1
                ks = kst[ti % 2]
                for kc in range(8):
                    S.op('tensor', lambda e, pk=pk, kc=kc: e.matmul(pk[:], lhsT=TT[:, kc, tsl], rhs=wi[:, kc, 512:1024],
                                                                    start=(kc == 0), stop=(kc == 7)), reads=[wi, TT], writes=[pk])
                S.op('scalar', lambda e, pk=pk, ks=ks: e.copy(out=ks[:], in_=pk[:]), reads=[pk], writes=[ks])
                S.dma('sync', lambda e, ks=ks, r0=r0: e.dma_start(out=G.k[r0:r0 + 128, :], in_=ks[:]), reads=[ks], pwrites=[G.T['k']])
                vs_ = vst[ti % 2]; rs_ = rst[ti % 2]
                for which, c00 in (('v', 1024), ('r', 2048)):
                    if which == 'r' and bi == 0:
                        continue
                    for nh in range(2):
                        pk = psK[iK % 3]; iK += 1
                        for kc in range(8):
                            S.op('tensor', lambda e, pk=pk, kc=kc, c00=c00, nh=nh: e.matmul(
                                pk[:], lhsT=TT[:, kc, tsl], rhs=wi[:, kc, c00 + nh * 512:c00 + (nh + 1) * 512],
                                start=(kc == 0), stop=(kc == 7)), reads=[wi, TT], writes=[pk])
                        dstt = vs_ if which == 'v' else rs_
                        eng = 'vector' if which == 'v' else 'scalar'
                        if eng == 'vector':
                            S.op('vector', lambda e, pk=pk, dstt=dstt, nh=nh: e.tensor_copy(out=dstt[:, nh * 512:(nh + 1) * 512], in_=pk[:]),
                                 reads=[pk], pwrites=[dstt])
                        else:
                            S.op('scalar', lambda e, pk=pk, dstt=dstt, nh=nh: e.copy(out=dstt[:, nh * 512:(nh + 1) * 512], in_=pk[:]),
                                 reads=[pk], pwrites=[dstt])
                S.dma('sync', lambda e, vs_=vs_, r0=r0: e.dma_start(out=G.v[r0:r0 + 128, :], in_=vs_[:]), reads=[vs_], pwrites=[G.T['v']])
                if bi != 0:
                    S.dma('sync', lambda e, rs_=rs_, r0=r0: e.dma_start(out=G.r[r0:r0 + 128, :], in_=rs_[:]), reads=[rs_], pwrites=[G.T['r']])
                for d in range(2):
                    pk = psK[iK % 3]; iK += 1
                    ge = gex[ig % 2]; gs = gst[ig % 2]; ig += 1
                    S.op('tensor', lambda e, pk=pk, d=d: e.matmul(pk[:], lhsT=zaug[d][0:17, tsl], rhs=wg[d][0:17, :],
                                                                  start=True, stop=True), reads=[zaug[d], wg[d]], writes=[pk])
                    S.op('scalar', lambda e, pk=pk, ge=ge: e.activation(out=ge[:], in_=pk[:], func=AF.Exp, scale=-1.0),
                         reads=[pk], writes=[ge])
                    S.op('scalar', lambda e, ge=ge, gs=gs: e.activation(out=gs[:], in_=ge[:], func=AF.Ln, bias=1.0),
                         reads=[ge], writes=[gs])
                    dst = G.LgA if d == 0 else G.LgB
                    S.dma('sync', lambda e, gs=gs, dst=dst, r0=r0: e.dma_start(out=dst[r0:r0 + 128, :], in_=gs[:]),
                          reads=[gs], pwrites=[G.T['LgA' if d == 0 else 'LgB']])
        S.barrier()


def emit_gla_scan(S, nc, G, direction, cmats, S_init, S_final, oA, oAT, post=None):
    A = direction == 'A'
    Lg = G.LgA if A else G.LgB
    LgT = G.T['LgA' if A else 'LgB']
    col = 127 if A else 0
    with ExitStack() as st:
        cm = {}
        for nm in ('MinclT', 'MafterT', 'maskT'):
            t = S.sbuf("gs_" + nm, [128, 128], F32, st)
            S.dma('sync', lambda e, t=t, nm=nm: e.dma_start(out=t[:], in_=cmats[nm][:, :]), writes=[t])
            cm[nm] = t
        Sf = S.sbuf("gs_Sf", [128, 4, 256], F32, st)
        Sb = S.sbuf("gs_Sb", [128, 4, 256], BF16, st)
        SfT = [T(None) for _ in range(4)]; SbT = [T(None) for _ in range(4)]
        if S_init is None:
            S.op('vector', lambda e: e.memset(Sf[:], 0.0), writes=SfT)
        else:
            S.dma('sync', lambda e: e.dma_start(out=Sf[:], in_=S_init[:, :, :]), writes=SfT)
        for h in range(4):
            S.op('gpsimd', lambda e, h=h: e.tensor_copy(out=Sb[:, h, :], in_=Sf[:, h, :]), reads=[SfT[h]], writes=[SbT[h]])
        NB = 3
        qTu = [S.sbuf(f"gs_qT{i}", [128, 4, 128], F32, st) for i in range(NB)]
        kTu = [S.sbuf(f"gs_kT{i}", [128, 4, 128], F32, st) for i in range(NB)]
        ku = [S.sbuf(f"gs_k{i}", [128, 512], F32, st) for i in range(NB)]
        vu = [S.sbuf(f"gs_v{i}", [128, DM], BF16, st) for i in range(NB)]
        Lgu = [S.sbuf(f"gs_Lg{i}", [128, 512], F32, st) for i in range(NB)]
        class _V:
            def __init__(self, bankT, ap):
                self.ap = ap; self.bt = bankT

            def __getitem__(self, k):
                return self.ap[k]
        nbank = 5 if post is not None else 7
        bank = [S.psum(f"gs_bank{i}", [128, 512], F32, st) for i in range(nbank)]
        psB = [_V(bank[0], bank[0].t[:, h * 128:(h + 1) * 128]) for h in range(4)]
        psW = [_V(bank[1], bank[1].t[:, h * 128:(h + 1) * 128]) for h in range(4)]
        psA = [_V(bank[2], bank[2].t[:, h * 128:(h + 1) * 128]) for h in range(4)]
        if post is not None:
            psO = [_V(bank[3], bank[3].t[:, i * 256:(i + 1) * 256]) for i in range(2)]
            psD = [_V(bank[4], bank[4].t[:, i * 256:(i + 1) * 256]) for i in range(2)]
        else:
            psO = [_V(bank[3 + i], bank[3 + i].t[:, 0:256]) for i in range(2)]
            psD = [_V(bank[5 + i], bank[5 + i].t[:, 0:256]) for i in range(2)]
        Eq = [S.sbuf(f"gs_Eq{h}", [128, 128], F32, st) for h in range(4)]
        Ek = [S.sbuf(f"gs_Ek{h}", [128, 128], F32, st) for h in range(4)]
        Ew = [S.sbuf(f"gs_Ew{h}", [128, 128], F32, st) for h in range(4)]
        qin = [S.sbuf(f"gs_qin{h}", [128, 128], BF16, st) for h in range(4)]
        kin = [S.sbuf(f"gs_kin{h}", [128, 128], BF16, st) for h in range(4)]
        kst = [S.sbuf(f"gs_kst{h}", [128, 128], BF16, st) for h in range(4)]
        atm = [S.sbuf(f"gs_atm{h}", [128, 128], BF16, st) for h in range(4)]
        ou = [S.sbuf(f"gs_ou{i}", [128, DM], F32, st) for i in range(2)]
        if post is not None:
            P = post
            identb = S.sbuf("go_identb", [128, 128], BF16, st)
            S.dma('gpsimd', lambda e: e.dma_start(out=identb[:], in_=P['ident'][:, :]), writes=[identb])
            wo = S.sbuf("go_wo", [128, 8, DM], BF16, st)
            S.dma('gpsimd', lambda e: e.dma_start(out=wo[:], in_=P['w_out'].rearrange("(kc p) n -> p kc n", p=128)), writes=[wo])
            g1b = load_bcast(S, st, "go_g1b", P['mods_d'][0:1, 2 * DM:3 * DM], P['modsT'])
            lngb = load_bcast(S, st, "go_lngb", P['ln_g'])
            lnbb = load_bcast(S, st, "go_lnbb", P['ln_b'])
            nwb = S.sbuf("go_nwb", [128, 256], F32, st)
            S.dma('sync', lambda e: e.dma_start(out=nwb[:], in_=P['norm_w'].partition_broadcast(128)), writes=[nwb])
            oAu = [S.sbuf(f"go_oA{i}", [128, DM], F32, st) for i in range(NB)]
            ru = [S.sbuf(f"go_r{i}", [128, DM], F32, st) for i in range(NB)]
            xu = [S.sbuf(f"go_x{i}", [128, DM], F32, st) for i in range(NB)]
            sqt = S.sbuf("go_sq", [128, DM], F32, st)
            ms = S.sbuf("go_ms", [128, 4], F32, st)
            onb = S.sbuf("go_onb", [128, DM], BF16, st)
            onT = S.sbuf("go_onT", [128, 8, 128], BF16, st)
            psXb = S.psum("go_psXb", [128, 1024], BF16, st)
            psY = [S.psum(f"go_psY{i}", [128, 512], F32, st) for i in range(2)]
            xo = S.sbuf("go_xo", [128, DM], F32, st)
            lnsc = LNScratch(S, st, "go_lnc")
        units = list(range(NU)) if A else list(range(NU - 1, 1, -1))

        def load(i, n):
            r0 = n * 128
            S.dma('sync', lambda e: e.dma_start(out=kTu[i][:], in_=G.kT[n]), reads=[G.T['kT']], writes=[kTu[i]])
            S.dma('sync', lambda e: e.dma_start(out=ku[i][:], in_=G.k[r0:r0 + 128, :]), reads=[G.T['k']], writes=[ku[i]])
            S.dma('sync', lambda e: e.dma_start(out=vu[i][:], in_=G.v[r0:r0 + 128, :]), reads=[G.T['v']], writes=[vu[i]])
            S.dma('sync', lambda e: e.dma_start(out=Lgu[i][:], in_=Lg[r0:r0 + 128, :]), reads=[LgT], writes=[Lgu[i]])
            if n >= 2:
                S.dma('sync', lambda e: e.dma_start(out=qTu[i][:], in_=G.qT[n]), reads=[G.T['qT']], writes=[qTu[i]])
                if post is not None:
                    j = i
                    S.dma('sync', lambda e: e.dma_start(out=oAu[j][:], in_=oA[r0 - NCTX:r0 - NCTX + 128, :]), reads=[oAT], writes=[oAu[j]])
                    S.dma('sync', lambda e: e.dma_start(out=ru[j][:], in_=G.r[r0:r0 + 128, :]), reads=[G.T['r']], writes=[ru[j]])
                    S.dma('sync', lambda e: e.dma_start(out=xu[j][:], in_=P['xin'][r0:r0 + 128, :]), reads=[P['xinT']], writes=[xu[j]])

        load(0, units[0])
        for ui, n in enumerate(units):
            i = ui % NB
            if ui + 1 < len(units):
                load((ui + 1) % NB, units[ui + 1])
            full = n >= 2
            O = ou[ui % 2]
            HS = [slice(h * 128, (h + 1) * 128) for h in range(4)]
            for h in range(4):
                S.op('tensor', lambda e: e.matmul(psB[h][:], lhsT=Lgu[i][:, HS[h]], rhs=cm['MinclT'][:], start=True, stop=True),
                     reads=[Lgu[i], cm['MinclT']], pwrites=[psB[h].bt])
                S.op('tensor', lambda e: e.matmul(psW[h][:], lhsT=cm['MafterT'][:], rhs=Lgu[i][:, HS[h]], start=True, stop=True),
                     reads=[Lgu[i], cm['MafterT']], pwrites=[psW[h].bt])
            for h in range(4):
                S.op('scalar', lambda e: e.activation(out=Eq[h][:], in_=psB[h][:], func=AF.Exp), reads=[psB[h].bt], writes=[Eq[h]])
                if full:
                    S.op('scalar', lambda e: e.activation(out=Ek[h][:], in_=psB[h][:], func=AF.Exp, scale=-1.0),
                         reads=[psB[h].bt], writes=[Ek[h]])
                S.op('scalar', lambda e: e.activation(out=Ew[h][:], in_=psW[h][:], func=AF.Exp), reads=[psW[h].bt], writes=[Ew[h]])
            for h in range(4):
                if full:
                    S.op('vector', lambda e: e.tensor_tensor(out=qin[h][:], in0=qTu[i][:, h, :], in1=Eq[h][:], op=ALU.mult),
                         reads=[qTu[i], Eq[h]], writes=[qin[h]])
                    S.op('gpsimd', lambda e: e.tensor_tensor(out=kin[h][:], in0=kTu[i][:, h, :], in1=Ek[h][:], op=ALU.mult),
                         reads=[kTu[i], Ek[h]], writes=[kin[h]])
                S.op('vector' if h % 2 else 'gpsimd', lambda e: e.tensor_tensor(out=kst[h][:], in0=ku[i][:, HS[h]], in1=Ew[h][:], op=ALU.mult),
                     reads=[ku[i], Ew[h]], writes=[kst[h]])
            if full:
                for h in range(4):
                    S.op('tensor', lambda e: e.matmul(psA[h][:], lhsT=kin[h][:], rhs=qin[h][:], start=True, stop=True),
                         reads=[kin[h], qin[h]], pwrites=[psA[h].bt])
                for h in range(4):
                    S.op('vector', lambda e: e.tensor_tensor(out=atm[h][:], in0=psA[h][:], in1=cm['maskT'][:], op=ALU.mult),
                         reads=[psA[h].bt, cm['maskT']], writes=[atm[h]])
            for h in range(4):
                pd = psD[h % 2]
                S.op('tensor', lambda e: e.matmul(pd[:], lhsT=kst[h][:], rhs=vu[i][:, h * 256:(h + 1) * 256], start=True, stop=True),
                     reads=[kst[h], vu[i]], writes=[pd.bt])
                if full:
                    po = psO[h % 2]
                    S.op('tensor', lambda e: e.matmul(po[:], lhsT=atm[h][:], rhs=vu[i][:, h * 256:(h + 1) * 256], start=True, stop=False),
                         reads=[atm[h], vu[i]], writes=[po.bt])
                    S.op('tensor', lambda e: e.matmul(po[:], lhsT=qin[h][:], rhs=Sb[:, h, :], start=False, stop=True),
                         reads=[qin[h], SbT[h]], writes=[po.bt])
                    if post is None:
                        S.op('scalar', lambda e: e.copy(out=O[:, h * 256:(h + 1) * 256], in_=po[:]), reads=[po.bt], pwrites=[O])
                    else:
                        S.op('vector', lambda e: e.tensor_tensor(out=O[:, h * 256:(h + 1) * 256], in0=po[:],
                                                                 in1=oAu[i][:, h * 256:(h + 1) * 256], op=ALU.add),
                             reads=[po.bt, oAu[i]], pwrites=[O])
                S.op('vector', lambda e: e.scalar_tensor_tensor(out=Sf[:, h, :], in0=Sf[:, h, :], scalar=Eq[h][:, col:col + 1],
                                                                in1=pd[:], op0=ALU.mult, op1=ALU.add),
                     reads=[SfT[h], Eq[h], pd.bt], writes=[SfT[h]])
                S.op('gpsimd', lambda e: e.tensor_copy(out=Sb[:, h, :], in_=Sf[:, h, :]), reads=[SfT[h]], writes=[SbT[h]])
            if not full:
                continue
            r0 = n * 128
            if post is None:
                S.dma('sync', lambda e: e.dma_start(out=oA[r0 - NCTX:r0 - NCTX + 128, :], in_=O[:]), reads=[O], pwrites=[oAT])
                continue
            j = i
            S.op('gpsimd', lambda e: e.tensor_tensor(out=sqt[:], in0=O[:], in1=O[:], op=ALU.mult), reads=[O], writes=[sqt])
            S.op('vector', lambda e: e.reduce_sum(out=ms[:], in_=sqt[:].rearrange("p (h d) -> p h d", h=4), axis=AX.X),
                 reads=[sqt], writes=[ms])
            S.op('vector', lambda e: e.tensor_scalar(out=ms[:], in0=ms[:], scalar1=1.0 / 256, scalar2=EPS, op0=ALU.mult, op1=ALU.add),
                 reads=[ms], writes=[ms])
            S.op('scalar', lambda e: e.activation(out=ms[:], in_=ms[:], func=AF.Sqrt), reads=[ms], writes=[ms])
            S.op('vector', lambda e: e.reciprocal(out=ms[:], in_=ms[:]), reads=[ms], writes=[ms])
            for h in range(4):
                S.op('vector', lambda e: e.scalar_tensor_tensor(out=O[:, h * 256:(h + 1) * 256], in0=O[:, h * 256:(h + 1) * 256],
                                                                scalar=ms[:, h:h + 1], in1=nwb[:], op0=ALU.mult, op1=ALU.mult),
                     reads=[O, ms, nwb], writes=[O])
            S.op('scalar', lambda e: e.activation(out=sqt[:], in_=ru[j][:], func=AF.Silu), reads=[ru[j]], writes=[sqt])
            S.op('gpsimd', lambda e: e.tensor_tensor(out=onb[:], in0=O[:], in1=sqt[:], op=ALU.mult), reads=[O, sqt], writes=[onb])
            for kc in range(8):
                S.op('tensor', lambda e: e.transpose(out=psXb[:, kc * 128:(kc + 1) * 128], in_=onb[:, kc * 128:(kc + 1) * 128],
                                                     identity=identb[:]), reads=[onb, identb], pwrites=[psXb])
            S.op('scalar', lambda e: e.copy(out=onT[:], in_=psXb[:].rearrange("p (k t) -> p k t", k=8)), reads=[psXb], writes=[onT])
            for nh in range(2):
                py = psY[nh]
                for kc in range(8):
                    S.op('tensor', lambda e: e.matmul(py[:], lhsT=onT[:, kc, :], rhs=wo[:, kc, nh * 512:(nh + 1) * 512],
                                                      start=(kc == 0), stop=(kc == 7)), reads=[onT, wo], writes=[py])
                S.op('vector', lambda e: e.tensor_tensor(out=sqt[:, nh * 512:(nh + 1) * 512], in0=py[:],
                                                         in1=g1b[:, nh * 512:(nh + 1) * 512], op=ALU.mult),
                     reads=[py, g1b], pwrites=[sqt])
            S.op('vector', lambda e: e.scalar_tensor_tensor(out=sqt[:], in0=xu[j][:], scalar=ALPHA, in1=sqt[:],
                                                            op0=ALU.mult, op1=ALU.add), reads=[xu[j], sqt], writes=[sqt])
            emit_ln(S, lnsc, sqt, xo, lngb, lnbb)
            S.dma('sync', lambda e: e.dma_start(out=P['xout'][r0 - NCTX:r0 - NCTX + 128, :], in_=xo[:]),
                  reads=[xo], pwrites=[P['xoutT']])
        if S_final is not None:
            S.dma('sync', lambda e: e.dma_start(out=S_final[:, :, :], in_=Sf[:]), reads=SfT)
        S.barrier()


def _gla_common_inputs(nc):
    EI = "ExternalInput"
    d = dict(
        xin=_dram(nc, "xin", [NQ, DM], F32, EI),
        w_in=_dram(nc, "w_in", [DM, GIN], F32, EI),
        wgA=_dram(nc, "wgA", [17, 512], F32, EI),
        wgB=_dram(nc, "wgB", [17, 512], F32, EI),
        ident=_dram(nc, "ident", [128, 128], F32, EI),
    )
    return d


def _cmats(nc, sfx):
    return {nm: _dram(nc, nm + sfx, [128, 128], F32, "ExternalInput") for nm in ('MinclT', 'MafterT', 'maskT')}


def build_gla1():
    nc = bass.Bass("TRN2", target_bir_lowering=False)
    EI = "ExternalInput"
    I = _gla_common_inputs(nc)
    cT = _dram(nc, "cT", [DM, 2], F32, EI)
    ada_w = _dram(nc, "ada_w", [DM, 6 * DM], F32, EI)
    ada_b = _dram(nc, "ada_b", [1, 6 * DM], F32, EI)
    cmA = _cmats(nc, "A")
    mods_d = _dram(nc, "mods", [2, 6 * DM], F32, "ExternalOutput")
    oA = _dram(nc, "oA", [NOWN, DM], F32, "ExternalOutput")
    SA = _dram(nc, "SA", [128, 4, 256], F32, "ExternalOutput")
    G = GlaScratch(nc)
    with ExitStack() as st0:
        S = Sched(nc, st0)
        modsT = T(mods_d)
        with ExitStack() as st:
            emit_mods(S, nc, st, cT, ada_w, ada_b, mods_d, modsT)
            S.barrier()
        xinT = T(I['xin'])
        emit_gla_proj(S, nc, I['xin'], xinT, mods_d, modsT, I['w_in'], I['wgA'], I['wgB'], I['ident'], G)
        emit_gla_scan(S, nc, G, 'A', cmA, None, SA, oA, T(oA))
    return nc


def build_gla2():
    nc = bass.Bass("TRN2", target_bir_lowering=False)
    EI = "ExternalInput"
    I = _gla_common_inputs(nc)
    mods_d = _dram(nc, "mods", [2, 6 * DM], F32, EI)
    cmB = _cmats(nc, "B")
    oA = _dram(nc, "oA", [NOWN, DM], F32, EI)
    SB0 = _dram(nc, "SB0", [128, 4, 256], F32, EI)
    w_out = _dram(nc, "w_out", [DM, DM], F32, EI)
    norm_w = _dram(nc, "norm_w", [1, 256], F32, EI)
    ln_g = _dram(nc, "ln_g", [1, DM], F32, EI)
    ln_b = _dram(nc, "ln_b", [1, DM], F32, EI)
    xout = _dram(nc, "xout", [NOWN, DM], F32, "ExternalOutput")
    G = GlaScratch(nc)
    with ExitStack() as st0:
        S = Sched(nc, st0)
        modsT = T(mods_d); xinT = T(I['xin'])
        emit_gla_proj(S, nc, I['xin'], xinT, mods_d, modsT, I['w_in'], I['wgA'], I['wgB'], I['ident'], G)
        post = dict(ident=I['ident'], w_out=w_out, mods_d=mods_d, modsT=modsT, ln_g=ln_g, ln_b=ln_b, norm_w=norm_w,
                    xin=I['xin'], xinT=xinT, xout=xout, xoutT=T(xout))
        emit_gla_scan(S, nc, G, 'B', cmB, SB0, None, oA, T(oA), post=post)
    return nc


def _gla_cmats():
    s = np.arange(128)[:, None]; t = np.arange(128)[None, :]
    c = np.float32(-1.0 / 16.0)
    return dict(
        MinclTA=(s <= t) * c, MafterTA=(s > t) * c, maskTA=(s <= t) * np.float32(1),
        MinclTB=(s >= t) * c, MafterTB=(s < t) * c, maskTB=(s >= t) * np.float32(1),
    )


def shared_gla(inp):
    cm = {k: np.ascontiguousarray(v.astype(np.float32)) for k, v in _gla_cmats().items()}
    w = inp['gla_w_in'][0]
    wsw = np.ascontiguousarray(np.concatenate([w[:, :3072], w[:, 3088:3104], w[:, 3072:3088]], 1))
    wg = inp['gla_w_gate'][0]; bg = inp['gla_b_gate'][0]
    aug = [np.ascontiguousarray(np.concatenate([wg[d], bg[d][None, :]], 0)) for d in range(2)]
    return dict(cm=cm, w_in=[w, wsw], aug=aug, ada_w=inp['ada_w'][1], ada_b=inp['ada_b'][1][None, :],
                w_out=inp['gla_w_out'][0], norm_w=inp['gla_norm_w'][0][None, :],
                ln_g=inp['ln_g'][1, 0][None, :], ln_b=inp['ln_b'][1, 0][None, :], ident=np.eye(128, dtype=np.float32))


def prep_gla_common(sh, hf, xin):
    return dict(xin=xin, w_in=sh['w_in'][hf], wgA=sh['aug'][hf], wgB=sh['aug'][1 - hf], ident=sh['ident'])


def _moe_inputs(nc, p):
    EI = "ExternalInput"
    return dict(router_w=_dram(nc, p + "router_w", [DM, 32], F32, EI), router_b=_dram(nc, p + "router_b", [1, 32], F32, EI),
                w_gu=_dram(nc, p + "w_gu", [32, DM, 2 * DM], F32, EI), b_gu=_dram(nc, p + "b_gu", [32, 2 * DM], F32, EI),
                w_down=_dram(nc, p + "w_down", [32, DM, DM], F32, EI), b_down=_dram(nc, p + "b_down", [32, DM], F32, EI),
                ln_g=_dram(nc, p + "ln_g", [1, DM], F32, EI), ln_b=_dram(nc, p + "ln_b", [1, DM], F32, EI))


def build_fused():
    nc = bass.Bass("TRN2", target_bir_lowering=False)
    EI = "ExternalInput"
    A = attn_inputs(nc)
    M0 = _moe_inputs(nc, "m0_"); M1 = _moe_inputs(nc, "m1_")
    g_w_in = _dram(nc, "g_w_in", [DM, GIN], F32, EI)
    wgA = _dram(nc, "wgA", [17, 512], F32, EI); wgB = _dram(nc, "wgB", [17, 512], F32, EI)
    ada_w1 = _dram(nc, "ada_w1", [DM, 6 * DM], F32, EI); ada_b1 = _dram(nc, "ada_b1", [1, 6 * DM], F32, EI)
    cmA = _cmats(nc, "A"); cmB = _cmats(nc, "B")
    g_w_out = _dram(nc, "g_w_out", [DM, DM], F32, EI)
    norm_w = _dram(nc, "norm_w", [1, 256], F32, EI)
    g_ln_g = _dram(nc, "g_ln_g", [1, DM], F32, EI); g_ln_b = _dram(nc, "g_ln_b", [1, DM], F32, EI)
    psel = _dram(nc, "psel", [128, 8], F32, EI)
    out = _dram(nc, "out", [NOWN, DM], F32, "ExternalOutput")
    x1 = _dram(nc, "x1", [NQ, DM], F32); mods0 = _dram(nc, "mods0", [2, 6 * DM], F32)
    x2 = _dram(nc, "x2", [NQ, DM], F32); mods1 = _dram(nc, "mods1", [2, 6 * DM], F32)
    oA = _dram(nc, "oA", [NOWN, DM], F32); SA = _dram(nc, "SA", [128, DM], F32)
    SG = _dram(nc, "SG", [8 * 128, DM], F32); SB0 = _dram(nc, "SB0", [128, DM], F32)
    x3 = _dram(nc, "x3", [NOWN, DM], F32)
    with ExitStack() as st0:
        S = Sched(nc, st0)
        x1T = T(x1); m0T = T(mods0); x2T = T(x2); m1T = T(mods1); oAT = T(oA); x3T = T(x3)
        emit_attn(S, nc, A, _lambda_init(0), x1, x1T, mods0, m0T)
        emit_moe(S, nc, x1, x1T, mods0, m0T, M0['router_w'], M0['router_b'], M0['w_gu'], M0['b_gu'], M0['w_down'],
                 M0['b_down'], M0['ln_g'], M0['ln_b'], A['ident'], x2, x2T, NQ // 128, NCTX // 128)
        with ExitStack() as st:
            emit_mods(S, nc, st, A['cT'], ada_w1, ada_b1, mods1, m1T)
            S.barrier()
        G = GlaScratch(nc)
        emit_gla_proj(S, nc, x2, x2T, mods1, m1T, g_w_in, wgA, wgB, A['ident'], G)
        emit_gla_scan(S, nc, G, 'A', cmA, None, SA.rearrange("p (h d) -> p h d", h=4), oA, oAT)
        SGT = T(SG)
        S.special('gpsimd', lambda e: e.collective_compute("AllGather", ALU.bypass, replica_groups=[list(range(8))],
                                                           ins=[SA.opt()], outs=[SG.opt()]), writes=[SGT])
        with ExitStack() as st:
            ps_ = S.sbuf("ex_psel", [128, 8], F32, st)
            S.dma('sync', lambda e: e.dma_start(out=ps_[:], in_=psel[:, :]), writes=[ps_])
            gt = [S.sbuf(f"ex_g{i}", [128, DM], F32, st) for i in range(2)]
            acc = S.sbuf("ex_acc", [128, DM], F32, st)
            for r in range(8):
                g = gt[r % 2]
                S.dma('sync', lambda e: e.dma_start(out=g[:], in_=SG[r * 128:(r + 1) * 128, :]), reads=[SGT], writes=[g])
                if r == 0:
                    S.op('vector', lambda e: e.tensor_scalar_mul(out=acc[:], in0=g[:], scalar1=ps_[:, 0:1]),
                         reads=[g, ps_], writes=[acc])
                else:
                    S.op('vector', lambda e: e.scalar_tensor_tensor(out=acc[:], in0=g[:], scalar=ps_[:, r:r + 1], in1=acc[:],
                                                                    op0=ALU.mult, op1=ALU.add), reads=[g, ps_, acc], writes=[acc])
            S.dma('sync', lambda e: e.dma_start(out=SB0[:, :], in_=acc[:]), reads=[acc])
            S.barrier()
        post = dict(ident=A['ident'], w_out=g_w_out, mods_d=mods1, modsT=m1T, ln_g=g_ln_g, ln_b=g_ln_b, norm_w=norm_w,
                    xin=x2, xinT=x2T, xout=x3, xoutT=x3T)
        emit_gla_scan(S, nc, G, 'B', cmB, SB0.rearrange("p (h d) -> p h d", h=4), None, oA, oAT, post=post)
        emit_moe(S, nc, x3, x3T, mods1, m1T, M1['router_w'], M1['router_b'], M1['w_gu'], M1['b_gu'], M1['w_down'],
                 M1['b_down'], M1['ln_g'], M1['ln_b'], A['ident'], out, T(out), NOWN // 128, 0)
    return nc


def kernel(**inp):
    inp = {k: np.asarray(v) for k, v in inp.items()}
    cores = list(range(8))
    sh = shared_attn(inp)
    shg = shared_gla(inp)
    shared = dict(sh)
    for i, p in ((0, "m0_"), (1, "m1_")):
        sm = shared_moe(inp, i)
        for k in ('router_w', 'router_b', 'w_gu', 'b_gu', 'w_down', 'b_down', 'ln_g', 'ln_b'):
            shared[p + k] = sm[k]
    shared.update(ada_w1=shg['ada_w'], ada_b1=shg['ada_b'], g_w_out=shg['w_out'], norm_w=shg['norm_w'],
                  g_ln_g=shg['ln_g'], g_ln_b=shg['ln_b'])
    for k, v in shg['cm'].items():
        shared[k] = v
    maps = []
    for c in cores:
        b, hf = divmod(c, 2)
        d = prep_attn(inp, c, shared)
        sel = np.zeros((128, 8), np.float32); sel[:, c ^ 1] = 1.0
        d.update(g_w_in=shg['w_in'][hf], wgA=shg['aug'][hf], wgB=shg['aug'][1 - hf], psel=sel)
        maps.append(d)
    if 'fused' not in _NC_CACHE:
        _NC_CACHE['fused'] = build_fused()
    r = run_bass_kernel_spmd(_NC_CACHE['fused'], maps, core_ids=cores).results
    out = np.empty((4, 2 * NOWN, DM), np.float32)
    for c in cores:
        b, hf = divmod(c, 2)
        xo = r[c]['out']
        out[b, hf * NOWN:(hf + 1) * NOWN] = xo[::-1] if hf == 1 else xo
    return out


_NC_CACHE = {}


def kernel_unfused(**inp):
    inp = {k: np.asarray(v) for k, v in inp.items()}
    cores = list(range(8))
    run = lambda nc, maps: run_bass_kernel_spmd(nc, maps, core_ids=cores).results
    get = lambda name, fn: _NC_CACHE.setdefault(name, None) or _NC_CACHE.__setitem__(name, fn()) or _NC_CACHE[name]
    sh = shared_attn(inp)
    r = run(get('attn', lambda: build_attn(_lambda_init(0))), [prep_attn(inp, c, sh) for c in cores])
    mods0 = [r[c]['mods'] for c in cores]; x1 = [r[c]['x1'] for c in cores]
    shm = shared_moe(inp, 0)
    maps = []
    for c in cores:
        d = dict(shm); d['mods'] = mods0[c]; d['xin'] = x1[c]; maps.append(d)
    r = run(get('moe0', lambda: build_moe(NQ // 128, NCTX // 128)), maps)
    x2 = [r[c]['xout'] for c in cores]
    shg = shared_gla(inp)
    maps = []
    for c in cores:
        b, hf = divmod(c, 2)
        d = prep_gla_common(shg, hf, x2[c])
        d.update(cT=np.ascontiguousarray(np.stack([inp['c'][b], inp['c_ctx']], 1)), ada_w=shg['ada_w'], ada_b=shg['ada_b'],
                 MinclTA=shg['cm']['MinclTA'], MafterTA=shg['cm']['MafterTA'], maskTA=shg['cm']['maskTA'])
        maps.append(d)
    r1 = run(get('gla1', build_gla1), maps)
    maps = []
    for c in cores:
        b, hf = divmod(c, 2)
        d = prep_gla_common(shg, hf, x2[c])
        d.update(mods=r1[c]['mods'], oA=r1[c]['oA'], SB0=r1[c ^ 1]['SA'], w_out=shg['w_out'], norm_w=shg['norm_w'],
                 ln_g=shg['ln_g'], ln_b=shg['ln_b'],
                 MinclTB=shg['cm']['MinclTB'], MafterTB=shg['cm']['MafterTB'], maskTB=shg['cm']['maskTB'])
        maps.append(d)
    r2 = run(get('gla2', build_gla2), maps)
    shm = shared_moe(inp, 1)
    maps = []
    for c in cores:
        d = dict(shm); d['mods'] = r1[c]['mods']; d['xin'] = r2[c]['xout']; maps.append(d)
    r3 = run(get('moe1', lambda: build_moe(NOWN // 128, 0)), maps)
    out = np.empty((4, 2 * NOWN, DM), np.float32)
    for c in cores:
        b, hf = divmod(c, 2)
        xo = r3[c]['xout']
        out[b, hf * NOWN:(hf + 1) * NOWN] = xo[::-1] if hf == 1 else xo
    return out


kernel_fused = kernel
kernel = kernel_unfused
```
